# Optimizing a Trainium2 kernel written in Bass

```python
import math
import jax
import jax.numpy as jnp
from jax import lax
import numpy as np

D_MODEL = 2048
BATCH = 2
SEQ = 4096
DEPTH = 2
DEC_BATCH = 8
DEC_SEQ = 4
PAST_LEN = 16384
PAGE_SIZE = 128

HEAD_DIM = 128
ROPE_THETA = 10000.0
RMS_EPS = 1e-6
N_HYB = (DEPTH + 1) // 2
N_SSM = DEPTH // 2

GDN_HEADS = D_MODEL // 256
GDN_DK = 128
GDN_DV = 128
GDN_CONV = 4
GDN_CHUNK = 64
GDN_QK = GDN_HEADS * GDN_DK
GDN_VW = GDN_HEADS * GDN_DV
GDN_CONV_DIM = 2 * GDN_QK + GDN_VW

NSA_HEADS = D_MODEL // 256
NSA_KV_HEADS = 2
NSA_GROUP = NSA_HEADS // NSA_KV_HEADS
NSA_QW = NSA_HEADS * HEAD_DIM
NSA_KVW = NSA_KV_HEADS * HEAD_DIM
CMP_BLOCK = 32
CMP_STRIDE = 16
CMP_HIDDEN = 256
SEL_BLOCK = 64
SEL_TOPK = 16
WINDOW = 512
Q_BLOCK = 128
FORCE_BONUS = 1e4

HYB_SPLITS = (GDN_QK, GDN_QK, GDN_VW, GDN_VW, GDN_HEADS, GDN_HEADS,
              NSA_QW, NSA_KVW, NSA_KVW, NSA_KVW, NSA_KVW, NSA_KVW, NSA_KVW, 3 * NSA_HEADS)
HYB_IN = 2 * GDN_QK + 2 * GDN_VW + 2 * GDN_HEADS + NSA_QW + 6 * NSA_KVW + 3 * NSA_HEADS
HYB_MIX = GDN_VW + NSA_QW

SSM_EXPAND = 2
SSM_D_INNER = SSM_EXPAND * D_MODEL
SSM_HEAD_DIM = 64
SSM_HEADS = SSM_D_INNER // SSM_HEAD_DIM
SSM_GROUPS = 8
SSM_HPG = SSM_HEADS // SSM_GROUPS
SSM_STATE = 128
SSM_CONV = 4
SSM_CHUNK = 128
SSM_BC = SSM_GROUPS * SSM_STATE
SSM_CONV_DIM = SSM_D_INNER + 2 * SSM_BC
SSM_IN = SSM_D_INNER + SSM_CONV_DIM + SSM_HEADS

D_FF = 11 * D_MODEL // 4
N_EXPERTS = 8
TOP_K = 2
D_FF_EXPERT = 7 * D_MODEL // 2

kernel_name = 'hybrid_gdn_nsa_mamba2_moe_step'


def rmsnorm(x, w):
    xf = x.astype(jnp.float32)
    y = xf * lax.rsqrt(jnp.mean(xf * xf, axis=-1, keepdims=True) + RMS_EPS)
    return (y * w.astype(jnp.float32)).astype(x.dtype)


def l2norm(x):
    xf = x.astype(jnp.float32)
    return (xf * lax.rsqrt(jnp.sum(xf * xf, axis=-1, keepdims=True) + 1e-6)).astype(x.dtype)


def rope(x, pos):
    half = x.shape[-1] // 2
    inv_freq = 1.0 / (ROPE_THETA ** (jnp.arange(half, dtype=jnp.float32) / half))
    ang = pos.astype(jnp.float32)[:, None] * inv_freq[None, :]
    cos = jnp.cos(ang)[None, :, None, :]
    sin = jnp.sin(ang)[None, :, None, :]
    xf = x.astype(jnp.float32)
    x1, x2 = xf[..., :half], xf[..., half:]
    return jnp.concatenate([x1 * cos - x2 * sin, x2 * cos + x1 * sin], axis=-1).astype(x.dtype)


def split_cols(a, sizes):
    out, s = [], 0
    for n in sizes:
        out.append(a[..., s:s + n])
        s += n
    return out


def causal_dwconv(x_ext, w):
    c = x_ext.shape[-1]
    return lax.conv_general_dilated(x_ext, w[:, None, :], window_strides=(1,), padding='VALID',
                                    dimension_numbers=('NWC', 'WIO', 'NWC'), feature_group_count=c)


def masked_softmax(s, mask):
    s = jnp.where(mask, s, -jnp.inf)
    m = jnp.max(s, axis=-1, keepdims=True)
    m = jnp.where(jnp.isfinite(m), m, 0.0)
    e = jnp.where(mask, jnp.exp(s - m), 0.0)
    den = jnp.sum(e, axis=-1, keepdims=True)
    return e / jnp.where(den > 0, den, 1.0)


def to_chunks(a, n, c, heads_first):
    b, l = a.shape[:2]
    a = jnp.pad(a.astype(jnp.float32), [(0, 0), (0, n * c - l)] + [(0, 0)] * (a.ndim - 2))
    a = a.reshape((b, n, c) + a.shape[2:])
    if heads_first:
        perm = (1, 0, 3, 2) + tuple(range(4, a.ndim))
    else:
        perm = (1, 0) + tuple(range(2, a.ndim))
    return a.transpose(perm)


def gdn_chunked(q, k, v, g, beta, s0):
    b, l = q.shape[:2]
    dv = v.shape[-1]
    c = min(GDN_CHUNK, l)
    n = -(-l // c)
    qc, kc, vc = (to_chunks(a, n, c, True) for a in (q, k, v))
    gc, bc = (to_chunks(a, n, c, True) for a in (g, beta))
    incl = jnp.tri(c, dtype=bool)
    strict = jnp.tri(c, k=-1, dtype=jnp.float32)
    eye = jnp.eye(c, dtype=jnp.float32)

    def step(s, inp):
        qi, ki, vi, gi, bi = inp
        gam = jnp.cumsum(gi, axis=-1)
        dmask = jnp.exp(jnp.where(incl, gam[..., :, None] - gam[..., None, :], -jnp.inf))
        kb = ki * bi[..., None]
        a_mat = eye + jnp.einsum('bhid,bhjd->bhij', kb, ki) * dmask * strict
        rhs = jnp.concatenate([vi * bi[..., None], kb * jnp.exp(gam)[..., None]], axis=-1)
        sol = lax.linalg.triangular_solve(a_mat, rhs, left_side=True, lower=True, unit_diagonal=True)
        u, w = sol[..., :dv], sol[..., dv:]
        v_new = u - jnp.einsum('bhcd,bhde->bhce', w, s)
        attn = jnp.einsum('bhid,bhjd->bhij', qi, ki) * dmask
        o = (jnp.einsum('bhcd,bhde->bhce', qi * jnp.exp(gam)[..., None], s)
             + jnp.einsum('bhij,bhje->bhie', attn, v_new))
        g_end = gam[..., -1:]
        s = (s * jnp.exp(g_end)[..., None]
             + jnp.einsum('bhcd,bhce->bhde', ki * jnp.exp(g_end - gam)[..., None], v_new))
        return s, o

    s_fin, o = lax.scan(step, s0.astype(jnp.float32), (qc, kc, vc, gc, bc))
    o = o.transpose(1, 0, 3, 2, 4).reshape(b, n * c, q.shape[2], dv)[:, :l]
    return o.astype(v.dtype), s_fin.astype(s0.dtype)


def ssd_chunked(x, dt, a_neg, bmat, cmat, h0):
    b, l = x.shape[:2]
    c = min(SSM_CHUNK, l)
    n = -(-l // c)
    xc, dtc, bc, cc = (to_chunks(a, n, c, False) for a in (x, dt, bmat, cmat))
    incl = jnp.tri(c, dtype=bool)
    a_neg = a_neg.astype(jnp.float32)

    def step(h, inp):
        xi, dti, bi, ci = inp
        bn = dti.shape[0]
        gam = jnp.cumsum(dti * a_neg, axis=1)
        diff = gam[:, :, None, :] - gam[:, None, :, :]
        lmat = jnp.exp(jnp.where(incl[None, :, :, None], diff, -jnp.inf)).reshape(bn, c, c, SSM_GROUPS, SSM_HPG)
        xdt = (xi * dti[..., None]).reshape(bn, c, SSM_GROUPS, SSM_HPG, SSM_HEAD_DIM)
        cb = jnp.einsum('bign,bjgn->bijg', ci, bi)
        y_in = jnp.einsum('bijg,bijgh,bjghp->bighp', cb, lmat, xdt)
        h5 = h.reshape(bn, SSM_GROUPS, SSM_HPG, SSM_HEAD_DIM, SSM_STATE)
        dec_in = jnp.exp(gam).reshape(bn, c, SSM_GROUPS, SSM_HPG)
        y_st = jnp.einsum('bign,bghpn->bighp', ci, h5) * dec_in[..., None]
        g_end = gam[:, -1]
        dec_out = jnp.exp(g_end[:, None, :] - gam).reshape(bn, c, SSM_GROUPS, SSM_HPG)
        h_new = (h * jnp.exp(g_end)[:, :, None, None]
                 + jnp.einsum('bjgn,bjghp->bghpn', bi, xdt * dec_out[..., None]).reshape(h.shape))
        return h_new, (y_in + y_st).reshape(bn, c, SSM_HEADS, SSM_HEAD_DIM)

    h_fin, y = lax.scan(step, h0.astype(jnp.float32), (xc, dtc, bc, cc))
    y = y.transpose(1, 0, 2, 3, 4).reshape(b, n * c, SSM_HEADS, SSM_HEAD_DIM)[:, :l]
    return y, h_fin.astype(h0.dtype)


def compress_blocks(rows, pe, w1, w2):
    b, lk = rows.shape[:2]
    n_cmp = (lk - CMP_BLOCK) // CMP_STRIDE + 1
    per = CMP_BLOCK // CMP_STRIDE
    segs = rows[:, :(n_cmp + per - 1) * CMP_STRIDE].reshape(
        b, n_cmp + per - 1, CMP_STRIDE, 2, NSA_KV_HEADS, HEAD_DIM)
    blocks = jnp.concatenate([segs[:, i:i + n_cmp] for i in range(per)], axis=2)
    blocks = blocks + jnp.transpose(pe, (1, 0, 2))[:, :, None, :]
    flat = jnp.transpose(blocks, (0, 1, 3, 4, 2, 5)).reshape(b, n_cmp, 2, NSA_KV_HEADS, CMP_BLOCK * HEAD_DIM)
    hid = jax.nn.silu(jnp.einsum('bcskf,sfh->bcskh', flat, w1))
    out = jnp.einsum('bcskh,she->bcske', hid, w2)
    c_end = jnp.arange(n_cmp) * CMP_STRIDE + CMP_BLOCK - 1
    return out[:, :, 0], out[:, :, 1], c_end


def selection_blocks(rows):
    b, lk = rows.shape[:2]
    n_sel = -(-lk // SEL_BLOCK)
    rows = jnp.pad(rows, ((0, 0), (0, n_sel * SEL_BLOCK - lk), (0, 0), (0, 0), (0, 0)))
    blk = rows.reshape(b, n_sel, SEL_BLOCK, 2, NSA_KV_HEADS, HEAD_DIM).transpose(3, 0, 4, 1, 2, 5)
    return blk[0], blk[1]


def cmp_to_sel_map(n_cmp, n_sel):
    cs = jnp.arange(n_cmp)[:, None] * CMP_STRIDE
    ss = jnp.arange(n_sel)[None, :] * SEL_BLOCK
    return ((cs < ss + SEL_BLOCK) & (cs + CMP_BLOCK > ss)).astype(jnp.float32)


def nsa_core(q, q_pos, kc, vc, c_end, ks_blk, vs_blk, kw, vw, kw_pos, gates):
    f32 = jnp.float32
    b, lq = q.shape[:2]
    qg = q.astype(f32).reshape(b, lq, NSA_KV_HEADS, NSA_GROUP, HEAD_DIM) * HEAD_DIM ** -0.5
    s_c = jnp.einsum('bqkgd,bckd->bqkgc', qg, kc.astype(f32))
    m_c = c_end[None, :] <= q_pos[:, None]
    p_c = masked_softmax(s_c, m_c[None, :, None, None, :])
    o_c = jnp.einsum('bqkgc,bckd->bqkgd', p_c, vc.astype(f32))
    n_cmp, n_sel = kc.shape[1], ks_blk.shape[2]
    imp = jnp.einsum('bqkgc,cj->bqkj', p_c, cmp_to_sel_map(n_cmp, n_sel))
    blk = jnp.arange(n_sel)[None, :]
    cur = (q_pos // SEL_BLOCK)[:, None]
    forced = (blk == 0) | (blk == cur) | (blk == cur - 1)
    score = jnp.where((blk <= cur)[None, :, None, :],
                      imp + jnp.where(forced, FORCE_BONUS, 0.0)[None, :, None, :], -jnp.inf)
    n_top = min(SEL_TOPK, n_sel)
    top_s, top_i = lax.top_k(score, n_top)
    bi = jnp.arange(b)[:, None, None, None]
    ki = jnp.arange(NSA_KV_HEADS)[None, None, :, None]
    k_s = ks_blk[bi, ki, top_i].astype(f32).reshape(b, lq, NSA_KV_HEADS, n_top * SEL_BLOCK, HEAD_DIM)
    v_s = vs_blk[bi, ki, top_i].astype(f32).reshape(b, lq, NSA_KV_HEADS, n_top * SEL_BLOCK, HEAD_DIM)
    key_pos = top_i[..., None] * SEL_BLOCK + jnp.arange(SEL_BLOCK)
    m_s = jnp.isfinite(top_s)[..., None] & (key_pos <= q_pos[None, :, None, None, None])
    s_s = jnp.einsum('bqkgd,bqkjd->bqkgj', qg, k_s)
    p_s = masked_softmax(s_s, m_s.reshape(b, lq, NSA_KV_HEADS, 1, n_top * SEL_BLOCK))
    o_s = jnp.einsum('bqkgj,bqkjd->bqkgd', p_s, v_s)
    s_w = jnp.einsum('bqkgd,blkd->bqkgl', qg, kw.astype(f32))
    dist = q_pos[:, None] - kw_pos[None, :]
    m_w = (dist >= 0) & (dist < WINDOW) & (kw_pos[None, :] >= 0)
    p_w = masked_softmax(s_w, m_w[None, :, None, None, :])
    o_w = jnp.einsum('bqkgl,blkd->bqkgd', p_w, vw.astype(f32))
    g = gates.astype(f32)[..., None]
    o = g[:, :, 0] * o_c + g[:, :, 1] * o_s + g[:, :, 2] * o_w
    return o.reshape(b, lq, NSA_QW).astype(q.dtype)


def hybrid_mixer(h, pos0, w_in, gdn_conv_w, gdn_a_log, gdn_dt_bias, gdn_norm_w, cmp_pe, cmp_w1, cmp_w2, w_out,
                 gdn_conv_buf, gdn_s0, past_cmp, past_sel, win_buf):
    b, l, _ = h.shape
    (gq, gk, gv, gz, ga, gb, nq, ck, cv, sk, sv, wk, wv, ng) = split_cols(h @ w_in, HYB_SPLITS)
    qkv_ext = jnp.concatenate([gdn_conv_buf, jnp.concatenate([gq, gk, gv], axis=-1)], axis=1)
    new_conv = qkv_ext[:, -(GDN_CONV - 1):]
    qkv = jax.nn.silu(causal_dwconv(qkv_ext, gdn_conv_w))
    q_a, k_a, v_a = split_cols(qkv, (GDN_QK, GDN_QK, GDN_VW))
    q_a = l2norm(q_a.reshape(b, l, GDN_HEADS, GDN_DK)) * GDN_DK ** -0.5
    k_a = l2norm(k_a.reshape(b, l, GDN_HEADS, GDN_DK))
    v_a = v_a.reshape(b, l, GDN_HEADS, GDN_DV)
    beta = jax.nn.sigmoid(gb.astype(jnp.float32))
    g_log = -jnp.exp(gdn_a_log.astype(jnp.float32)) * jax.nn.softplus(ga.astype(jnp.float32) + gdn_dt_bias.astype(jnp.float32))
    o_a, s_new = gdn_chunked(q_a, k_a, v_a, g_log, beta, gdn_s0)
    o_a = rmsnorm(o_a, gdn_norm_w) * jax.nn.silu(gz.reshape(b, l, GDN_HEADS, GDN_DV))
    gdn_out = o_a.reshape(b, l, GDN_VW)
    pos = pos0 + jnp.arange(l)
    q_b = rope(nq.reshape(b, l, NSA_HEADS, HEAD_DIM), pos)
    kvr = lambda a: a.reshape(b, l, NSA_KV_HEADS, HEAD_DIM)
    cmp_rows = jnp.stack([rope(kvr(ck), pos), kvr(cv)], axis=2)
    sel_rows = jnp.stack([rope(kvr(sk), pos), kvr(sv)], axis=2)
    win_rows = jnp.stack([rope(kvr(wk), pos), kvr(wv)], axis=2)
    gates = jax.nn.sigmoid(ng.astype(jnp.float32)).reshape(b, l, 3, NSA_KV_HEADS, NSA_GROUP)
    cmp_all = cmp_rows if past_cmp is None else jnp.concatenate([past_cmp, cmp_rows], axis=1)
    sel_all = sel_rows if past_sel is None else jnp.concatenate([past_sel, sel_rows], axis=1)
    kc, vc, c_end = compress_blocks(cmp_all, cmp_pe, cmp_w1, cmp_w2)
    ks_blk, vs_blk = selection_blocks(sel_all)
    if win_buf is None:
        qb_len = min(Q_BLOCK, l)
        n_qb = l // qb_len
        kw_pad = jnp.pad(win_rows, ((0, 0), (WINDOW, 0), (0, 0), (0, 0), (0, 0)))

        def query_block(i):
            s0 = i * qb_len
            q_blk = lax.dynamic_slice_in_dim(q_b, s0, qb_len, axis=1)
            g_blk = lax.dynamic_slice_in_dim(gates, s0, qb_len, axis=1)
            kw_blk = lax.dynamic_slice_in_dim(kw_pad, s0, WINDOW + qb_len, axis=1)
            q_pos = pos0 + s0 + jnp.arange(qb_len)
            kw_pos = pos0 + s0 - WINDOW + jnp.arange(WINDOW + qb_len)
            return nsa_core(q_blk, q_pos, kc, vc, c_end, ks_blk, vs_blk,
                            kw_blk[:, :, 0], kw_blk[:, :, 1], kw_pos, g_blk)

        nsa_out = lax.map(query_block, jnp.arange(n_qb))
        nsa_out = jnp.moveaxis(nsa_out, 0, 1).reshape(b, l, NSA_QW)
        new_win = win_rows[:, -min(WINDOW, l):]
    else:
        w_buf = win_buf.shape[1]
        win_all = jnp.concatenate([win_buf, win_rows], axis=1)
        kw_pos = pos0 - w_buf + jnp.arange(w_buf + l)
        nsa_out = nsa_core(q_b, pos, kc, vc, c_end, ks_blk, vs_blk,
                           win_all[:, :, 0], win_all[:, :, 1], kw_pos, gates)
        new_win = win_all[:, -w_buf:]
    out = jnp.concatenate([gdn_out, nsa_out], axis=-1) @ w_out
    return out, cmp_rows, sel_rows, new_win, new_conv, s_new


def ssm_mixer(h, w_in, conv_w, conv_b, a_log, dt_bias, d_skip, norm_w, w_out, conv_buf, h0):
    b, l, _ = h.shape
    z, xbc, dt = split_cols(h @ w_in, (SSM_D_INNER, SSM_CONV_DIM, SSM_HEADS))
    xbc_ext = jnp.concatenate([conv_buf, xbc], axis=1)
    new_conv = xbc_ext[:, -(SSM_CONV - 1):]
    xbc = jax.nn.silu(causal_dwconv(xbc_ext, conv_w) + conv_b)
    x, bm, cm = split_cols(xbc, (SSM_D_INNER, SSM_BC, SSM_BC))
    x = x.reshape(b, l, SSM_HEADS, SSM_HEAD_DIM)
    bm = bm.reshape(b, l, SSM_GROUPS, SSM_STATE)
    cm = cm.reshape(b, l, SSM_GROUPS, SSM_STATE)
    dt = jax.nn.softplus(dt.astype(jnp.float32) + dt_bias.astype(jnp.float32))
    a_neg = -jnp.exp(a_log.astype(jnp.float32))
    y, h_new = ssd_chunked(x, dt, a_neg, bm, cm, h0)
    y = y + d_skip.astype(jnp.float32)[:, None] * x.astype(jnp.float32)
    y = y.reshape(b, l, SSM_D_INNER) * jax.nn.silu(z.astype(jnp.float32))
    y = rmsnorm(y.reshape(b, l, SSM_GROUPS, SSM_D_INNER // SSM_GROUPS), norm_w.reshape(SSM_GROUPS, -1))
    return y.reshape(b, l, SSM_D_INNER).astype(h.dtype) @ w_out, new_conv, h_new


def swiglu(h, w_gate, w_up, w_down):
    return (jax.nn.silu(h @ w_gate) * (h @ w_up)) @ w_down


def moe_ffn(h, router, w_gate, w_up, w_down):
    logits = jnp.einsum('bld,de->ble', h, router).astype(jnp.float32)
    top_v, top_i = lax.top_k(logits, TOP_K)
    top_w = jax.nn.softmax(top_v, axis=-1)
    gate = jnp.sum(jax.nn.one_hot(top_i, N_EXPERTS, dtype=jnp.float32) * top_w[..., None], axis=-2)
    out = jnp.zeros(h.shape, jnp.float32)
    for e in range(N_EXPERTS):
        out = out + swiglu(h, w_gate[e], w_up[e], w_down[e]).astype(jnp.float32) * gate[..., e:e + 1]
    return out.astype(h.dtype)


def gather_pages(pool, page_table):
    g = pool[page_table]
    return g.reshape((g.shape[0], g.shape[1] * g.shape[2]) + g.shape[3:])


def _normal(key, shape, scale):
    return jax.random.normal(key, shape, jnp.float32) * scale


def _gain(key, shape):
    return 1.0 + 0.02 * jax.random.normal(key, shape, jnp.float32)


def _a_log(key, shape):
    return jnp.log(jax.random.uniform(key, shape, jnp.float32, 1.0, 16.0))


def _dt_bias(key, shape):
    dt = jnp.exp(jax.random.uniform(key, shape, jnp.float32, math.log(1e-3), math.log(1e-1)))
    return jnp.log(jnp.expm1(dt))


def setup_inputs(seed: int = 0) -> dict:
    key = jax.random.key(seed)
    k = jax.random.split(key, 48)
    n_pages = PAST_LEN // PAGE_SIZE
    n_used = DEC_BATCH * n_pages
    n_phys = n_used + n_used // 4
    w_buf = min(WINDOW, PAST_LEN)
    page_table = jax.random.permutation(k[9], n_phys)[:n_used].reshape(DEC_BATCH, n_pages).astype(jnp.int32)
    return {
        'x_prompt': _normal(k[0], (BATCH, SEQ, D_MODEL), 1.0),
        'x_sample': _normal(k[1], (DEC_BATCH, DEC_SEQ, D_MODEL), 1.0),
        'cache_cmp_kv': _normal(k[2], (N_HYB, n_phys, PAGE_SIZE, 2, NSA_KV_HEADS, HEAD_DIM), 1.0),
        'cache_sel_kv': _normal(k[3], (N_HYB, n_phys, PAGE_SIZE, 2, NSA_KV_HEADS, HEAD_DIM), 1.0),
        'cache_win_kv': _normal(k[4], (N_HYB, DEC_BATCH, w_buf, 2, NSA_KV_HEADS, HEAD_DIM), 1.0),
        'state_gdn_conv': _normal(k[5], (N_HYB, DEC_BATCH, GDN_CONV - 1, GDN_CONV_DIM), 1.0),
        'state_gdn': _normal(k[6], (N_HYB, DEC_BATCH, GDN_HEADS, GDN_DK, GDN_DV), 0.1),
        'state_ssm_conv': _normal(k[7], (N_SSM, DEC_BATCH, SSM_CONV - 1, SSM_CONV_DIM), 1.0),
        'state_ssm': _normal(k[8], (N_SSM, DEC_BATCH, SSM_HEADS, SSM_HEAD_DIM, SSM_STATE), 0.05),
        'page_table': page_table,
        'hyb_norm_mix': _gain(k[10], (N_HYB, D_MODEL)),
        'hyb_w_in': _normal(k[11], (N_HYB, D_MODEL, HYB_IN), D_MODEL ** -0.5),
        'hyb_gdn_conv_w': _normal(k[12], (N_HYB, GDN_CONV, GDN_CONV_DIM), GDN_CONV ** -0.5),
        'hyb_gdn_a_log': _a_log(k[13], (N_HYB, GDN_HEADS)),
        'hyb_gdn_dt_bias': _dt_bias(k[14], (N_HYB, GDN_HEADS)),
        'hyb_gdn_norm_w': _gain(k[15], (N_HYB, GDN_DV)),
        'hyb_cmp_pe': _normal(k[16], (N_HYB, 2, CMP_BLOCK, HEAD_DIM), 0.1),
        'hyb_cmp_w1': _normal(k[17], (N_HYB, 2, CMP_BLOCK * HEAD_DIM, CMP_HIDDEN), (CMP_BLOCK * HEAD_DIM) ** -0.5),
        'hyb_cmp_w2': _normal(k[18], (N_HYB, 2, CMP_HIDDEN, HEAD_DIM), CMP_HIDDEN ** -0.5),
        'hyb_w_out': _normal(k[19], (N_HYB, HYB_MIX, D_MODEL), HYB_MIX ** -0.5),
        'hyb_norm_ffn': _gain(k[20], (N_HYB, D_MODEL)),
        'ffn_w_gate': _normal(k[21], (N_HYB, D_MODEL, D_FF), D_MODEL ** -0.5),
        'ffn_w_up': _normal(k[22], (N_HYB, D_MODEL, D_FF), D_MODEL ** -0.5),
        'ffn_w_down': _normal(k[23], (N_HYB, D_FF, D_MODEL), D_FF ** -0.5),
        'ssm_norm_mix': _gain(k[24], (N_SSM, D_MODEL)),
        'ssm_w_in': _normal(k[25], (N_SSM, D_MODEL, SSM_IN), D_MODEL ** -0.5),
        'ssm_conv_w': _normal(k[26], (N_SSM, SSM_CONV, SSM_CONV_DIM), SSM_CONV ** -0.5),
        'ssm_conv_b': _normal(k[27], (N_SSM, SSM_CONV_DIM), 0.02),
        'ssm_a_log': _a_log(k[28], (N_SSM, SSM_HEADS)),
        'ssm_dt_bias': _dt_bias(k[29], (N_SSM, SSM_HEADS)),
        'ssm_d_skip': 1.0 + _normal(k[30], (N_SSM, SSM_HEADS), 0.1),
        'ssm_norm_w': _gain(k[31], (N_SSM, SSM_D_INNER)),
        'ssm_w_out': _normal(k[32], (N_SSM, SSM_D_INNER, D_MODEL), SSM_D_INNER ** -0.5),
        'ssm_norm_ffn': _gain(k[33], (N_SSM, D_MODEL)),
        'moe_router': _normal(k[34], (N_SSM, D_MODEL, N_EXPERTS), D_MODEL ** -0.5),
        'moe_w_gate': _normal(k[35], (N_SSM, N_EXPERTS, D_MODEL, D_FF_EXPERT), D_MODEL ** -0.5),
        'moe_w_up': _normal(k[36], (N_SSM, N_EXPERTS, D_MODEL, D_FF_EXPERT), D_MODEL ** -0.5),
        'moe_w_down': _normal(k[37], (N_SSM, N_EXPERTS, D_FF_EXPERT, D_MODEL), D_FF_EXPERT ** -0.5),
        'final_norm': _gain(k[38], (D_MODEL,)),
    }


def reference(x_prompt, x_sample, cache_cmp_kv, cache_sel_kv, cache_win_kv, state_gdn_conv, state_gdn,
              state_ssm_conv, state_ssm, page_table, hyb_norm_mix, hyb_w_in, hyb_gdn_conv_w, hyb_gdn_a_log,
              hyb_gdn_dt_bias, hyb_gdn_norm_w, hyb_cmp_pe, hyb_cmp_w1, hyb_cmp_w2, hyb_w_out, hyb_norm_ffn,
              ffn_w_gate, ffn_w_up, ffn_w_down, ssm_norm_mix, ssm_w_in, ssm_conv_w, ssm_conv_b, ssm_a_log,
              ssm_dt_bias, ssm_d_skip, ssm_norm_w, ssm_w_out, ssm_norm_ffn, moe_router, moe_w_gate, moe_w_up,
              moe_w_down, final_norm):
    hp, hs = x_prompt, x_sample
    bp = hp.shape[0]
    (cmp_p, cmp_s, sel_p, sel_s, win_p, win_s, gconv_p, gconv_s,
     gst_p, gst_s, sconv_p, sconv_s, sst_p, sst_s) = ([] for _ in range(14))
    for layer in range(DEPTH):
        i = layer // 2
        if layer % 2 == 0:
            hw = (hyb_w_in[i], hyb_gdn_conv_w[i], hyb_gdn_a_log[i], hyb_gdn_dt_bias[i], hyb_gdn_norm_w[i],
                  hyb_cmp_pe[i], hyb_cmp_w1[i], hyb_cmp_w2[i], hyb_w_out[i])
            zero_conv = jnp.zeros((bp, GDN_CONV - 1, GDN_CONV_DIM), hp.dtype)
            zero_s = jnp.zeros((bp, GDN_HEADS, GDN_DK, GDN_DV), hp.dtype)
            m, c_rows, s_rows, w_rows, conv_new, s_new = hybrid_mixer(
                rmsnorm(hp, hyb_norm_mix[i]), 0, *hw, zero_conv, zero_s, None, None, None)
            hp = hp + m
            hp = hp + swiglu(rmsnorm(hp, hyb_norm_ffn[i]), ffn_w_gate[i], ffn_w_up[i], ffn_w_down[i])
            cmp_p.append(c_rows); sel_p.append(s_rows); win_p.append(w_rows)
            gconv_p.append(conv_new); gst_p.append(s_new)
            past_cmp = gather_pages(cache_cmp_kv[i], page_table)
            past_sel = gather_pages(cache_sel_kv[i], page_table)
            m, c_rows, s_rows, w_rows, conv_new, s_new = hybrid_mixer(
                rmsnorm(hs, hyb_norm_mix[i]), PAST_LEN, *hw, state_gdn_conv[i], state_gdn[i],
                past_cmp, past_sel, cache_win_kv[i])
            hs = hs + m
            hs = hs + swiglu(rmsnorm(hs, hyb_norm_ffn[i]), ffn_w_gate[i], ffn_w_up[i], ffn_w_down[i])
            cmp_s.append(c_rows); sel_s.append(s_rows); win_s.append(w_rows)
            gconv_s.append(conv_new); gst_s.append(s_new)
        else:
            sw = (ssm_w_in[i], ssm_conv_w[i], ssm_conv_b[i], ssm_a_log[i], ssm_dt_bias[i], ssm_d_skip[i],
                  ssm_norm_w[i], ssm_w_out[i])
            mw = (moe_router[i], moe_w_gate[i], moe_w_up[i], moe_w_down[i])
            zero_conv = jnp.zeros((bp, SSM_CONV - 1, SSM_CONV_DIM), hp.dtype)
            zero_h = jnp.zeros((bp, SSM_HEADS, SSM_HEAD_DIM, SSM_STATE), hp.dtype)
            m, conv_new, h_new = ssm_mixer(rmsnorm(hp, ssm_norm_mix[i]), *sw, zero_conv, zero_h)
            hp = hp + m
            hp = hp + moe_ffn(rmsnorm(hp, ssm_norm_ffn[i]), *mw)
            sconv_p.append(conv_new); sst_p.append(h_new)
            m, conv_new, h_new = ssm_mixer(rmsnorm(hs, ssm_norm_mix[i]), *sw, state_ssm_conv[i], state_ssm[i])
            hs = hs + m
            hs = hs + moe_ffn(rmsnorm(hs, ssm_norm_ffn[i]), *mw)
            sconv_s.append(conv_new); sst_s.append(h_new)
    y_prompt = rmsnorm(hp, final_norm)
    y_sample = rmsnorm(hs, final_norm)
    new_cmp_kv_prompt, new_cmp_kv_sample = jnp.stack(cmp_p), jnp.stack(cmp_s)
    new_sel_kv_prompt, new_sel_kv_sample = jnp.stack(sel_p), jnp.stack(sel_s)
    new_win_kv_prompt, new_win_kv_sample = jnp.stack(win_p), jnp.stack(win_s)
    new_gdn_conv_prompt, new_gdn_conv_sample = jnp.stack(gconv_p), jnp.stack(gconv_s)
    new_gdn_state_prompt, new_gdn_state_sample = jnp.stack(gst_p), jnp.stack(gst_s)
    new_ssm_conv_prompt, new_ssm_conv_sample = jnp.stack(sconv_p), jnp.stack(sconv_s)
    new_ssm_state_prompt, new_ssm_state_sample = jnp.stack(sst_p), jnp.stack(sst_s)
    return (y_prompt, y_sample,
            new_cmp_kv_prompt, new_cmp_kv_sample,
            new_sel_kv_prompt, new_sel_kv_sample,
            new_win_kv_prompt, new_win_kv_sample,
            new_gdn_conv_prompt, new_gdn_conv_sample,
            new_gdn_state_prompt, new_gdn_state_sample,
            new_ssm_conv_prompt, new_ssm_conv_sample,
            new_ssm_state_prompt, new_ssm_state_sample)
```

```python
import numpy as np
import concourse.bass as bass
import concourse.mybir as mybir
from concourse.bass_utils import run_bass_kernel_spmd
from contextlib import ExitStack

F32 = mybir.dt.float32
BF16 = mybir.dt.bfloat16
I32 = mybir.dt.int32
AF = mybir.ActivationFunctionType
ALU = mybir.AluOpType
AX = mybir.AxisListType

D = 2048
HYB_IN = 6696
NFM = 3072
NPROJ = HYB_IN - NFM
C_GZ, C_GA, C_GB, C_NQ, C_CK, C_CV, C_SK, C_SV, C_WK, C_WV, C_NG = (
    0, 1024, 1032, 1040, 2064, 2320, 2576, 2832, 3088, 3344, 3600)
PAST = 16384
EPS = 1e-6


class Tk:
    __slots__ = ("w", "r", "excl")

    def __init__(self, excl=False):
        self.w = None
        self.r = {}
        self.excl = excl


class Prog:
    ENG = ("pe", "act", "dve", "pool", "sp")

    def __init__(self, nc, es, ndma=24):
        self.nc = nc
        self.es = es
        self.ops = {e: [] for e in self.ENG}
        self.cnt = {e: 0 for e in self.ENG}
        self.sem = {e: es.enter_context(nc.semaphore("s_" + e)) for e in self.ENG}
        self.ndma = ndma
        self.dsem = [es.enter_context(nc.semaphore("d%d" % i)) for i in range(ndma)]
        self.dcnt = [0] * ndma
        self.dnext = 0
        self.seen = {e: {} for e in self.ENG}
        self.uid = 0
        self.total = 0
        self.limit = None
        self.icnt = 0
        self.isem = es.enter_context(nc.semaphore("isem"))
        self.iscr = es.enter_context(nc.sbuf_tensor("iscr", [128, 1], F32))

    def name(self, s):
        self.uid += 1
        return "%s_%d" % (s, self.uid)

    def sb(self, st, name, shape, dt=F32):
        return st.enter_context(self.nc.sbuf_tensor(self.name(name), list(shape), dt))

    def ps(self, st, name, shape, dt=F32):
        esz = 4 if dt == F32 else 2
        n = 1
        for d in shape[1:]:
            n *= d
        assert n * esz <= 2048
        t = st.enter_context(self.nc.psum_tensor(self.name(name), [128, 2048 // esz], dt))
        v = t[:, 0:n]
        if len(shape) == 3:
            v = v.rearrange("p (a b) -> p a b", a=shape[1])
        return v

    def dram(self, name, shape, dt=F32, kind="Internal"):
        return self.nc.dram_tensor(name, list(shape), dt, kind=kind).ap()

    def _need(self, eng, tok, waits):
        if tok is None:
            return
        if tok[0] == "e":
            _, e2, idx = tok
            if e2 == eng and self.cnt[eng] - idx > 1:
                return
            key = ("e", e2)
            val = idx
        else:
            _, k, val = tok
            key = ("d", k)
        if self.seen[eng].get(key, 0) >= val:
            return
        if waits.get(key, 0) < val:
            waits[key] = val

    def _deps(self, eng, reads, writes, waits=None):
        waits = {} if waits is None else waits
        for t in reads:
            self._need(eng, t.w, waits)
        for t in writes:
            self._need(eng, t.w, waits)
            for tok in t.r.values():
                self._need(eng, tok, waits)
        wl = []
        for key, val in waits.items():
            self.seen[eng][key] = val
            sem = self.sem[key[1]] if key[0] == "e" else self.dsem[key[1]]
            wl.append((sem, val))
        return wl

    def _mark(self, tok, reads, writes):
        rk = tok[:2]
        for t in reads:
            t.r[rk] = tok
        for t in writes:
            t.w = tok
            t.r = {}

    def op(self, eng, fn, reads=(), writes=()):
        self.total += 1
        if self.limit is not None and self.total > self.limit:
            return None
        if any(t.excl for t in reads):
            writes = list(writes) + [t for t in reads if t.excl]
            reads = [t for t in reads if not t.excl]
        wl = self._deps(eng, reads, writes)
        self.cnt[eng] += 1
        tok = ("e", eng, self.cnt[eng])
        self.ops[eng].append((wl, fn, (self.sem[eng], 1)))
        self._mark(tok, reads, writes)
        return tok

    def dma(self, out, in_, reads=(), writes=(), q="sp", **kw):
        self.total += 1
        if self.limit is not None and self.total > self.limit:
            return None
        k = self.dnext
        self.dnext = (self.dnext + 1) % self.ndma
        waits = {}
        if self.dcnt[k] > 0:
            self._need(q, ("d", k, self.dcnt[k]), waits)
        wl = self._deps(q, reads, writes, waits)
        self.dcnt[k] += 16
        tok = ("d", k, self.dcnt[k])
        self.ops[q].append((wl, (lambda e, o=out, i=in_, kw=kw: e.dma_start(out=o, in_=i, **kw)),
                            (self.dsem[k], 16)))
        self._mark(tok, reads, writes)
        return tok

    def idma(self, out, in_, idx, reads=(), writes=()):
        if self.total is not None:
            self.total += 1
            if self.limit is not None and self.total > self.limit:
                return None
        wl = self._deps("pool", reads, writes)
        self.cnt["pool"] += 1
        tok = ("e", "pool", self.cnt["pool"])
        isem, iscr = self.isem, self.iscr
        self.icnt += 16
        target = self.icnt

        def fn(e, o=out, i=in_, x=idx, target=target):
            e.indirect_dma_start(out=o, out_offset=None, in_=i,
                                 in_offset=bass.IndirectOffsetOnAxis(ap=x, axis=0)).then_inc(isem, 16)
            e.wait_ge(isem, target)
            return e.memset(iscr[:], 0.0)
        self.ops["pool"].append((wl, fn, (self.sem["pool"], 1)))
        self._mark(tok, reads, writes)
        return tok

    def flush(self, final=False):
        wl = [(self.dsem[k], self.dcnt[k]) for k in range(self.ndma) if self.dcnt[k] > 0]
        for e in ("pe", "act", "dve", "pool"):
            if self.cnt[e] > 0:
                wl.append((self.sem[e], self.cnt[e]))
        for e in self.ENG:
            self.ops[e].append((list(wl), None, None))
            for k in range(self.ndma):
                self.seen[e][("d", k)] = self.dcnt[k]
            for e2 in ("pe", "act", "dve", "pool"):
                self.seen[e][("e", e2)] = self.cnt[e2]
        ops = self.ops
        self.ops = {e: [] for e in self.ENG}
        with self.nc.Block() as block:
            def run(eng, lst):
                for wl, fn, inc in lst:
                    for sem, val in wl:
                        eng.wait_ge(sem, val)
                    if fn is not None:
                        fn(eng).then_inc(inc[0], inc[1])

            @block.tensor
            def _(e):
                run(e, ops["pe"])

            @block.scalar
            def _(e):
                run(e, ops["act"])

            @block.vector
            def _(e):
                run(e, ops["dve"])

            @block.gpsimd
            def _(e):
                run(e, ops["pool"])

            @block.sync
            def _(e):
                run(e, ops["sp"])

    def mm(self, out, lhsT, rhs, start=True, stop=True, reads=(), writes=()):
        return self.op("pe", lambda e: e.matmul(out, lhsT, rhs, start=start, stop=stop), reads, writes)

    def tr(self, out, in_, ident, reads=(), writes=()):
        return self.op("pe", lambda e: e.transpose(out, in_, ident), reads, writes)

    def act(self, out, in_, func, reads=(), writes=(), **kw):
        return self.op("act", lambda e: e.activation(out, in_, func, **kw), reads, writes)

    def copy(self, eng, out, in_, reads=(), writes=()):
        if eng == "act":
            return self.op("act", lambda e: e.copy(out, in_), reads, writes)
        return self.op(eng, lambda e: e.tensor_copy(out, in_), reads, writes)


class Ctx:
    pass


_FILL_REGS = {}
I_NPHYS = [1280]


def fillreg(e, val):
    key = (id(e), val)
    if key not in _FILL_REGS:
        _FILL_REGS[key] = e.to_reg(val)
    return _FILL_REGS[key]


def make_consts(P, C):
    st = C.glob
    C.idf = P.sb(st, "idf", [128, 128], F32)
    C.idb = P.sb(st, "idb", [128, 128], BF16)
    C.t_id = Tk()
    P.op("pool", lambda e: e.memset(C.idf[:], 1.0), writes=[C.t_id])
    P.op("pool", lambda e: e.affine_select(C.idf[:], C.idf[:], [[-1, 128]], ALU.is_equal, fillreg(e, 0.0),
                                           base=0, channel_multiplier=1), reads=[C.t_id], writes=[C.t_id])
    P.op("dve", lambda e: e.tensor_copy(C.idb[:], C.idf[:]), reads=[C.t_id], writes=[C.t_id])


def norm_linear(P, C, x_ap, gamma_ap, w_ap, ntiles, n_out, fm_cols, fm_out, tm_out, t_out, sb_tiles=17,
                cb=256, t_in=None, norm=True, resid=None, t_res=None):
    Dm = x_ap.shape[1]
    KC = Dm // 128
    with ExitStack() as st:
        XT = P.sb(st, "XT", [128, KC, sb_tiles * 128], BF16)
        t_XT = Tk()
        t_g = Tk()
        rd_in = [t_in] if t_in is not None else []
        rd_res = [t_res] if t_res is not None else []
        if norm:
            gt = P.sb(st, "gt", [128, Dm])
            P.dma(gt[:], gamma_ap.partition_broadcast(128), writes=[t_g])
        rb = [P.sb(st, "rb", [128, cb]) for _ in range(2)] if resid is not None else None
        t_rb = [Tk(), Tk()]
        xt = [P.sb(st, "xt", [128, Dm]) for _ in range(2)]
        t_x = [Tk(), Tk()]
        sq = P.sb(st, "sq", [128, Dm], BF16)
        t_sq = Tk()
        ss = [P.sb(st, "ss", [128, 1]) for _ in range(2)]
        t_ss = [Tk(), Tk()]
        xn = [P.sb(st, "xn", [128, Dm], BF16) for _ in range(2)]
        t_xn = [Tk(), Tk()]
        ptr = [P.ps(st, "ptr", [128, 4, 128], BF16) for _ in range(2)]
        t_ptr = [Tk(1), Tk(1)]
        wf = [P.sb(st, "wf", [128, KC, cb]) for _ in range(2)]
        t_wf = [Tk(), Tk()]
        wb = [P.sb(st, "wb", [128, KC, cb], BF16) for _ in range(2)]
        t_wb = [Tk(), Tk()]
        po = [P.ps(st, "po", [128, 512]) for _ in range(3)]
        t_po = [Tk(1) for _ in range(3)]
        ob = [P.sb(st, "ob", [128, 512]) for _ in range(3)]
        t_ob = [Tk() for _ in range(3)]
        ipo = 0
        iw = 0
        for s0 in range(0, ntiles, sb_tiles):
            nts = min(sb_tiles, ntiles - s0)
            for i in range(nts):
                b = i % 2
                r0 = (s0 + i) * 128
                P.dma(xt[b][:], x_ap[r0:r0 + 128, :], reads=rd_in, writes=[t_x[b]])
                if norm:
                    P.act(sq[:], xt[b][:], AF.Square, reads=[t_x[b]], writes=[t_sq, t_ss[b]], accum_out=ss[b][:])
                    P.op("dve", lambda e, b=b: e.tensor_scalar(ss[b][:], ss[b][:], 1.0 / Dm, EPS, ALU.mult, ALU.add),
                         reads=[t_ss[b]], writes=[t_ss[b]])
                    P.act(ss[b][:], ss[b][:], AF.Sqrt, reads=[t_ss[b]], writes=[t_ss[b]])
                    P.op("dve", lambda e, b=b: e.reciprocal(ss[b][:], ss[b][:]), reads=[t_ss[b]], writes=[t_ss[b]])
                    P.op("dve", lambda e, b=b: e.scalar_tensor_tensor(xn[b][:], xt[b][:], ss[b][:, 0:1], gt[:],
                                                                      ALU.mult, ALU.mult),
                         reads=[t_x[b], t_ss[b], t_g], writes=[t_xn[b]])
                else:
                    P.op("dve", lambda e, b=b: e.tensor_copy(xn[b][:], xt[b][:]), reads=[t_x[b]], writes=[t_xn[b]])
                for kg in range(KC // 4):
                    pb = kg % 2
                    for j in range(4):
                        k = kg * 4 + j
                        P.tr(ptr[pb][:, j, :], xn[b][:, k * 128:(k + 1) * 128], C.idb[:],
                             reads=[t_xn[b], C.t_id], writes=[t_ptr[pb]])
                    P.copy("dve" if kg % 2 == 0 else "act", XT[:, kg * 4:kg * 4 + 4, i * 128:(i + 1) * 128],
                           ptr[pb][:], reads=[t_ptr[pb]], writes=[t_XT])
            for c0 in range(0, n_out, cb):
                nc_ = min(cb, n_out - c0)
                w_ = iw % 2
                iw += 1
                P.dma(wf[w_][:, :, 0:nc_], w_ap[:, c0:c0 + nc_].rearrange("(k p) n -> p k n", p=128),
                      writes=[t_wf[w_]])
                P.op("pool", lambda e, w_=w_, nc_=nc_: e.tensor_copy(wb[w_][:, :, 0:nc_], wf[w_][:, :, 0:nc_]),
                     reads=[t_wf[w_]], writes=[t_wb[w_]])
                if c0 < fm_cols:
                    assert c0 + nc_ <= fm_cols
                    for h0 in range(0, nc_, 128):
                        for g0 in range(0, nts * 128, 512):
                            gw = min(512, nts * 128 - g0)
                            p_ = ipo % 3
                            ipo += 1
                            for k in range(KC):
                                P.mm(po[p_][:, 0:gw], wb[w_][:, k, h0:h0 + 128], XT[:, k, g0:g0 + gw],
                                     start=(k == 0), stop=(k == KC - 1), reads=[t_wb[w_], t_XT], writes=[t_po[p_]])
                            P.copy("act" if p_ % 2 else "dve", ob[p_][:, 0:gw], po[p_][:, 0:gw],
                                   reads=[t_po[p_]], writes=[t_ob[p_]])
                            P.dma(fm_out[c0 + h0:c0 + h0 + 128, s0 * 128 + g0:s0 * 128 + g0 + gw], ob[p_][:, 0:gw],
                                  reads=[t_ob[p_]], writes=[t_out])
                else:
                    for i in range(nts):
                        p_ = ipo % 3
                        ipo += 1
                        for k in range(KC):
                            P.mm(po[p_][:, 0:nc_], XT[:, k, i * 128:(i + 1) * 128], wb[w_][:, k, 0:nc_],
                                 start=(k == 0), stop=(k == KC - 1), reads=[t_wb[w_], t_XT], writes=[t_po[p_]])
                        r0 = (s0 + i) * 128
                        if resid is not None:
                            rb_ = ipo % 2
                            P.dma(rb[rb_][:, 0:nc_], resid[r0:r0 + 128, c0 - fm_cols:c0 - fm_cols + nc_], reads=rd_res, writes=[t_rb[rb_]])
                            P.op("dve", lambda e, p_=p_, rb_=rb_, nc_=nc_: e.tensor_tensor(ob[p_][:, 0:nc_], po[p_][:, 0:nc_], rb[rb_][:, 0:nc_], ALU.add),
                                 reads=[t_po[p_], t_rb[rb_]], writes=[t_ob[p_]])
                        else:
                            P.copy("act" if p_ % 2 else "dve", ob[p_][:, 0:nc_], po[p_][:, 0:nc_],
                                   reads=[t_po[p_]], writes=[t_ob[p_]])
                        P.dma(tm_out[r0:r0 + 128, c0 - fm_cols:c0 - fm_cols + nc_], ob[p_][:, 0:nc_],
                              reads=[t_ob[p_]], writes=[t_out])
        P.flush()


def kv_outputs(P, C, PROJ, QKVT, ROPE, win_cache, outs, TP, t_A, KVR=None, KT=None, t_kvr=None):
    NTP = TP // 128
    t_o = outs["t"]
    with ExitStack() as st:
        kv = [P.sb(st, "kv", [128, 1536]) for _ in range(2)]
        t_kv = [Tk(), Tk()]
        rp = [P.sb(st, "rp", [128, 128]) for _ in range(2)]
        t_rp = [Tk(), Tk()]
        tmp = [P.sb(st, "tmp", [128, 4, 2, 64]) for _ in range(2)]
        t_tmp = [Tk(), Tk()]
        pkt = [P.ps(st, "pkt", [128, 4, 128]) for _ in range(2)]
        t_pkt = [Tk(1), Tk(1)]
        ktb = [P.sb(st, "ktb", [128, 4, 128]) for _ in range(2)]
        t_ktb = [Tk(), Tk()]
        for i in range(NTP + 1):
            b = i % 2
            r0 = i * 128
            P.dma(kv[b][:], PROJ[r0:r0 + 128, C_CK:C_CK + 1536], reads=[t_A], writes=[t_kv[b]])
            P.dma(rp[b][:], ROPE[r0:r0 + 128, :], writes=[t_rp[b]])
            cosb = rp[b][:, 0:64].unsqueeze(1).to_broadcast([128, 2, 64])
            sinb = rp[b][:, 64:128].unsqueeze(1).to_broadcast([128, 2, 64])
            for j in range(3):
                eng = "dve" if j != 1 else "pool"
                kk = kv[b][:, j * 512:j * 512 + 256].rearrange("p (h t d) -> p h t d", h=2, t=2)
                x1 = kk[:, :, 0, :]
                x2 = kk[:, :, 1, :]
                T = tmp[b]
                rd = [t_kv[b], t_rp[b]]
                P.op(eng, lambda e, T=T, x1=x1, cosb=cosb: e.tensor_tensor(T[:, 0], x1, cosb, ALU.mult), reads=rd, writes=[t_tmp[b]])
                P.op(eng, lambda e, T=T, x2=x2, sinb=sinb: e.tensor_tensor(T[:, 1], x2, sinb, ALU.mult), reads=rd, writes=[t_tmp[b]])
                P.op(eng, lambda e, T=T, x2=x2, cosb=cosb: e.tensor_tensor(T[:, 2], x2, cosb, ALU.mult), reads=rd, writes=[t_tmp[b]])
                P.op(eng, lambda e, T=T, x1=x1, sinb=sinb: e.tensor_tensor(T[:, 3], x1, sinb, ALU.mult), reads=rd, writes=[t_tmp[b]])
                P.op(eng, lambda e, T=T, x1=x1: e.tensor_tensor(x1, T[:, 0], T[:, 1], ALU.subtract), reads=[t_tmp[b]], writes=[t_kv[b]])
                P.op(eng, lambda e, T=T, x2=x2: e.tensor_tensor(x2, T[:, 2], T[:, 3], ALU.add), reads=[t_tmp[b]], writes=[t_kv[b]])
            if KVR is not None:
                P.dma(KVR[r0:r0 + 128, :], kv[b][:], reads=[t_kv[b]], writes=[t_kvr])
                for g_, cols in enumerate(((0, 128, 256, 384), (512, 640, 1024, 1152))):
                    for j, c_ in enumerate(cols):
                        P.tr(pkt[g_][:, j, :], kv[b][:, c_:c_ + 128], C.idf[:], reads=[t_kv[b], C.t_id], writes=[t_pkt[g_]])
                    P.copy("act", ktb[g_][:], pkt[g_][:], reads=[t_pkt[g_]], writes=[t_ktb[g_]])
                    P.dma(KT[g_ * 4:g_ * 4 + 4, :, r0:r0 + 128].rearrange("a d t -> d a t"), ktb[g_][:],
                          reads=[t_ktb[g_]], writes=[t_kvr])
            if i < NTP:
                P.dma(outs["cmp_p"][r0:r0 + 128, :], kv[b][:, 0:512], reads=[t_kv[b]], writes=[t_o])
                P.dma(outs["sel_p"][r0:r0 + 128, :], kv[b][:, 512:1024], reads=[t_kv[b]], writes=[t_o])
                wr = r0 - (TP - min(512, TP))
                if wr >= 0:
                    P.dma(outs["win_p"][wr:wr + 128, :], kv[b][:, 1024:1536], reads=[t_kv[b]], writes=[t_o])
            else:
                P.dma(outs["cmp_s"][0:4, :], kv[b][0:4, 0:512], reads=[t_kv[b]], writes=[t_o])
                P.dma(outs["sel_s"][0:4, :], kv[b][0:4, 512:1024], reads=[t_kv[b]], writes=[t_o])
                P.dma(outs["win_s"][508:512, :], kv[b][0:4, 1024:1536], reads=[t_kv[b]], writes=[t_o])
        P.dma(outs["win_s"][0:508, :], win_cache[4:512, :], writes=[t_o])
        for r in range(3):
            P.dma(outs["gconv_p"][r:r + 1, :], QKVT[:, TP - 3 + r:TP - 2 + r].rearrange("c o -> o c"),
                  reads=[t_A], writes=[t_o], allow_slow_non_contiguous=True)
            P.dma(outs["gconv_s"][r:r + 1, :], QKVT[:, TP + 1 + r:TP + 2 + r].rearrange("c o -> o c"),
                  reads=[t_A], writes=[t_o], allow_slow_non_contiguous=True)
        P.flush()


def host_nsa_consts(TP):
    i = np.arange(128)
    caus = (i[None, :] <= i[:, None]).astype(np.float32)
    n_cmp = (TP - 32) // 16 + 1
    ncp = -(-n_cmp // 128) * 128
    nsel = max(8, TP // 64)
    cs = np.arange(ncp)[:, None] * 16
    ss = np.arange(nsel)[None, :] * 64
    m = ((cs < ss + 64) & (cs + 32 > ss)).astype(np.float32)
    m[n_cmp:] = 0
    return caus, m


def host_consts():
    i = np.arange(128)
    same = (i[:, None] // 64) == (i[None, :] // 64)
    c = {}
    c["TRI"] = (same & (i[:, None] <= i[None, :])).astype(np.float32)
    c["EEND"] = (i[:, None] == (i[None, :] // 64) * 64 + 63).astype(np.float32)
    c["E0"] = np.repeat((i == 63)[:, None], 128, 1).astype(np.float32)
    c["E1"] = np.repeat((i == 127)[:, None], 128, 1).astype(np.float32)
    c["MADD"] = np.where(same & (i[:, None] >= i[None, :]), 0.0, 1e4).astype(np.float32)
    c["MSTR"] = (same & (i[:, None] > i[None, :])).astype(np.float32)
    c["ONES"] = np.ones((128, 128), np.float32)
    return np.concatenate([c[k] for k in ("TRI", "EEND", "E0", "E1", "MADD", "MSTR", "ONES")], axis=1)


def softplus(P, st, x, t_x, n, tag):
    a = P.sb(st, tag + "a", [128, n])
    t_a = Tk()
    P.act(a[:], x, AF.Abs, reads=[t_x], writes=[t_a])
    P.act(a[:], a[:], AF.Exp, reads=[t_a], writes=[t_a], scale=-1.0)
    P.act(a[:], a[:], AF.Ln, reads=[t_a], writes=[t_a], bias=1.0)
    P.op("dve", lambda e: e.tensor_scalar_max(x, x, 0.0), reads=[t_x], writes=[t_x])
    P.op("dve", lambda e: e.tensor_add(x, x, a[:]), reads=[t_x, t_a], writes=[t_x])


def gdn_stage(P, C, QKVT, PROJ, MIX, I, outs, TP, t_A, t_mix):
    NTP = TP // 128
    with ExitStack() as st:
        K_ = P.sb(st, "gconst", [128, 7, 128])
        t_K = Tk()
        P.dma(K_[:], I["GCONST"].rearrange("p (k n) -> p k n", k=7), writes=[t_K])
        TRI, EEND, E0, E1, MADD, MSTR, ONES = (K_[:, k, :] for k in range(7))
        cw = P.sb(st, "cw", [128, 8, 3, 4])
        P.dma(cw[:], I["gdn_cw"], writes=[t_K])
        alog = P.sb(st, "alog", [128, 8])
        dtb = P.sb(st, "dtb", [128, 8])
        gnw = P.sb(st, "gnw", [128, 128])
        P.dma(alog[:], I["gdn_a_log"].partition_broadcast(128), writes=[t_K])
        P.dma(dtb[:], I["gdn_dt_bias"].partition_broadcast(128), writes=[t_K])
        P.dma(gnw[:], I["gdn_norm_w"].partition_broadcast(128), writes=[t_K])
        P.act(alog[:], alog[:], AF.Exp, reads=[t_K], writes=[t_K])
        P.op("dve", lambda e: e.tensor_scalar_mul(alog[:], alog[:], -1.0), reads=[t_K], writes=[t_K])
        S = [P.sb(st, "S", [128, 8, 128]) for _ in range(2)]
        t_S = [[Tk() for _ in range(8)] for _ in range(2)]
        P.op("pool", lambda e: e.memset(S[0][:], 0.0), writes=t_S[0])
        qgm = [P.sb(st, "qgm", [128, 128]) for _ in range(2)]
        kdm = [P.sb(st, "kdm", [128, 128]) for _ in range(2)]
        t_qgm, t_kdm = [Tk(), Tk()], [Tk(), Tk()]
        for b in range(2):
            P.op("pool", lambda e, b=b: e.memset(qgm[b][:], 0.0), writes=[t_qgm[b]])
            P.op("pool", lambda e, b=b: e.memset(kdm[b][:], 0.0), writes=[t_kdm[b]])
        vnew = P.sb(st, "vnew", [128, 128])
        t_vn = Tk()
        P.op("pool", lambda e: e.memset(vnew[:], 0.0), writes=[t_vn])
        gab = P.sb(st, "gab", [128, 16]); t_gab = Tk()
        vm = P.sb(st, "vm", [128, 1]); t_vm = Tk()
        gz = P.sb(st, "gz", [128, 1024]); t_gz = Tk()
        beta = P.sb(st, "beta", [128, 8]); nbeta = P.sb(st, "nbeta", [128, 8]); t_beta = Tk()
        g = P.sb(st, "g", [128, 8]); t_g = Tk()
        gam = P.sb(st, "gam", [128, 8]); t_gam = Tk()
        sc = P.sb(st, "sc", [128, 5, 8]); t_sc = Tk()
        mixo = P.sb(st, "mixo", [128, 1024]); t_mixo = Tk()
        pg = P.ps(st, "pg", [128, 4, 8]); t_pg = Tk(1)
        xq = [P.sb(st, "xq", [128, 3, 131]) for _ in range(2)]; t_xq = [Tk(), Tk()]
        acc = P.sb(st, "acc", [128, 3, 128]); t_acc = Tk()
        tm = P.sb(st, "tm", [128, 3, 128]); t_tm = Tk()
        ssq = P.sb(st, "ssq", [128, 2]); t_ssq = Tk()
        junk = P.sb(st, "junk", [128, 128]); t_junk = Tk()
        qn = P.sb(st, "qn", [128, 128]); kn = P.sb(st, "kn", [128, 128]); t_qk = Tk()
        kbg = P.sb(st, "kbg", [128, 128]); vb = P.sb(st, "vb", [128, 128]); t_kv = Tk()
        fT = P.sb(st, "fT", [128, 4, 128]); t_fT = Tk()
        diag = P.sb(st, "diag", [128, 128]); t_diag = Tk()
        dm = P.sb(st, "dm", [128, 128]); t_dm = Tk()
        X = [P.sb(st, "X", [128, 2, 128]) for _ in range(2)]; t_X = [Tk(), Tk()]
        PT = P.sb(st, "PT", [128, 128]); t_PT = Tk()
        attn = P.sb(st, "attn", [128, 128]); attnT = P.sb(st, "attnT", [128, 128]); t_at = Tk(); t_atT = Tk()
        uw = P.sb(st, "uw", [128, 2, 128]); t_uw = Tk()
        o_sb = P.sb(st, "o_sb", [128, 128]); t_o = Tk()
        oss = P.sb(st, "oss", [128, 1]); t_oss = Tk()
        pA = P.ps(st, "pA", [128, 4, 128]); t_pA = Tk(1)
        pB = P.ps(st, "pB", [128, 4, 128]); t_pB = Tk(1)
        pC = P.ps(st, "pC", [128, 128]); t_pC = Tk(1)
        pD = P.ps(st, "pD", [128, 128]); t_pD = Tk(1)
        pE = P.ps(st, "pE", [128, 4, 128]); t_pE = Tk(1)
        pF = P.ps(st, "pF", [128, 2, 128]); t_pF = Tk(1)
        pS = P.ps(st, "pS", [128, 128]); t_pS = Tk(1)
        idf = C.idf
        cur = 0
        ih = 0
        for i in range(NTP + 1):
            r0 = i * 128
            sample = (i == NTP)
            if sample:
                P.dma(outs["gst_p"].rearrange("h k v -> k h v"), S[cur][:], reads=t_S[cur], writes=[outs["t"]])
                P.dma(S[cur][:], I["gdn_state"].rearrange("h k v -> k h v"), writes=t_S[cur])
            P.dma(gab[:], PROJ[r0:r0 + 128, C_GA:C_GA + 16], reads=[t_A], writes=[t_gab])
            P.dma(vm[:], I["VMASK"][r0:r0 + 128, :], writes=[t_vm])
            P.dma(gz[:], PROJ[r0:r0 + 128, C_GZ:C_GZ + 1024], reads=[t_A], writes=[t_gz])
            P.act(gz[:], gz[:], AF.Silu, reads=[t_gz], writes=[t_gz])
            P.act(beta[:], gab[:, 8:16], AF.Sigmoid, reads=[t_gab], writes=[t_beta])
            P.op("dve", lambda e: e.tensor_scalar(beta[:], beta[:], vm[:, 0:1], None, ALU.mult), reads=[t_beta, t_vm], writes=[t_beta])
            P.op("dve", lambda e: e.tensor_scalar_mul(nbeta[:], beta[:], -1.0), reads=[t_beta], writes=[t_beta])
            P.op("dve", lambda e: e.tensor_add(g[:], gab[:, 0:8], dtb[:]), reads=[t_gab, t_K], writes=[t_g])
            softplus(P, st, g[:], t_g, 8, "sp%d" % i)
            P.op("dve", lambda e: e.tensor_mul(g[:], g[:], alog[:]), reads=[t_g, t_K], writes=[t_g])
            P.op("dve", lambda e: e.tensor_scalar(g[:], g[:], vm[:, 0:1], None, ALU.mult), reads=[t_g, t_vm], writes=[t_g])
            P.mm(pg[:, 0, :], TRI, g[:], reads=[t_K, t_g], writes=[t_pg])
            P.copy("dve", gam[:], pg[:, 0, :], reads=[t_pg], writes=[t_gam])
            P.mm(pg[:, 1, :], EEND, gam[:], reads=[t_K, t_gam], writes=[t_pg])
            P.mm(pg[:, 2, :], E0, gam[:], reads=[t_K, t_gam], writes=[t_pg])
            P.mm(pg[:, 3, :], E1, gam[:], reads=[t_K, t_gam], writes=[t_pg])
            P.act(sc[:, 0, :], gam[:], AF.Exp, reads=[t_gam], writes=[t_sc])
            P.op("dve", lambda e: e.tensor_sub(sc[:, 1, :], pg[:, 1, :], gam[:]), reads=[t_pg, t_gam], writes=[t_sc])
            P.act(sc[:, 1, :], sc[:, 1, :], AF.Exp, reads=[t_sc], writes=[t_sc])
            P.op("dve", lambda e: e.tensor_mul(sc[:, 2, :], sc[:, 0, :], beta[:]), reads=[t_sc, t_beta], writes=[t_sc])
            P.act(sc[:, 3, :], pg[:, 2, :], AF.Exp, reads=[t_pg], writes=[t_sc])
            P.act(sc[:, 4, :], pg[:, 3, :], AF.Exp, reads=[t_pg], writes=[t_sc])
            for h in range(8):
                xb = ih % 2
                ih += 1
                src = QKVT.rearrange("(w c) t -> c w t", w=3)[h * 128:(h + 1) * 128]
                if i == 0:
                    P.op("pool", lambda e, xb=xb: e.memset(xq[xb][:, :, 0:3], 0.0), writes=[t_xq[xb]])
                    P.dma(xq[xb][:, :, 3:131], src[:, :, 0:128], reads=[t_A], writes=[t_xq[xb]])
                elif sample:
                    P.dma(xq[xb][:, :, 0:3], I["gdn_convT"].rearrange("(w c) t -> c w t", w=3)[h * 128:(h + 1) * 128],
                          writes=[t_xq[xb]])
                    P.dma(xq[xb][:, :, 3:131], src[:, :, r0:r0 + 128], reads=[t_A], writes=[t_xq[xb]])
                else:
                    P.dma(xq[xb][:], src[:, :, r0 - 3:r0 + 128], reads=[t_A], writes=[t_xq[xb]])
                for w in range(3):
                    eng = "dve"
                    for j in range(4):
                        if j == 0:
                            P.op(eng, lambda e, xb=xb, w=w, h=h: e.tensor_scalar(acc[:, w, :], xq[xb][:, w, 0:128], cw[:, h, w, 0:1], None, ALU.mult),
                                 reads=[t_xq[xb], t_K], writes=[t_acc])
                        else:
                            P.op(eng, lambda e, xb=xb, w=w, h=h, j=j: e.scalar_tensor_tensor(acc[:, w, :], xq[xb][:, w, j:j + 128], cw[:, h, w, j:j + 1], acc[:, w, :], ALU.mult, ALU.add),
                                 reads=[t_xq[xb], t_K, t_acc], writes=[t_acc])
                P.act(acc[:], acc[:], AF.Silu, reads=[t_acc], writes=[t_acc])
                for w in range(3):
                    P.tr(pA[:, w, :], acc[:, w, :], idf[:], reads=[t_acc, C.t_id], writes=[t_pA])
                P.copy("dve", tm[:], pA[:, 0:3, :], reads=[t_pA], writes=[t_tm])
                P.act(junk[:], tm[:, 0, :], AF.Square, reads=[t_tm], writes=[t_junk, t_ssq], accum_out=ssq[:, 0:1])
                P.act(junk[:], tm[:, 1, :], AF.Square, reads=[t_tm], writes=[t_junk, t_ssq], accum_out=ssq[:, 1:2])
                P.op("dve", lambda e: e.tensor_scalar_add(ssq[:], ssq[:], 1e-6), reads=[t_ssq], writes=[t_ssq])
                P.act(ssq[:], ssq[:], AF.Sqrt, reads=[t_ssq], writes=[t_ssq])
                P.op("dve", lambda e: e.reciprocal(ssq[:], ssq[:]), reads=[t_ssq], writes=[t_ssq])
                P.op("dve", lambda e: e.tensor_scalar(qn[:], tm[:, 0, :], ssq[:, 0:1], 128.0 ** -0.5, ALU.mult, ALU.mult), reads=[t_tm, t_ssq], writes=[t_qk])
                P.op("dve", lambda e: e.tensor_scalar(kn[:], tm[:, 1, :], ssq[:, 1:2], None, ALU.mult), reads=[t_tm, t_ssq], writes=[t_qk])
                P.op("pool", lambda e, h=h: e.tensor_scalar(kbg[:], kn[:], sc[:, 2, h:h + 1], None, ALU.mult), reads=[t_qk, t_sc], writes=[t_kv])
                P.op("pool", lambda e, h=h: e.tensor_scalar(vb[:], tm[:, 2, :], beta[:, h:h + 1], None, ALU.mult), reads=[t_tm, t_beta], writes=[t_kv])
                for a in range(2):
                    rs = slice(a * 64, a * 64 + 64)
                    P.op("dve", lambda e, a=a, rs=rs, h=h: e.tensor_scalar(qgm[a][rs, :], qn[rs, :], sc[rs, 0, h:h + 1], None, ALU.mult), reads=[t_qk, t_sc], writes=[t_qgm[a]])
                    P.op("pool", lambda e, a=a, rs=rs, h=h: e.tensor_scalar(kdm[a][rs, :], kn[rs, :], sc[rs, 1, h:h + 1], None, ALU.mult), reads=[t_qk, t_sc], writes=[t_kdm[a]])
                P.tr(pB[:, 0, :], kn[:], idf[:], reads=[t_qk, C.t_id], writes=[t_pB])
                P.tr(pB[:, 1, :], qn[:], idf[:], reads=[t_qk, C.t_id], writes=[t_pB])
                P.tr(pB[:, 2, :], qgm[0][:], idf[:], reads=[t_qgm[0], C.t_id], writes=[t_pB])
                P.tr(pB[:, 3, :], qgm[1][:], idf[:], reads=[t_qgm[1], C.t_id], writes=[t_pB])
                P.copy("act", fT[:], pB[:], reads=[t_pB], writes=[t_fT])
                kT, qT = fT[:, 0, :], fT[:, 1, :]
                P.op("dve", lambda e, h=h: e.tensor_scalar(diag[:], idf[:], gam[:, h:h + 1], None, ALU.mult), reads=[C.t_id, t_gam], writes=[t_diag])
                P.mm(pC[:], ONES, diag[:], start=True, stop=False, reads=[t_K, t_diag], writes=[t_pC])
                P.mm(pC[:], idf[:], MADD, start=False, stop=True, reads=[t_K, C.t_id], writes=[t_pC])
                P.act(dm[:], pC[:], AF.Exp, reads=[t_pC, t_gam], writes=[t_dm], scale=-1.0, bias=gam[:, h:h + 1])
                P.mm(pD[:], kT, kT, reads=[t_fT], writes=[t_pD])
                x0 = X[0]
                P.op("dve", lambda e: e.tensor_tensor(x0[:, 0, :], pD[:], dm[:], ALU.mult), reads=[t_pD, t_dm], writes=[t_X[0]])
                P.op("dve", lambda e, h=h: e.scalar_tensor_tensor(x0[:, 0, :], x0[:, 0, :], nbeta[:, h:h + 1], MSTR, ALU.mult, ALU.mult), reads=[t_X[0], t_beta, t_K], writes=[t_X[0]])
                P.mm(pC[:], qT, kT, reads=[t_fT], writes=[t_pC])
                P.op("dve", lambda e: e.tensor_tensor(attn[:], pC[:], dm[:], ALU.mult), reads=[t_pC, t_dm], writes=[t_at])
                P.tr(pE[:, 0, :], x0[:, 0, :], idf[:], reads=[t_X[0], C.t_id], writes=[t_pE])
                P.tr(pE[:, 1, :], attn[:], idf[:], reads=[t_at, C.t_id], writes=[t_pE])
                P.copy("act", x0[:, 1, :], pE[:, 0, :], reads=[t_pE], writes=[t_X[0]])
                P.copy("act", attnT[:], pE[:, 1, :], reads=[t_pE], writes=[t_atT])
                P.op("dve", lambda e: e.tensor_add(PT[:], pE[:, 0, :], idf[:]), reads=[t_pE, C.t_id], writes=[t_PT])
                xc = 0
                for step in range(5):
                    xa, xn_ = X[xc], X[1 - xc]
                    P.mm(pE[:, 2, :], xa[:, 1, :], xa[:, 0, :], reads=[t_X[xc]], writes=[t_pE])
                    if step < 4:
                        P.mm(pE[:, 3, :], xa[:, 0, :], xa[:, 1, :], reads=[t_X[xc]], writes=[t_pE])
                        P.copy("act", xn_[:], pE[:, 2:4, :], reads=[t_pE], writes=[t_X[1 - xc]])
                    else:
                        P.copy("act", xn_[:, 0, :], pE[:, 2, :], reads=[t_pE], writes=[t_X[1 - xc]])
                    P.mm(pD[:], xn_[:, 0, :], PT[:], reads=[t_X[1 - xc], t_PT], writes=[t_pD])
                    P.op("dve", lambda e: e.tensor_add(PT[:], pD[:], PT[:]), reads=[t_pD, t_PT], writes=[t_PT])
                    xc = 1 - xc
                P.mm(pF[:, 0, :], PT[:], vb[:], reads=[t_PT, t_kv], writes=[t_pF])
                P.mm(pF[:, 1, :], kbg[:], PT[:], reads=[t_PT, t_kv], writes=[t_pF])
                P.copy("act", uw[:], pF[:], reads=[t_pF], writes=[t_uw])
                u, wT = uw[:, 0, :], uw[:, 1, :]
                Sa, Sb = S[cur], S[1 - cur]
                tSa, tSb = t_S[cur][h], t_S[1 - cur][h]
                P.mm(pS[:], wT, Sa[:, h, :], reads=[t_uw, tSa], writes=[t_pS])
                P.op("dve", lambda e: e.tensor_sub(vnew[0:64, :], u[0:64, :], pS[0:64, :]), reads=[t_uw, t_pS], writes=[t_vn])
                P.mm(pD[:], kdm[0][:], vnew[:], reads=[t_kdm[0], t_vn], writes=[t_pD])
                P.op("dve", lambda e, h=h, Sa=Sa, Sb=Sb: e.scalar_tensor_tensor(Sb[:, h, :], Sa[:, h, :], sc[:, 3, h:h + 1], pD[:], ALU.mult, ALU.add),
                     reads=[tSa, t_sc, t_pD], writes=[tSb])
                P.mm(pS[:], wT, Sb[:, h, :], reads=[t_uw, tSb], writes=[t_pS])
                P.op("dve", lambda e: e.tensor_sub(vnew[64:128, :], u[64:128, :], pS[64:128, :]), reads=[t_uw, t_pS], writes=[t_vn])
                P.mm(pC[:], fT[:, 2, :], Sa[:, h, :], start=True, stop=False, reads=[t_fT, tSa], writes=[t_pC])
                P.mm(pC[:], fT[:, 3, :], Sb[:, h, :], start=False, stop=False, reads=[t_fT, tSb], writes=[t_pC])
                P.mm(pC[:], attnT[:], vnew[:], start=False, stop=True, reads=[t_atT, t_vn], writes=[t_pC])
                P.mm(pD[:], kdm[1][:], vnew[:], reads=[t_kdm[1], t_vn], writes=[t_pD])
                P.op("dve", lambda e, h=h, Sa=Sa, Sb=Sb: e.scalar_tensor_tensor(Sa[:, h, :], Sb[:, h, :], sc[:, 4, h:h + 1], pD[:], ALU.mult, ALU.add),
                     reads=[tSb, t_sc, t_pD], writes=[tSa])
                P.copy("act", o_sb[:], pC[:], reads=[t_pC], writes=[t_o])
                P.act(junk[:], o_sb[:], AF.Square, reads=[t_o], writes=[t_junk, t_oss], accum_out=oss[:])
                P.op("dve", lambda e: e.tensor_scalar(oss[:], oss[:], 1.0 / 128, EPS, ALU.mult, ALU.add), reads=[t_oss], writes=[t_oss])
                P.act(oss[:], oss[:], AF.Sqrt, reads=[t_oss], writes=[t_oss])
                P.op("dve", lambda e: e.reciprocal(oss[:], oss[:]), reads=[t_oss], writes=[t_oss])
                P.op("dve", lambda e: e.scalar_tensor_tensor(o_sb[:], o_sb[:], oss[:, 0:1], gnw[:], ALU.mult, ALU.mult), reads=[t_o, t_oss, t_K], writes=[t_o])
                P.op("dve", lambda e, h=h: e.tensor_mul(mixo[:, h * 128:(h + 1) * 128], o_sb[:], gz[:, h * 128:(h + 1) * 128]), reads=[t_o, t_gz], writes=[t_mixo])
            P.dma(MIX[r0:r0 + 128, 0:1024], mixo[:], reads=[t_mixo], writes=[t_mix])
        P.dma(outs["gst_s"].rearrange("h k v -> k h v"), S[cur][:], reads=t_S[cur], writes=[outs["t"]])
        P.flush()


def compress(P, C, CT, n_cmp, I, KCT, VC, t_ct, t_kc):
    with ExitStack() as st:
        w1 = P.sb(st, "w1", [128, 32, 256]); t_w1 = Tk()
        w2 = P.sb(st, "w2", [128, 2, 128]); t_w2 = Tk()
        pe = P.sb(st, "pe", [128, 32]); t_pe = Tk()
        rows = P.sb(st, "rows", [128, 4112]); t_rows = Tk()
        AT = P.sb(st, "AT", [128, 4112]); BT = P.sb(st, "BT", [128, 4112]); t_AB = Tk()
        hidT = P.sb(st, "hidT", [128, 2, 256]); t_hid = Tk()
        ph = [P.ps(st, "ph", [128, 256]) for _ in range(2)]; t_ph = [Tk(1), Tk(1)]
        pk = P.ps(st, "pk", [128, 256]); t_pk = Tk(1)
        P.op("pool", lambda e: e.memset(KCT[:], 0.0), writes=[t_kc])
        P.op("pool", lambda e: e.memset(VC[:], 0.0), writes=[t_kc])
        for s_ in range(2):
            P.dma(w1[:], I["cmp_w1"][s_].rearrange("(r d) h -> d r h", d=128), writes=[t_w1])
            P.dma(w2[:], I["cmp_w2"][s_].rearrange("(c h) e -> h c e", h=128), writes=[t_w2])
            P.dma(pe[:], I["cmp_peT"][s_], writes=[t_pe])
            for kvh in range(2):
                a = s_ * 2 + kvh
                for c0 in range(0, n_cmp, 256):
                    nb = min(256, n_cmp - c0)
                    ncol = 16 * nb + 16
                    P.dma(rows[:, 0:ncol], CT[a, :, 16 * c0:16 * c0 + ncol], reads=[t_ct], writes=[t_rows])
                    rv = rows[:, 0:ncol].rearrange("p (m r) -> p m r", r=16)
                    av = AT[:, 0:ncol].rearrange("p (m r) -> p m r", r=16)
                    bv = BT[:, 0:ncol].rearrange("p (m r) -> p m r", r=16)
                    plo = pe[:, 0:16].unsqueeze(1).to_broadcast([128, nb + 1, 16])
                    phi = pe[:, 16:32].unsqueeze(1).to_broadcast([128, nb + 1, 16])
                    P.op("dve", lambda e, av=av, rv=rv, plo=plo: e.tensor_tensor(av, rv, plo, ALU.add), reads=[t_rows, t_pe], writes=[t_AB])
                    P.op("pool", lambda e, bv=bv, rv=rv, phi=phi: e.tensor_tensor(bv, rv, phi, ALU.add), reads=[t_rows, t_pe], writes=[t_AB])
                    for hc in range(2):
                        for r in range(32):
                            src = AT if r < 16 else BT
                            P.mm(ph[hc][:, 0:nb], w1[:, r, hc * 128:(hc + 1) * 128], src[:, r:r + 16 * (nb - 1) + 1:16],
                                 start=(r == 0), stop=(r == 31), reads=[t_w1, t_AB], writes=[t_ph[hc]])
                        P.act(hidT[:, hc, 0:nb], ph[hc][:, 0:nb], AF.Silu, reads=[t_ph[hc]], writes=[t_hid])
                    if s_ == 0:
                        for hc in range(2):
                            P.mm(pk[:, 0:nb], w2[:, hc, :], hidT[:, hc, 0:nb], start=(hc == 0), stop=(hc == 1),
                                 reads=[t_w2, t_hid], writes=[t_pk])
                        P.copy("dve", KCT[:, kvh, c0:c0 + nb], pk[:, 0:nb], reads=[t_pk], writes=[t_kc])
                    else:
                        for sub in range(0, nb, 128):
                            ns = min(128, nb - sub)
                            for hc in range(2):
                                P.mm(pk[0:ns, 0:128], hidT[:, hc, sub:sub + ns], w2[:, hc, :], start=(hc == 0), stop=(hc == 1),
                                     reads=[t_w2, t_hid], writes=[t_pk])
                            P.copy("dve", VC[0:ns, (c0 + sub) // 128, kvh, :], pk[0:ns, 0:128], reads=[t_pk], writes=[t_kc])
        P.flush()


def nsa_qtiles(P, C, tiles, X, PROJ, ROPE, MIX, I, KCT, VC, t_A, t_kvr, t_kc, t_mix):
    ncp, nsel = X["ncp"], X["nsel"]
    ncc = ncp // 128
    nkmax = max(t["nk"] for t in tiles)
    with ExitStack() as st:
        msel = P.sb(st, "msel", [128, ncc, nsel]); t_c = Tk()
        P.dma(msel[:], X["msel"].rearrange("(c p) j -> p c j", p=128), writes=[t_c])
        caus = P.sb(st, "caus", [128, 128])
        P.dma(caus[:], I["CAUS"], writes=[t_c])
        qr = P.sb(st, "qr", [128, 8, 2, 64]); t_qr = Tk()
        rp = P.sb(st, "rp", [128, 128]); t_rp = Tk()
        tmp = P.sb(st, "tmpq", [128, 4, 8, 64]); t_tmp = Tk()
        qT = P.sb(st, "qT", [128, 8, 128]); t_qT = Tk()
        gts = P.sb(st, "gts", [128, 24]); t_gts = Tk()
        O = P.sb(st, "Oall", [128, 8, 128]); t_O = Tk()
        sc = P.sb(st, "sc", [128, ncp]); t_sc = Tk()
        st4 = P.sb(st, "st4", [128, 4]); t_st = Tk()
        pcT = P.sb(st, "pcT", [128, ncc, 128]); t_pcT = Tk()
        score = P.sb(st, "score", [128, nsel]); work = P.sb(st, "work", [128, nsel]); t_score = Tk()
        bonus = P.sb(st, "bonus", [128, nsel]); t_bon = Tk()
        m8 = P.sb(st, "m8", [128, 16]); t_m8 = Tk()
        mk = P.sb(st, "mk", [128, nsel]); t_mk = Tk()
        S = P.sb(st, "Sall", [128, nkmax]); t_S = Tk()
        ktc = [P.sb(st, "ktc", [128, 2048]) for _ in range(2)]; t_ktc = [Tk(), Tk()]
        vch = [P.sb(st, "vch", [128, 16, 128]) for _ in range(2)]; t_vch = [Tk(), Tk()]
        pT = [P.sb(st, "pT", [128, 4, 128]) for _ in range(2)]; t_pT = [Tk(), Tk()]
        wS = P.sb(st, "wS", [128, 640]); t_wS = Tk()
        wkt = P.sb(st, "wkt", [128, 640]); t_wkt = Tk()
        wv = P.sb(st, "wv", [128, 5, 128]); t_wv = Tk()
        psc = [P.ps(st, "psc", [128, 512]) for _ in range(2)]; t_psc = [Tk(1), Tk(1)]
        ptr = [P.ps(st, "ptrn", [128, 4, 128]) for _ in range(2)]; t_ptr = [Tk(1), Tk(1)]
        po = P.ps(st, "pon", [128, 128]); t_po = Tk(1)
        pimp = P.ps(st, "pimp", [128, nsel]); t_pimp = Tk(1)
        idf = C.idf
        ik = 0; iv = 0; ipt = 0; ips = 0
        for T in tiles:
            r0, qpos0, nk = T["r0"], T["qpos0"], T["nk"]
            nblk = nk // 64
            P.dma(qr[:], PROJ[r0:r0 + 128, C_NQ:C_NQ + 1024].rearrange("p (h t d) -> p h t d", h=8, t=2), reads=[t_A], writes=[t_qr])
            P.dma(rp[:], ROPE[r0:r0 + 128, :], writes=[t_rp])
            P.dma(gts[:], PROJ[r0:r0 + 128, C_NG:C_NG + 24], reads=[t_A], writes=[t_gts])
            P.act(gts[:], gts[:], AF.Sigmoid, reads=[t_gts], writes=[t_gts])
            cosb = rp[:, 0:64].unsqueeze(1).to_broadcast([128, 8, 64])
            sinb = rp[:, 64:128].unsqueeze(1).to_broadcast([128, 8, 64])
            x1, x2 = qr[:, :, 0, :], qr[:, :, 1, :]
            rd = [t_qr, t_rp]
            P.op("dve", lambda e: e.tensor_tensor(tmp[:, 0], x1, cosb, ALU.mult), reads=rd, writes=[t_tmp])
            P.op("pool", lambda e: e.tensor_tensor(tmp[:, 1], x2, sinb, ALU.mult), reads=rd, writes=[t_tmp])
            P.op("dve", lambda e: e.tensor_tensor(tmp[:, 2], x2, cosb, ALU.mult), reads=rd, writes=[t_tmp])
            P.op("pool", lambda e: e.tensor_tensor(tmp[:, 3], x1, sinb, ALU.mult), reads=rd, writes=[t_tmp])
            P.op("dve", lambda e: e.tensor_tensor(x1, tmp[:, 0], tmp[:, 1], ALU.subtract), reads=[t_tmp], writes=[t_qr])
            P.op("dve", lambda e: e.tensor_tensor(x2, tmp[:, 2], tmp[:, 3], ALU.add), reads=[t_tmp], writes=[t_qr])
            qf = qr[:].rearrange("p h t d -> p h (t d)")
            for g_ in range(2):
                for j in range(4):
                    P.tr(ptr[g_][:, j, :], qf[:, g_ * 4 + j, :], idf[:], reads=[t_qr, C.t_id], writes=[t_ptr[g_]])
                P.act(qT[:, g_ * 4:g_ * 4 + 4, :], ptr[g_][:], AF.Copy, reads=[t_ptr[g_]], writes=[t_qT], scale=128.0 ** -0.5)
            P.op("pool", lambda e: e.memset(bonus[:], 1e4), writes=[t_bon])
            P.op("pool", lambda e, qpos0=qpos0: e.affine_select(bonus[:], bonus[:], [[-64, nsel]], ALU.is_ge, fillreg(e, 0.0), base=qpos0, channel_multiplier=1), reads=[t_bon], writes=[t_bon])
            P.op("pool", lambda e, qpos0=qpos0: e.affine_select(bonus[:], bonus[:], [[64, nsel]], ALU.is_ge, fillreg(e, 0.0), base=127 - qpos0, channel_multiplier=-1), reads=[t_bon], writes=[t_bon])
            P.op("pool", lambda e: e.memset(bonus[:, 0:1], 1e4), reads=[t_bon], writes=[t_bon])
            for kvh in range(2):
                for g in range(4):
                    h = kvh * 4 + g
                    for c0 in range(0, ncp, 512):
                        cw_ = min(512, ncp - c0)
                        p_ = ips % 2; ips += 1
                        P.mm(psc[p_][:, 0:cw_], qT[:, h, :], KCT[:, kvh, c0:c0 + cw_], reads=[t_qT, t_kc], writes=[t_psc[p_]])
                        P.copy("act", sc[:, c0:c0 + cw_], psc[p_][:, 0:cw_], reads=[t_psc[p_]], writes=[t_sc])
                    P.op("pool", lambda e, qpos0=qpos0: e.affine_select(sc[:], sc[:], [[-16, ncp]], ALU.is_ge, fillreg(e, -1e30), base=qpos0 - 31, channel_multiplier=1), reads=[t_sc], writes=[t_sc])
                    P.op("dve", lambda e: e.reduce_max(st4[:, 0:1], sc[:], AX.X), reads=[t_sc], writes=[t_st])
                    P.op("dve", lambda e: e.tensor_scalar(st4[:, 0:1], st4[:, 0:1], -1e20, -1.0, ALU.max, ALU.mult), reads=[t_st], writes=[t_st])
                    P.act(sc[:], sc[:], AF.Exp, reads=[t_sc, t_st], writes=[t_sc, t_st], bias=st4[:, 0:1], accum_out=st4[:, 1:2])
                    P.op("dve", lambda e: e.tensor_scalar_max(st4[:, 1:2], st4[:, 1:2], 1e-30), reads=[t_st], writes=[t_st])
                    P.op("dve", lambda e: e.reciprocal(st4[:, 2:3], st4[:, 1:2]), reads=[t_st], writes=[t_st])
                    P.op("dve", lambda e: e.tensor_scalar(sc[:], sc[:], st4[:, 2:3], None, ALU.mult), reads=[t_sc, t_st], writes=[t_sc])
                    for c4 in range(0, ncc, 4):
                        n4 = min(4, ncc - c4)
                        p_ = ipt % 2; ipt += 1
                        for j in range(n4):
                            P.tr(ptr[p_][:, j, :], sc[:, (c4 + j) * 128:(c4 + j + 1) * 128], idf[:], reads=[t_sc, C.t_id], writes=[t_ptr[p_]])
                        P.copy("act", pcT[:, c4:c4 + n4, :], ptr[p_][:, 0:n4, :], reads=[t_ptr[p_]], writes=[t_pcT])
                    for ch in range(ncc):
                        P.mm(po[:], pcT[:, ch, :], VC[:, ch, kvh, :], start=(ch == 0), stop=(ch == ncc - 1), reads=[t_pcT, t_kc], writes=[t_po])
                    P.op("dve", lambda e, h=h: e.tensor_scalar(O[:, h, :], po[:], gts[:, h:h + 1], None, ALU.mult), reads=[t_po, t_gts], writes=[t_O])
                    for ch in range(ncc):
                        P.mm(pimp[:], pcT[:, ch, :], msel[:, ch, :], start=(g == 0 and ch == 0), stop=(g == 3 and ch == ncc - 1),
                             reads=[t_pcT, t_c], writes=[t_pimp])
                P.op("dve", lambda e: e.tensor_tensor(score[:], pimp[:], bonus[:], ALU.add), reads=[t_pimp, t_bon], writes=[t_score])
                P.op("pool", lambda e, qpos0=qpos0: e.affine_select(score[:], score[:], [[-64, nsel]], ALU.is_ge, fillreg(e, -1e30), base=qpos0, channel_multiplier=1), reads=[t_score], writes=[t_score])
                P.op("dve", lambda e: e.max(m8[:, 0:8], score[:]), reads=[t_score], writes=[t_m8])
                P.op("dve", lambda e: e.match_replace(work[:], m8[:, 0:8], score[:], -1e30), reads=[t_score, t_m8], writes=[t_score])
                P.op("dve", lambda e: e.max(m8[:, 8:16], work[:]), reads=[t_score], writes=[t_m8])
                P.op("dve", lambda e: e.tensor_scalar(mk[:], score[:], m8[:, 15:16], None, ALU.is_ge), reads=[t_score, t_m8], writes=[t_mk])
                P.op("dve", lambda e: e.tensor_scalar(work[:], score[:], -1e29, None, ALU.is_gt), reads=[t_score], writes=[t_score])
                P.op("dve", lambda e: e.tensor_tensor(mk[:], mk[:], work[:], ALU.mult), reads=[t_score, t_mk], writes=[t_mk])
                for g in range(4):
                    h = kvh * 4 + g
                    for k0 in range(0, nk, 2048):
                        kw_ = min(2048, nk - k0)
                        kb_ = ik % 2; ik += 1
                        P.dma(ktc[kb_][:, 0:kw_], X["SKT"][kvh, :, k0:k0 + kw_], reads=[t_kvr], writes=[t_ktc[kb_]])
                        for c0 in range(0, kw_, 512):
                            cw_ = min(512, kw_ - c0)
                            p_ = ips % 2; ips += 1
                            P.mm(psc[p_][:, 0:cw_], qT[:, h, :], ktc[kb_][:, c0:c0 + cw_], reads=[t_qT, t_ktc[kb_]], writes=[t_psc[p_]])
                            P.copy("act", S[:, k0 + c0:k0 + c0 + cw_], psc[p_][:, 0:cw_], reads=[t_psc[p_]], writes=[t_S])
                    Sv = S[:, 0:nk]
                    P.op("dve", lambda e, Sv=Sv: e.reduce_max(st4[:, 0:1], Sv, AX.X), reads=[t_S], writes=[t_st])
                    P.op("dve", lambda e: e.tensor_scalar_mul(st4[:, 0:1], st4[:, 0:1], -1.0), reads=[t_st], writes=[t_st])
                    P.act(Sv, Sv, AF.Exp, reads=[t_S, t_st], writes=[t_S], bias=st4[:, 0:1])
                    Sb = Sv.rearrange("p (j k) -> p j k", k=64)
                    mb = mk[:, 0:nblk].unsqueeze(2).to_broadcast([128, nblk, 64])
                    P.op("dve", lambda e, Sb=Sb, mb=mb: e.tensor_tensor(Sb, Sb, mb, ALU.mult), reads=[t_S, t_mk], writes=[t_S])
                    Sd = S[:, nk - 128:nk]
                    P.op("dve", lambda e, Sd=Sd: e.tensor_tensor(Sd, Sd, caus[:], ALU.mult), reads=[t_S, t_c], writes=[t_S])
                    P.op("dve", lambda e, Sv=Sv: e.reduce_sum(st4[:, 1:2], Sv, AX.X), reads=[t_S], writes=[t_st])
                    P.op("dve", lambda e: e.tensor_scalar_max(st4[:, 1:2], st4[:, 1:2], 1e-30), reads=[t_st], writes=[t_st])
                    P.op("dve", lambda e: e.reciprocal(st4[:, 2:3], st4[:, 1:2]), reads=[t_st], writes=[t_st])
                    P.op("dve", lambda e, h=h: e.tensor_tensor(st4[:, 3:4], st4[:, 2:3], gts[:, 8 + h:9 + h], ALU.mult), reads=[t_st, t_gts], writes=[t_st])
                    nch = nk // 128
                    for v0 in range(0, nch, 16):
                        nv = min(16, nch - v0)
                        vb_ = iv % 2; iv += 1
                        P.dma(vch[vb_][:, 0:nv, :], X["SELR"][v0 * 128:(v0 + nv) * 128, 256 + kvh * 128:384 + kvh * 128].rearrange("(c p) d -> p c d", p=128),
                              reads=[t_kvr], writes=[t_vch[vb_]])
                        for c4 in range(0, nv, 4):
                            n4 = min(4, nv - c4)
                            p_ = ipt % 2; ipt += 1
                            for j in range(n4):
                                cc = v0 + c4 + j
                                P.tr(ptr[p_][:, j, :], S[:, cc * 128:(cc + 1) * 128], idf[:], reads=[t_S, C.t_id], writes=[t_ptr[p_]])
                            P.copy("act", pT[p_][:, 0:n4, :], ptr[p_][:, 0:n4, :], reads=[t_ptr[p_]], writes=[t_pT[p_]])
                            for j in range(n4):
                                cc = v0 + c4 + j
                                P.mm(po[:], pT[p_][:, j, :], vch[vb_][:, c4 + j, :], start=(cc == 0), stop=(cc == nch - 1),
                                     reads=[t_pT[p_], t_vch[vb_]], writes=[t_po])
                    P.op("dve", lambda e, h=h: e.scalar_tensor_tensor(O[:, h, :], po[:], st4[:, 3:4], O[:, h, :], ALU.mult, ALU.add), reads=[t_po, t_st, t_O], writes=[t_O])
                w0, nw, wpos0 = T["w0"], T["nw"], T["wpos0"]
                P.dma(wkt[:, 0:nw], X["WKT"][kvh, :, w0:w0 + nw], reads=[t_kvr], writes=[t_wkt])
                nwc = nw // 128
                P.dma(wv[:, 0:nwc, :], X["WINR"][w0:w0 + nw, 256 + kvh * 128:384 + kvh * 128].rearrange("(c p) d -> p c d", p=128),
                      reads=[t_kvr], writes=[t_wv])
                for g in range(4):
                    h = kvh * 4 + g
                    for c0 in range(0, nw, 512):
                        cw_ = min(512, nw - c0)
                        p_ = ips % 2; ips += 1
                        P.mm(psc[p_][:, 0:cw_], qT[:, h, :], wkt[:, c0:c0 + cw_], reads=[t_qT, t_wkt], writes=[t_psc[p_]])
                        P.copy("act", wS[:, c0:c0 + cw_], psc[p_][:, 0:cw_], reads=[t_psc[p_]], writes=[t_wS])
                    Wv = wS[:, 0:nw]
                    d0 = qpos0 - wpos0 - w0
                    P.op("pool", lambda e, Wv=Wv, d0=d0, nw=nw: e.affine_select(Wv, Wv, [[-1, nw]], ALU.is_ge, fillreg(e, -1e30), base=d0, channel_multiplier=1), reads=[t_wS], writes=[t_wS])
                    if d0 + 127 > 511:
                        P.op("pool", lambda e, Wv=Wv, d0=d0, nw=nw: e.affine_select(Wv, Wv, [[1, nw]], ALU.is_ge, fillreg(e, -1e30), base=511 - d0, channel_multiplier=-1), reads=[t_wS], writes=[t_wS])
                    P.op("dve", lambda e, Wv=Wv: e.reduce_max(st4[:, 0:1], Wv, AX.X), reads=[t_wS], writes=[t_st])
                    P.op("dve", lambda e: e.tensor_scalar(st4[:, 0:1], st4[:, 0:1], -1e20, -1.0, ALU.max, ALU.mult), reads=[t_st], writes=[t_st])
                    P.act(Wv, Wv, AF.Exp, reads=[t_wS, t_st], writes=[t_wS, t_st], bias=st4[:, 0:1], accum_out=st4[:, 1:2])
                    P.op("dve", lambda e: e.tensor_scalar_max(st4[:, 1:2], st4[:, 1:2], 1e-30), reads=[t_st], writes=[t_st])
                    P.op("dve", lambda e: e.reciprocal(st4[:, 2:3], st4[:, 1:2]), reads=[t_st], writes=[t_st])
                    P.op("dve", lambda e, h=h: e.tensor_tensor(st4[:, 3:4], st4[:, 2:3], gts[:, 16 + h:17 + h], ALU.mult), reads=[t_st, t_gts], writes=[t_st])
                    for c4 in range(0, nwc, 4):
                        n4 = min(4, nwc - c4)
                        p_ = ipt % 2; ipt += 1
                        for j in range(n4):
                            P.tr(ptr[p_][:, j, :], wS[:, (c4 + j) * 128:(c4 + j + 1) * 128], idf[:], reads=[t_wS, C.t_id], writes=[t_ptr[p_]])
                        P.copy("act", pT[p_][:, 0:n4, :], ptr[p_][:, 0:n4, :], reads=[t_ptr[p_]], writes=[t_pT[p_]])
                        for j in range(n4):
                            P.mm(po[:], pT[p_][:, j, :], wv[:, c4 + j, :], start=(c4 + j == 0), stop=(c4 + j == nwc - 1),
                                 reads=[t_pT[p_], t_wv], writes=[t_po])
                    P.op("dve", lambda e, h=h: e.scalar_tensor_tensor(O[:, h, :], po[:], st4[:, 3:4], O[:, h, :], ALU.mult, ALU.add), reads=[t_po, t_st, t_O], writes=[t_O])
            P.dma(MIX[r0:r0 + 128, 1024:2048], O[:].rearrange("p h d -> p (h d)"), reads=[t_O], writes=[t_mix])
        P.flush()


def swiglu_ffn(P, C, x_ap, gamma_ap, wg, wu, wd, Dff, ntiles, resid, out_ap, t_in, t_out, rowscale=None, sbt=4,
               t_rs=None):
    Dm = x_ap.shape[1]
    KC = Dm // 128
    FC = Dff // 128
    NOC = Dm // 128
    rd_in = [t_in] if t_in is not None else []
    with ExitStack() as st:
        XT = P.sb(st, "fXT", [128, KC, sbt * 128], BF16); t_XT = Tk()
        AT = P.sb(st, "fAT", [128, FC, sbt * 128], BF16); t_AT = Tk()
        gt = P.sb(st, "fgt", [128, Dm]); t_g = Tk()
        P.dma(gt[:], gamma_ap.partition_broadcast(128), writes=[t_g])
        xt = P.sb(st, "fxt", [128, Dm]); t_x = Tk()
        sq = P.sb(st, "fsq", [128, Dm], BF16); t_sq = Tk()
        ss = P.sb(st, "fss", [128, 1]); t_ss = Tk()
        xn = P.sb(st, "fxn", [128, Dm], BF16); t_xn = Tk()
        ptr = [P.ps(st, "fptr", [128, 4, 128], BF16) for _ in range(2)]; t_ptr = [Tk(1), Tk(1)]
        wgs = P.sb(st, "wgs", [128, KC, 128]); wus = P.sb(st, "wus", [128, KC, 128]); t_wgs = Tk(); t_wus = Tk()
        wgb = [P.sb(st, "wgb", [128, KC, 128], BF16) for _ in range(2)]; t_wgb = [Tk(), Tk()]
        wub = [P.sb(st, "wub", [128, KC, 128], BF16) for _ in range(2)]; t_wub = [Tk(), Tk()]
        wds = P.sb(st, "wds", [128, FC, 128]); t_wds = Tk()
        wdb = [P.sb(st, "wdb", [128, FC, 128], BF16) for _ in range(2)]; t_wdb = [Tk(), Tk()]
        pg = [P.ps(st, "fpg", [128, 512]) for _ in range(2)]; t_pg = [Tk(1), Tk(1)]
        pu = [P.ps(st, "fpu", [128, 512]) for _ in range(2)]; t_pu = [Tk(1), Tk(1)]
        pd = P.ps(st, "fpd", [128, 512]); t_pd = Tk(1)
        pt2 = P.ps(st, "fpt2", [128, 4, 128]); t_pt2 = Tk(1)
        sg = [P.sb(st, "fsg", [128, 512]) for _ in range(2)]; t_sg = [Tk(), Tk()]
        oT = P.sb(st, "foT", [128, 512]); t_oT = Tk()
        rb = [P.sb(st, "frb", [128, sbt, 128]) for _ in range(2)]; t_rb = [Tk(), Tk()]
        ob = [P.sb(st, "fob", [128, sbt, 128]) for _ in range(2)]; t_ob = [Tk(), Tk()]
        rs = P.sb(st, "frs", [128, sbt]); t_rsb = Tk()
        iw = 0
        io = 0
        for s0 in range(0, ntiles, sbt):
            nts = min(sbt, ntiles - s0)
            ntok = nts * 128
            for i in range(nts):
                r0 = (s0 + i) * 128
                P.dma(xt[:], x_ap[r0:r0 + 128, :], reads=rd_in, writes=[t_x])
                P.act(sq[:], xt[:], AF.Square, reads=[t_x], writes=[t_sq, t_ss], accum_out=ss[:])
                P.op("dve", lambda e: e.tensor_scalar(ss[:], ss[:], 1.0 / Dm, EPS, ALU.mult, ALU.add), reads=[t_ss], writes=[t_ss])
                P.act(ss[:], ss[:], AF.Sqrt, reads=[t_ss], writes=[t_ss])
                P.op("dve", lambda e: e.reciprocal(ss[:], ss[:]), reads=[t_ss], writes=[t_ss])
                P.op("dve", lambda e: e.scalar_tensor_tensor(xn[:], xt[:], ss[:, 0:1], gt[:], ALU.mult, ALU.mult),
                     reads=[t_x, t_ss, t_g], writes=[t_xn])
                for kg in range(KC // 4):
                    pb = kg % 2
                    for j in range(4):
                        k = kg * 4 + j
                        P.tr(ptr[pb][:, j, :], xn[:, k * 128:(k + 1) * 128], C.idb[:], reads=[t_xn, C.t_id], writes=[t_ptr[pb]])
                    P.copy("dve" if kg % 2 == 0 else "act", XT[:, kg * 4:kg * 4 + 4, i * 128:(i + 1) * 128], ptr[pb][:],
                           reads=[t_ptr[pb]], writes=[t_XT])
            if rowscale is not None:
                P.dma(rs[:, 0:nts], rowscale[s0 * 128:s0 * 128 + ntok, :].rearrange("(t p) o -> p (t o)", p=128),
                      reads=[t_rs] if t_rs is not None else [], writes=[t_rsb], allow_slow_non_contiguous=True)
            for fc in range(FC):
                b = iw % 2; iw += 1
                P.dma(wgs[:], wg[:, fc * 128:(fc + 1) * 128].rearrange("(k p) n -> p k n", p=128), writes=[t_wgs])
                P.dma(wus[:], wu[:, fc * 128:(fc + 1) * 128].rearrange("(k p) n -> p k n", p=128), writes=[t_wus])
                P.op("pool", lambda e, b=b: e.tensor_copy(wgb[b][:], wgs[:]), reads=[t_wgs], writes=[t_wgb[b]])
                P.op("pool", lambda e, b=b: e.tensor_copy(wub[b][:], wus[:]), reads=[t_wus], writes=[t_wub[b]])
                for k in range(KC):
                    P.mm(pg[b][:, 0:ntok], wgb[b][:, k, :], XT[:, k, 0:ntok], start=(k == 0), stop=(k == KC - 1),
                         reads=[t_wgb[b], t_XT], writes=[t_pg[b]])
                for k in range(KC):
                    P.mm(pu[b][:, 0:ntok], wub[b][:, k, :], XT[:, k, 0:ntok], start=(k == 0), stop=(k == KC - 1),
                         reads=[t_wub[b], t_XT], writes=[t_pu[b]])
                P.act(sg[b][:, 0:ntok], pg[b][:, 0:ntok], AF.Silu, reads=[t_pg[b]], writes=[t_sg[b]])
                P.op("dve", lambda e, b=b, fc=fc, ntok=ntok: e.tensor_tensor(AT[:, fc, 0:ntok], sg[b][:, 0:ntok], pu[b][:, 0:ntok], ALU.mult),
                     reads=[t_sg[b], t_pu[b]], writes=[t_AT])
            for cb in range(NOC):
                b = iw % 2; iw += 1
                P.dma(wds[:], wd[:, cb * 128:(cb + 1) * 128].rearrange("(c p) n -> p c n", p=128), writes=[t_wds])
                P.op("pool", lambda e, b=b: e.tensor_copy(wdb[b][:], wds[:]), reads=[t_wds], writes=[t_wdb[b]])
                for kc in range(FC):
                    P.mm(pd[:, 0:ntok], wdb[b][:, kc, :], AT[:, kc, 0:ntok], start=(kc == 0), stop=(kc == FC - 1),
                         reads=[t_wdb[b], t_AT], writes=[t_pd])
                P.copy("act", oT[:, 0:ntok], pd[:, 0:ntok], reads=[t_pd], writes=[t_oT])
                for i in range(nts):
                    P.tr(pt2[:, i, :], oT[:, i * 128:(i + 1) * 128], C.idf[:], reads=[t_oT, C.t_id], writes=[t_pt2])
                o_ = io % 2; io += 1
                dst = out_ap[s0 * 128:s0 * 128 + ntok, cb * 128:(cb + 1) * 128].rearrange("(t p) n -> p t n", p=128)
                if resid is not None:
                    src = resid[s0 * 128:s0 * 128 + ntok, cb * 128:(cb + 1) * 128].rearrange("(t p) n -> p t n", p=128)
                    P.dma(rb[o_][:, 0:nts, :], src, reads=rd_in, writes=[t_rb[o_]])
                if rowscale is None:
                    if resid is not None:
                        P.op("dve", lambda e, o_=o_, nts=nts: e.tensor_tensor(ob[o_][:, 0:nts, :], pt2[:, 0:nts, :], rb[o_][:, 0:nts, :], ALU.add),
                             reads=[t_pt2, t_rb[o_]], writes=[t_ob[o_]])
                    else:
                        P.copy("dve", ob[o_][:, 0:nts, :], pt2[:, 0:nts, :], reads=[t_pt2], writes=[t_ob[o_]])
                else:
                    for i in range(nts):
                        if resid is not None:
                            P.op("dve", lambda e, o_=o_, i=i: e.scalar_tensor_tensor(ob[o_][:, i, :], pt2[:, i, :], rs[:, i:i + 1], rb[o_][:, i, :], ALU.mult, ALU.add),
                                 reads=[t_pt2, t_rb[o_], t_rsb], writes=[t_ob[o_]])
                        else:
                            P.op("dve", lambda e, o_=o_, i=i: e.tensor_scalar(ob[o_][:, i, :], pt2[:, i, :], rs[:, i:i + 1], None, ALU.mult),
                                 reads=[t_pt2, t_rsb], writes=[t_ob[o_]])
                P.dma(dst, ob[o_][:, 0:nts, :], reads=[t_ob[o_]], writes=[t_out])
        P.flush()


def host_ssd_consts():
    i = np.arange(128)
    tri = (i[:, None] <= i[None, :]).astype(np.float32)
    e127 = np.repeat((i == 127)[:, None], 128, 1).astype(np.float32)
    maddT = np.where(i[None, :] >= i[:, None], 0.0, -1e4).astype(np.float32)
    ones = np.ones((128, 128), np.float32)
    return np.concatenate([tri, e127, maddT, ones], axis=1)


def ssd_stage(P, C, XBCT, ZDT, Y, I, outs, TP, t_in, t_y):
    NTP = TP // 128
    with ExitStack() as st:
        K_ = P.sb(st, "sconst", [128, 4, 128]); t_K = Tk()
        P.dma(K_[:], I["SCONST"].rearrange("p (k n) -> p k n", k=4), writes=[t_K])
        TRI, E127, MADDT, ONES = (K_[:, k, :] for k in range(4))
        cw = P.sb(st, "scw", [128, 48, 5])
        P.dma(cw[:], I["ssm_cw"], writes=[t_K])
        alog = P.sb(st, "salog", [128, 64]); dtb = P.sb(st, "sdtb", [128, 64]); dsk = P.sb(st, "sdsk", [128, 64])
        nw = P.sb(st, "snw", [128, 4096])
        P.dma(alog[:], I["ssm_a_log"].partition_broadcast(128), writes=[t_K])
        P.dma(dtb[:], I["ssm_dt_bias"].partition_broadcast(128), writes=[t_K])
        P.dma(dsk[:], I["ssm_d_skip"].partition_broadcast(128), writes=[t_K])
        P.dma(nw[:], I["ssm_norm_w"].partition_broadcast(128), writes=[t_K])
        P.act(alog[:], alog[:], AF.Exp, reads=[t_K], writes=[t_K])
        P.op("dve", lambda e: e.tensor_scalar_mul(alog[:], alog[:], -1.0), reads=[t_K], writes=[t_K])
        hT = P.sb(st, "hT", [128, 64, 64]); t_h = [Tk() for _ in range(64)]
        P.op("pool", lambda e: e.memset(hT[:], 0.0), writes=t_h)
        hio = P.sb(st, "hio", [64, 64, 128]); t_hio = Tk()
        xq = [P.sb(st, "sxq", [128, 131]) for _ in range(3)]; t_xq = [Tk(), Tk(), Tk()]
        acc = [P.sb(st, "sacc", [128, 128]) for _ in range(2)]; t_acc = [Tk(), Tk()]
        xtm = P.sb(st, "xtm", [128, 64, 64]); t_xtm = Tk()
        xdt = P.sb(st, "xdt", [128, 64, 64]); xdec = P.sb(st, "xdec", [128, 64, 64]); t_xd = Tk()
        Btm = P.sb(st, "Btm", [128, 8, 128]); t_B = Tk()
        CT = P.sb(st, "CTs", [128, 8, 128]); BTf = P.sb(st, "BTf", [128, 8, 128]); t_CT = Tk()
        cbT = P.sb(st, "cbT", [128, 8, 128]); t_cb = Tk()
        zd = P.sb(st, "zd", [128, 4160]); t_zd = Tk()
        vm = P.sb(st, "svm", [128, 1]); t_vm = Tk()
        dt = P.sb(st, "sdt", [128, 64]); t_dt = Tk()
        gam = P.sb(st, "sgam", [128, 64]); t_gam = Tk()
        sc = P.sb(st, "ssc", [128, 3, 64]); t_sc = Tk()
        yb = P.sb(st, "yb", [128, 64, 64]); t_yb = Tk()
        diag = P.sb(st, "sdiag", [128, 128]); t_diag = Tk()
        LT = P.sb(st, "sLT", [128, 128]); t_LT = Tk()
        yin = P.sb(st, "syin", [128, 64]); t_yin = Tk()
        gs = P.sb(st, "sgs", [128, 16]); t_gs = Tk()
        junk = P.sb(st, "sjunk", [128, 512]); t_junk = Tk()
        pt = [P.ps(st, "spt", [128, 4, 128]) for _ in range(2)]; t_pt = [Tk(1), Tk(1)]
        pg = P.ps(st, "spg", [128, 2, 64]); t_pg = Tk(1)
        pL = P.ps(st, "spL", [128, 128]); t_pL = Tk(1)
        py = P.ps(st, "spy", [128, 2, 64]); t_py = Tk(1)
        ph = P.ps(st, "sph", [128, 64]); t_ph = Tk(1)
        pc = P.ps(st, "spc", [128, 128]); t_pc = Tk(1)
        idf = C.idf
        ix = 0
        for i in range(NTP + 1):
            r0 = i * 128
            sample = (i == NTP)
            if sample:
                for g4 in range(16):
                    for j in range(4):
                        h = g4 * 4 + j
                        P.tr(pt[g4 % 2][0:64, j, :], hT[:, h, :], idf[:], reads=[t_h[h], C.t_id], writes=[t_pt[g4 % 2]])
                    P.copy("act", hio[:, g4 * 4:g4 * 4 + 4, :], pt[g4 % 2][0:64, :, :], reads=[t_pt[g4 % 2]], writes=[t_hio])
                P.dma(outs["sst_p"].rearrange("h p n -> p h n"), hio[:], reads=[t_hio], writes=[outs["t"]])
                P.dma(hio[:], I["ssm_state"].rearrange("h p n -> p h n"), writes=[t_hio])
                for g4 in range(16):
                    for j in range(4):
                        h = g4 * 4 + j
                        P.tr(pt[g4 % 2][:, j, 0:64], hio[:, h, :], idf[0:64, 0:64], reads=[t_hio, C.t_id], writes=[t_pt[g4 % 2]])
                    P.copy("act", hT[:, g4 * 4:g4 * 4 + 4, :], pt[g4 % 2][:, :, 0:64], reads=[t_pt[g4 % 2]], writes=t_h[g4 * 4:g4 * 4 + 4])
            P.dma(zd[:], ZDT[r0:r0 + 128, :], reads=[t_in], writes=[t_zd])
            P.dma(vm[:], I["VMASK"][r0:r0 + 128, :], writes=[t_vm])
            P.op("dve", lambda e: e.tensor_add(dt[:], zd[:, 4096:4160], dtb[:]), reads=[t_zd, t_K], writes=[t_dt])
            softplus(P, st, dt[:], t_dt, 64, "ssp%d" % i)
            P.op("dve", lambda e: e.tensor_scalar(dt[:], dt[:], vm[:, 0:1], None, ALU.mult), reads=[t_dt, t_vm], writes=[t_dt])
            P.op("dve", lambda e: e.tensor_mul(gam[:], dt[:], alog[:]), reads=[t_dt, t_K], writes=[t_gam])
            P.mm(pg[:, 0, :], TRI, gam[:], reads=[t_K, t_gam], writes=[t_pg])
            P.copy("dve", gam[:], pg[:, 0, :], reads=[t_pg], writes=[t_gam])
            P.mm(pg[:, 1, :], E127, gam[:], reads=[t_K, t_gam], writes=[t_pg])
            P.act(sc[:, 0, :], gam[:], AF.Exp, reads=[t_gam], writes=[t_sc])
            P.op("dve", lambda e: e.tensor_sub(sc[:, 1, :], pg[:, 1, :], gam[:]), reads=[t_pg, t_gam], writes=[t_sc])
            P.act(sc[:, 1, :], sc[:, 1, :], AF.Exp, reads=[t_sc], writes=[t_sc])
            P.act(sc[:, 2, :], pg[:, 1, :], AF.Exp, reads=[t_pg], writes=[t_sc])
            P.act(zd[:, 0:4096], zd[:, 0:4096], AF.Silu, reads=[t_zd], writes=[t_zd])
            for ch in range(48):
                xb = ix % 3; ab = ix % 2; ix += 1
                src = XBCT[ch * 128:(ch + 1) * 128]
                if i == 0:
                    P.op("pool", lambda e, xb=xb: e.memset(xq[xb][:, 0:3], 0.0), writes=[t_xq[xb]])
                    P.dma(xq[xb][:, 3:131], src[:, 0:128], reads=[t_in], writes=[t_xq[xb]])
                elif sample:
                    P.dma(xq[xb][:, 0:3], I["ssm_convT"][ch * 128:(ch + 1) * 128, :], writes=[t_xq[xb]])
                    P.dma(xq[xb][:, 3:131], src[:, r0:r0 + 128], reads=[t_in], writes=[t_xq[xb]])
                else:
                    P.dma(xq[xb][:], src[:, r0 - 3:r0 + 128], reads=[t_in], writes=[t_xq[xb]])
                a_ = acc[ab]
                P.op("dve", lambda e, xb=xb, a_=a_, ch=ch: e.tensor_scalar(a_[:], xq[xb][:, 0:128], cw[:, ch, 0:1], cw[:, ch, 4:5], ALU.mult, ALU.add),
                     reads=[t_xq[xb], t_K], writes=[t_acc[ab]])
                for j in range(1, 4):
                    P.op("dve", lambda e, xb=xb, a_=a_, ch=ch, j=j: e.scalar_tensor_tensor(a_[:], xq[xb][:, j:j + 128], cw[:, ch, j:j + 1], a_[:], ALU.mult, ALU.add),
                         reads=[t_xq[xb], t_K, t_acc[ab]], writes=[t_acc[ab]])
                if ch < 32:
                    P.act(a_[:], a_[:], AF.Silu, reads=[t_acc[ab]], writes=[t_acc[ab]])
                    p_ = (ch // 4) % 2
                    P.tr(pt[p_][:, ch % 4, :], a_[:], idf[:], reads=[t_acc[ab], C.t_id], writes=[t_pt[p_]])
                    if ch % 4 == 3:
                        P.copy("act", xtm[:, (ch - 3) * 2:(ch + 1) * 2, :].rearrange("p h d -> p (h d)"),
                               pt[p_][:].rearrange("p a b -> p (a b)"), reads=[t_pt[p_]], writes=[t_xtm])
                elif ch < 40:
                    P.act(BTf[:, ch - 32, :], a_[:], AF.Silu, reads=[t_acc[ab]], writes=[t_CT])
                else:
                    P.act(CT[:, ch - 40, :], a_[:], AF.Silu, reads=[t_acc[ab]], writes=[t_CT])
            for g4 in range(2):
                for j in range(4):
                    P.tr(pt[g4][:, j, :], BTf[:, g4 * 4 + j, :], idf[:], reads=[t_CT, C.t_id], writes=[t_pt[g4]])
                P.copy("act", Btm[:, g4 * 4:g4 * 4 + 4, :], pt[g4][:], reads=[t_pt[g4]], writes=[t_B])
            for g_ in range(8):
                P.mm(pc[:], BTf[:, g_, :], CT[:, g_, :], reads=[t_CT], writes=[t_pc])
                P.copy("act", cbT[:, g_, :], pc[:], reads=[t_pc], writes=[t_cb])
            dtb_ = dt[:].unsqueeze(2).to_broadcast([128, 64, 64])
            decb = sc[:, 1, :].unsqueeze(2).to_broadcast([128, 64, 64])
            P.op("dve", lambda e: e.tensor_tensor(xdt[:], xtm[:], dtb_, ALU.mult), reads=[t_xtm, t_dt], writes=[t_xd])
            P.op("pool", lambda e: e.tensor_tensor(xdec[:], xdt[:], decb, ALU.mult), reads=[t_xd, t_sc], writes=[t_xd])
            for h in range(64):
                g_ = h // 8
                P.op("dve", lambda e, h=h: e.tensor_scalar(diag[:], idf[:], gam[:, h:h + 1], None, ALU.mult), reads=[C.t_id, t_gam], writes=[t_diag])
                P.op("dve", lambda e, h=h: e.tensor_scalar_mul(gs[:, 0:1], gam[:, h:h + 1], -1.0), reads=[t_gam], writes=[t_gs])
                P.mm(pL[:], ONES, diag[:], start=True, stop=False, reads=[t_K, t_diag], writes=[t_pL])
                P.mm(pL[:], idf[:], MADDT, start=False, stop=True, reads=[t_K, C.t_id], writes=[t_pL])
                P.act(LT[:], pL[:], AF.Exp, reads=[t_pL, t_gs], writes=[t_LT], bias=gs[:, 0:1])
                P.op("dve", lambda e, g_=g_: e.tensor_tensor(LT[:], LT[:], cbT[:, g_, :], ALU.mult), reads=[t_LT, t_cb], writes=[t_LT])
                P.mm(py[:, 0, :], LT[:], xdt[:, h, :], reads=[t_LT, t_xd], writes=[t_py])
                P.mm(py[:, 1, :], CT[:, g_, :], hT[:, h, :], reads=[t_CT, t_h[h]], writes=[t_py])
                P.copy("act", yin[:], py[:, 0, :], reads=[t_py], writes=[t_yin])
                P.op("dve", lambda e, h=h: e.scalar_tensor_tensor(yb[:, h, :], py[:, 1, :], sc[:, 0, h:h + 1], yin[:], ALU.mult, ALU.add),
                     reads=[t_py, t_sc, t_yin], writes=[t_yb])
                P.mm(ph[:], Btm[:, g_, :], xdec[:, h, :], reads=[t_B, t_xd], writes=[t_ph])
                P.op("dve", lambda e, h=h: e.scalar_tensor_tensor(hT[:, h, :], hT[:, h, :], sc[:, 2, h:h + 1], ph[:], ALU.mult, ALU.add),
                     reads=[t_h[h], t_sc, t_ph], writes=[t_h[h]])
            dskb = dsk[:].unsqueeze(2).to_broadcast([128, 64, 64])
            P.op("pool", lambda e: e.tensor_tensor(xdt[:], xtm[:], dskb, ALU.mult), reads=[t_xtm, t_K, t_xd], writes=[t_xd])
            P.op("dve", lambda e: e.tensor_add(yb[:], yb[:], xdt[:]), reads=[t_yb, t_xd], writes=[t_yb])
            ybf = yb[:].rearrange("p h d -> p (h d)")
            P.op("dve", lambda e: e.tensor_mul(ybf, ybf, zd[:, 0:4096]), reads=[t_yb, t_zd], writes=[t_yb])
            for g_ in range(8):
                P.act(junk[:], ybf[:, g_ * 512:(g_ + 1) * 512], AF.Square, reads=[t_yb], writes=[t_junk, t_gs], accum_out=gs[:, 8 + g_:9 + g_])
            P.op("dve", lambda e: e.tensor_scalar(gs[:, 8:16], gs[:, 8:16], 1.0 / 512, EPS, ALU.mult, ALU.add), reads=[t_gs], writes=[t_gs])
            P.act(gs[:, 8:16], gs[:, 8:16], AF.Sqrt, reads=[t_gs], writes=[t_gs])
            P.op("dve", lambda e: e.reciprocal(gs[:, 8:16], gs[:, 8:16]), reads=[t_gs], writes=[t_gs])
            rb_ = gs[:, 8:16].unsqueeze(2).to_broadcast([128, 8, 512])
            ybg = yb[:].rearrange("p (g h) d -> p g (h d)", g=8)
            P.op("dve", lambda e: e.tensor_tensor(ybg, ybg, rb_, ALU.mult), reads=[t_yb, t_gs], writes=[t_yb])
            P.op("dve", lambda e: e.tensor_mul(ybf, ybf, nw[:]), reads=[t_yb, t_K], writes=[t_yb])
            P.dma(Y[r0:r0 + 128, :], ybf, reads=[t_yb], writes=[t_y])
        for g4 in range(16):
            for j in range(4):
                h = g4 * 4 + j
                P.tr(pt[g4 % 2][0:64, j, :], hT[:, h, :], idf[:], reads=[t_h[h], C.t_id], writes=[t_pt[g4 % 2]])
            P.copy("act", hio[:, g4 * 4:g4 * 4 + 4, :], pt[g4 % 2][0:64, :, :], reads=[t_pt[g4 % 2]], writes=[t_hio])
        P.dma(outs["sst_s"].rearrange("h p n -> p h n"), hio[:], reads=[t_hio], writes=[outs["t"]])
        P.flush()


def host_msel(n_cmp, ncp, nsel):
    cs = np.arange(ncp)[:, None] * 16
    ss = np.arange(nsel)[None, :] * 64
    m = ((cs < ss + 64) & (cs + 32 > ss)).astype(np.float32)
    m[n_cmp:] = 0
    return m


def sample_ctx(P, C, I, KVR, KT, TP, t_kvr, X, t_sx):
    NPG = PAST // 128
    with ExitStack() as st:
        pti = P.sb(st, "pti", [128, NPG], I32); t_pt = Tk()
        P.dma(pti[:], I["pt_row"].partition_broadcast(128), writes=[t_pt])
        ptf = P.sb(st, "ptf", [128, NPG]); io = P.sb(st, "iota", [128, 1])
        P.dma(io[:], I["IOTA"], writes=[t_pt])
        P.op("dve", lambda e: e.tensor_copy(ptf[:], pti[:]), reads=[t_pt], writes=[t_pt])
        P.op("dve", lambda e: e.tensor_scalar(ptf[:], ptf[:], 128.0, io[:, 0:1], ALU.mult, ALU.add), reads=[t_pt], writes=[t_pt])
        idx = P.sb(st, "pidx", [128, NPG], I32)
        P.op("dve", lambda e: e.tensor_copy(idx[:], ptf[:]), reads=[t_pt], writes=[t_pt])
        pg_ = [P.sb(st, "pgt", [128, 512]) for _ in range(3)]; t_pg = [Tk(), Tk(), Tk()]
        ptr = [P.ps(st, "sptr", [128, 4, 128]) for _ in range(2)]; t_ptr = [Tk(1), Tk(1)]
        tb = [P.sb(st, "stb", [128, 4, 128]) for _ in range(2)]; t_tb = [Tk(), Tk()]
        ip = 0
        for which, pool in enumerate((I["pool_cmp"], I["pool_sel"])):
            for p in range(NPG):
                b = ip % 3; pb = ip % 2; ip += 1
                P.idma(pg_[b][:], pool, idx[:, p:p + 1], reads=[t_pt], writes=[t_pg[b]])
                if which == 0:
                    for j in range(4):
                        P.tr(ptr[pb][:, j, :], pg_[b][:, j * 128:(j + 1) * 128], C.idf[:], reads=[t_pg[b], C.t_id], writes=[t_ptr[pb]])
                    P.copy("act", tb[pb][:], ptr[pb][:], reads=[t_ptr[pb]], writes=[t_tb[pb]])
                    P.dma(X["CT"][:, :, p * 128:(p + 1) * 128].rearrange("a d t -> d a t"), tb[pb][:], reads=[t_tb[pb]], writes=[t_sx])
                else:
                    P.dma(X["SELR"][p * 128:(p + 1) * 128, :], pg_[b][:], reads=[t_pg[b]], writes=[t_sx])
                    for j in range(2):
                        P.tr(ptr[pb][:, j, :], pg_[b][:, j * 128:(j + 1) * 128], C.idf[:], reads=[t_pg[b], C.t_id], writes=[t_ptr[pb]])
                    P.copy("act", tb[pb][:, 0:2, :], ptr[pb][:, 0:2, :], reads=[t_ptr[pb]], writes=[t_tb[pb]])
                    P.dma(X["SKT"][:, :, p * 128:(p + 1) * 128].rearrange("a d t -> d a t"), tb[pb][:, 0:2, :], reads=[t_tb[pb]], writes=[t_sx])
        P.dma(X["SELR"][PAST:PAST + 128, :], KVR[TP:TP + 128, 512:1024], reads=[t_kvr], writes=[t_sx])
        P.dma(X["SKT"][:, :, PAST:PAST + 128], KT[4:6, :, TP:TP + 128], reads=[t_kvr], writes=[t_sx])
        P.dma(X["WINR"][0:512, :], I["win_cache"], writes=[t_sx])
        P.dma(X["WINR"][512:640, :], KVR[TP:TP + 128, 1024:1536], reads=[t_kvr], writes=[t_sx])
        P.dma(X["WKT"][:, :, 512:640], KT[6:8, :, TP:TP + 128], reads=[t_kvr], writes=[t_sx])
        for p in range(4):
            b = ip % 3; pb = ip % 2; ip += 1
            P.dma(pg_[b][:], I["win_cache"][p * 128:(p + 1) * 128, :], writes=[t_pg[b]])
            for j in range(2):
                P.tr(ptr[pb][:, j, :], pg_[b][:, j * 128:(j + 1) * 128], C.idf[:], reads=[t_pg[b], C.t_id], writes=[t_ptr[pb]])
            P.copy("act", tb[pb][:, 0:2, :], ptr[pb][:, 0:2, :], reads=[t_ptr[pb]], writes=[t_tb[pb]])
            P.dma(X["WKT"][:, :, p * 128:(p + 1) * 128].rearrange("a d t -> d a t"), tb[pb][:, 0:2, :], reads=[t_tb[pb]], writes=[t_sx])
        P.flush()


def allreduce(P, src, dst, nrows, t_src, t_dst, n_cores, chunk=1024):
    for r0 in range(0, nrows, chunk):
        r1 = min(nrows, r0 + chunk)
        P.op("pool", lambda e, r0=r0, r1=r1: e.collective_compute(
            "AllReduce", ALU.add, replica_groups=[list(range(n_cores))],
            ins=[src[r0:r1, :]], outs=[dst[r0:r1, :]]), reads=[t_src], writes=[t_dst])


def moe_stage(P, C, H3, I, outs, TP, NT, t_h3, n_cores, dbg_kind):
    nc = P.nc
    NG = 2 * TP + 128
    NGT = NG // 128
    NTP = TP // 128
    GS = nc.dram_tensor("GATHsrc", [NG, D], F32).ap()
    G = nc.dram_tensor("GATH", [NG, D], F32).ap()
    ES = nc.dram_tensor("EOUTsrc", [NG, D], F32).ap()
    EO = nc.dram_tensor("EOUT", [NG, D], F32, kind=dbg_kind).ap()
    GATE = nc.dram_tensor("GATE", [NG, 1], F32, kind=dbg_kind).ap()
    t_gs, t_g, t_es, t_eo, t_gate = Tk(), Tk(), Tk(), Tk(), Tk()
    with ExitStack() as st:
        fl = P.sb(st, "fl", [128, 12]); t_fl = Tk()
        P.dma(fl[:], I["FLAGS"], writes=[t_fl])
        ht = [P.sb(st, "mht", [128, D]) for _ in range(2)]; t_ht = [Tk(), Tk()]
        o0 = [P.sb(st, "mo0", [128, D]) for _ in range(2)]; t_o0 = [Tk(), Tk()]
        o1 = [P.sb(st, "mo1", [128, D]) for _ in range(2)]; t_o1 = [Tk(), Tk()]
        for i in range(NTP):
            b = i % 2
            P.dma(ht[b][:], H3[i * 128:(i + 1) * 128, :], reads=[t_h3], writes=[t_ht[b]])
            P.op("dve", lambda e, b=b: e.tensor_scalar(o0[b][:], ht[b][:], fl[:, 0:1], None, ALU.mult), reads=[t_ht[b], t_fl], writes=[t_o0[b]])
            P.op("pool", lambda e, b=b: e.tensor_scalar(o1[b][:], ht[b][:], fl[:, 1:2], None, ALU.mult), reads=[t_ht[b], t_fl], writes=[t_o1[b]])
            P.dma(GS[i * 128:(i + 1) * 128, :], o0[b][:], reads=[t_o0[b]], writes=[t_gs])
            P.dma(GS[TP + i * 128:TP + (i + 1) * 128, :], o1[b][:], reads=[t_o1[b]], writes=[t_gs])
        P.op("pool", lambda e: e.memset(o1[0][:], 0.0), reads=[t_o1[0]], writes=[t_o1[0]])
        P.dma(GS[2 * TP:2 * TP + 128, :], o1[0][:], reads=[t_o1[0]], writes=[t_gs])
        P.dma(ht[0][0:4, :], H3[TP:TP + 4, :], reads=[t_h3], writes=[t_ht[0]])
        for s_ in range(8):
            b = s_ % 2
            P.op("dve", lambda e, b=b, s_=s_: e.tensor_scalar(o0[b][0:4, :], ht[0][0:4, :], fl[0:4, 2 + s_:3 + s_], None, ALU.mult),
                 reads=[t_ht[0], t_fl], writes=[t_o0[b]])
            P.dma(GS[2 * TP + 4 * s_:2 * TP + 4 * s_ + 4, :], o0[b][0:4, :], reads=[t_o0[b]], writes=[t_gs])
        allreduce(P, GS, G, NG, t_gs, t_g, n_cores)
        P.flush()
    with ExitStack() as st:
        fl = P.sb(st, "fl2", [128, 12]); t_fl = Tk()
        P.dma(fl[:], I["FLAGS"], writes=[t_fl])
        gt = P.sb(st, "rgt", [128, D]); t_gt = Tk()
        P.dma(gt[:], I["ssm_norm_ffn"].partition_broadcast(128), writes=[t_gt])
        rw = P.sb(st, "rw", [128, 16, 8]); t_rw = Tk()
        P.dma(rw[:], I["moe_router"].rearrange("(k p) e -> p k e", p=128), writes=[t_rw])
        xt = [P.sb(st, "rxt", [128, D]) for _ in range(2)]; t_x = [Tk(), Tk()]
        sq = P.sb(st, "rsq", [128, D]); t_sq = Tk()
        ss = P.sb(st, "rss", [128, 1]); t_ss = Tk()
        xT = P.sb(st, "rxT", [128, 16, 128]); t_xT = Tk()
        ptr = [P.ps(st, "rptr", [128, 4, 128]) for _ in range(2)]; t_ptr = [Tk(1), Tk(1)]
        pl = P.ps(st, "rpl", [128, 8]); t_pl = Tk(1)
        lg = P.sb(st, "rlg", [128, 8]); t_lg = Tk()
        m8 = P.sb(st, "rm8", [128, 8]); t_m8 = Tk()
        w12 = P.sb(st, "rw12", [128, 2]); t_w = Tk()
        gte = P.sb(st, "rgte", [128, 8]); tmp = P.sb(st, "rtmp", [128, 8]); t_gte = Tk()
        gm = [P.sb(st, "rgm", [128, 1]) for _ in range(2)]; t_gm = [Tk(), Tk()]
        for i in range(NGT):
            b = i % 2
            P.dma(xt[b][:], G[i * 128:(i + 1) * 128, :], reads=[t_g], writes=[t_x[b]])
            P.act(sq[:], xt[b][:], AF.Square, reads=[t_x[b]], writes=[t_sq, t_ss], accum_out=ss[:])
            P.op("dve", lambda e: e.tensor_scalar(ss[:], ss[:], 1.0 / D, EPS, ALU.mult, ALU.add), reads=[t_ss], writes=[t_ss])
            P.act(ss[:], ss[:], AF.Sqrt, reads=[t_ss], writes=[t_ss])
            P.op("dve", lambda e: e.reciprocal(ss[:], ss[:]), reads=[t_ss], writes=[t_ss])
            P.op("dve", lambda e, b=b: e.scalar_tensor_tensor(sq[:], xt[b][:], ss[:, 0:1], gt[:], ALU.mult, ALU.mult),
                 reads=[t_x[b], t_ss, t_gt], writes=[t_sq])
            for kg in range(4):
                pb = kg % 2
                for j in range(4):
                    k = kg * 4 + j
                    P.tr(ptr[pb][:, j, :], sq[:, k * 128:(k + 1) * 128], C.idf[:], reads=[t_sq, C.t_id], writes=[t_ptr[pb]])
                P.copy("act", xT[:, kg * 4:kg * 4 + 4, :], ptr[pb][:], reads=[t_ptr[pb]], writes=[t_xT])
            for k in range(16):
                P.mm(pl[:], xT[:, k, :], rw[:, k, :], start=(k == 0), stop=(k == 15), reads=[t_xT, t_rw], writes=[t_pl])
            P.copy("dve", lg[:], pl[:], reads=[t_pl], writes=[t_lg])
            P.op("dve", lambda e: e.max(m8[:], lg[:]), reads=[t_lg], writes=[t_m8])
            P.op("dve", lambda e: e.tensor_sub(w12[:, 0:1], m8[:, 1:2], m8[:, 0:1]), reads=[t_m8], writes=[t_w])
            P.act(w12[:, 0:1], w12[:, 0:1], AF.Exp, reads=[t_w], writes=[t_w])
            P.op("dve", lambda e: e.tensor_scalar_add(w12[:, 0:1], w12[:, 0:1], 1.0), reads=[t_w], writes=[t_w])
            P.op("dve", lambda e: e.reciprocal(w12[:, 0:1], w12[:, 0:1]), reads=[t_w], writes=[t_w])
            P.op("dve", lambda e: e.tensor_scalar(w12[:, 1:2], w12[:, 0:1], -1.0, 1.0, ALU.mult, ALU.add), reads=[t_w], writes=[t_w])
            P.op("dve", lambda e: e.tensor_scalar(gte[:], lg[:], m8[:, 0:1], w12[:, 0:1], ALU.is_equal, ALU.mult), reads=[t_lg, t_m8, t_w], writes=[t_gte])
            P.op("dve", lambda e: e.tensor_scalar(tmp[:], lg[:], m8[:, 1:2], w12[:, 1:2], ALU.is_equal, ALU.mult), reads=[t_lg, t_m8, t_w], writes=[t_gte])
            P.op("dve", lambda e: e.tensor_add(gte[:], gte[:], tmp[:]), reads=[t_gte], writes=[t_gte])
            P.op("dve", lambda e: e.tensor_mul(gte[:], gte[:], fl[:, 2:10]), reads=[t_gte, t_fl], writes=[t_gte])
            P.op("dve", lambda e, b=b: e.reduce_sum(gm[b][:], gte[:], AX.X), reads=[t_gte], writes=[t_gm[b]])
            P.dma(GATE[i * 128:(i + 1) * 128, :], gm[b][:], reads=[t_gm[b]], writes=[t_gate])
        P.flush()
    swiglu_ffn(P, C, G, I["ssm_norm_ffn"], I["moe_wg"], I["moe_wu"], I["moe_wd"], 7168, NGT, None, ES, t_g, t_es,
               rowscale=GATE, t_rs=t_gate)
    allreduce(P, ES, EO, NG, t_es, t_eo, n_cores)
    P.flush()
    with ExitStack() as st:
        fl = P.sb(st, "fl3", [128, 12]); t_fl = Tk()
        P.dma(fl[:], I["FLAGS"], writes=[t_fl])
        gt = P.sb(st, "ngt", [128, D]); t_gt = Tk()
        P.dma(gt[:], I["final_norm"].partition_broadcast(128), writes=[t_gt])
        ht = [P.sb(st, "nht", [128, D]) for _ in range(2)]; t_ht = [Tk(), Tk()]
        e0 = [P.sb(st, "ne0", [128, D]) for _ in range(2)]; t_e0 = [Tk(), Tk()]
        e1 = [P.sb(st, "ne1", [128, D]) for _ in range(2)]; t_e1 = [Tk(), Tk()]
        sq = P.sb(st, "nsq", [128, D]); t_sq = Tk()
        ss = P.sb(st, "nss", [128, 1]); t_ss = Tk()
        yo = [P.sb(st, "nyo", [128, D]) for _ in range(2)]; t_yo = [Tk(), Tk()]
        es = P.sb(st, "nes", [128, 8, D // 4]); t_esb = Tk()
        for i in range(NTP + 1):
            b = i % 2
            P.dma(ht[b][:], H3[i * 128:(i + 1) * 128, :], reads=[t_h3], writes=[t_ht[b]])
            if i < NTP:
                P.dma(e0[b][:], EO[i * 128:(i + 1) * 128, :], reads=[t_eo], writes=[t_e0[b]])
                P.dma(e1[b][:], EO[TP + i * 128:TP + (i + 1) * 128, :], reads=[t_eo], writes=[t_e1[b]])
                P.op("dve", lambda e, b=b: e.scalar_tensor_tensor(ht[b][:], e0[b][:], fl[:, 10:11], ht[b][:], ALU.mult, ALU.add),
                     reads=[t_e0[b], t_fl, t_ht[b]], writes=[t_ht[b]])
                P.op("dve", lambda e, b=b: e.scalar_tensor_tensor(ht[b][:], e1[b][:], fl[:, 11:12], ht[b][:], ALU.mult, ALU.add),
                     reads=[t_e1[b], t_fl, t_ht[b]], writes=[t_ht[b]])
            else:
                for s_ in range(8):
                    P.dma(e0[b][0:4, :], EO[2 * TP + 4 * s_:2 * TP + 4 * s_ + 4, :], reads=[t_eo], writes=[t_e0[b]])
                    P.op("dve", lambda e, b=b, s_=s_: e.scalar_tensor_tensor(ht[b][0:4, :], e0[b][0:4, :], fl[0:4, 2 + s_:3 + s_], ht[b][0:4, :], ALU.mult, ALU.add),
                         reads=[t_e0[b], t_fl, t_ht[b]], writes=[t_ht[b]])
            P.act(sq[:], ht[b][:], AF.Square, reads=[t_ht[b]], writes=[t_sq, t_ss], accum_out=ss[:])
            P.op("dve", lambda e: e.tensor_scalar(ss[:], ss[:], 1.0 / D, EPS, ALU.mult, ALU.add), reads=[t_ss], writes=[t_ss])
            P.act(ss[:], ss[:], AF.Sqrt, reads=[t_ss], writes=[t_ss])
            P.op("dve", lambda e: e.reciprocal(ss[:], ss[:]), reads=[t_ss], writes=[t_ss])
            P.op("dve", lambda e, b=b: e.scalar_tensor_tensor(yo[b][:], ht[b][:], ss[:, 0:1], gt[:], ALU.mult, ALU.mult),
                 reads=[t_ht[b], t_ss, t_gt], writes=[t_yo[b]])
            if i < NTP:
                P.dma(outs["y_p"][i * 128:(i + 1) * 128, :], yo[b][:], reads=[t_yo[b]], writes=[outs["t"]])
            else:
                P.dma(outs["y_s"][0:4, :], yo[b][0:4, :], reads=[t_yo[b]], writes=[outs["t"]])
        P.flush()


def moe_local_stage(P, C, H3, I, outs, TP, t_h3, dbg_kind):
    nc = P.nc
    NTP = TP // 128
    nsplit = 4 if NTP % 4 == 0 else (2 if NTP % 2 == 0 else 1)
    NQ = NTP // nsplit
    NR = (NQ + 1) * 128
    HQ = nc.dram_tensor("HQ", [NR, D], F32, kind=dbg_kind).ap()
    ACC = [nc.dram_tensor("ACC%d" % i, [NR, D], F32).ap() for i in range(2)]
    GATE8 = nc.dram_tensor("GATE8", [NR, 8], F32, kind=dbg_kind).ap()
    t_hq, t_gate = Tk(), Tk()
    with ExitStack() as st:
        qi = P.sb(st, "qidx", [128, NQ], I32); t_qi = Tk()
        P.dma(qi[:], I["QIDX"], writes=[t_qi])
        gt = P.sb(st, "rgt", [128, D]); t_gt = Tk()
        P.dma(gt[:], I["ssm_norm_ffn"].partition_broadcast(128), writes=[t_gt])
        rw = P.sb(st, "rw", [128, 16, 8]); t_rw = Tk()
        P.dma(rw[:], I["moe_router"].rearrange("(k p) e -> p k e", p=128), writes=[t_rw])
        xt = [P.sb(st, "rxt", [128, D]) for _ in range(2)]; t_x = [Tk(), Tk()]
        sq = P.sb(st, "rsq", [128, D]); t_sq = Tk()
        ss = P.sb(st, "rss", [128, 1]); t_ss = Tk()
        xT = P.sb(st, "rxT", [128, 16, 128]); t_xT = Tk()
        ptr = [P.ps(st, "rptr", [128, 4, 128]) for _ in range(2)]; t_ptr = [Tk(1), Tk(1)]
        pl = P.ps(st, "rpl", [128, 8]); t_pl = Tk(1)
        lg = P.sb(st, "rlg", [128, 8]); t_lg = Tk()
        m8 = P.sb(st, "rm8", [128, 8]); t_m8 = Tk()
        w12 = P.sb(st, "rw12", [128, 2]); t_w = Tk()
        gte = [P.sb(st, "rgte", [128, 8]) for _ in range(2)]; tmp = P.sb(st, "rtmp", [128, 8]); t_gte = [Tk(), Tk()]
        for i in range(NQ + 1):
            b = i % 2
            if i < NQ:
                P.idma(xt[b][:], H3, qi[:, i:i + 1], reads=[t_h3, t_qi], writes=[t_x[b]])
            else:
                P.dma(xt[b][:], H3[TP:TP + 128, :], reads=[t_h3], writes=[t_x[b]])
            P.dma(HQ[i * 128:(i + 1) * 128, :], xt[b][:], reads=[t_x[b]], writes=[t_hq])
            P.act(sq[:], xt[b][:], AF.Square, reads=[t_x[b]], writes=[t_sq, t_ss], accum_out=ss[:])
            P.op("dve", lambda e: e.tensor_scalar(ss[:], ss[:], 1.0 / D, EPS, ALU.mult, ALU.add), reads=[t_ss], writes=[t_ss])
            P.act(ss[:], ss[:], AF.Sqrt, reads=[t_ss], writes=[t_ss])
            P.op("dve", lambda e: e.reciprocal(ss[:], ss[:]), reads=[t_ss], writes=[t_ss])
            P.op("dve", lambda e, b=b: e.scalar_tensor_tensor(sq[:], xt[b][:], ss[:, 0:1], gt[:], ALU.mult, ALU.mult),
                 reads=[t_x[b], t_ss, t_gt], writes=[t_sq])
            for kg in range(4):
                pb = kg % 2
                for j in range(4):
                    k = kg * 4 + j
                    P.tr(ptr[pb][:, j, :], sq[:, k * 128:(k + 1) * 128], C.idf[:], reads=[t_sq, C.t_id], writes=[t_ptr[pb]])
                P.copy("act", xT[:, kg * 4:kg * 4 + 4, :], ptr[pb][:], reads=[t_ptr[pb]], writes=[t_xT])
            for k in range(16):
                P.mm(pl[:], xT[:, k, :], rw[:, k, :], start=(k == 0), stop=(k == 15), reads=[t_xT, t_rw], writes=[t_pl])
            P.copy("dve", lg[:], pl[:], reads=[t_pl], writes=[t_lg])
            P.op("dve", lambda e: e.max(m8[:], lg[:]), reads=[t_lg], writes=[t_m8])
            P.op("dve", lambda e: e.tensor_sub(w12[:, 0:1], m8[:, 1:2], m8[:, 0:1]), reads=[t_m8], writes=[t_w])
            P.act(w12[:, 0:1], w12[:, 0:1], AF.Exp, reads=[t_w], writes=[t_w])
            P.op("dve", lambda e: e.tensor_scalar_add(w12[:, 0:1], w12[:, 0:1], 1.0), reads=[t_w], writes=[t_w])
            P.op("dve", lambda e: e.reciprocal(w12[:, 0:1], w12[:, 0:1]), reads=[t_w], writes=[t_w])
            P.op("dve", lambda e: e.tensor_scalar(w12[:, 1:2], w12[:, 0:1], -1.0, 1.0, ALU.mult, ALU.add), reads=[t_w], writes=[t_w])
            g_ = gte[b]
            P.op("dve", lambda e, g_=g_: e.tensor_scalar(g_[:], lg[:], m8[:, 0:1], w12[:, 0:1], ALU.is_equal, ALU.mult), reads=[t_lg, t_m8, t_w], writes=[t_gte[b]])
            P.op("dve", lambda e: e.tensor_scalar(tmp[:], lg[:], m8[:, 1:2], w12[:, 1:2], ALU.is_equal, ALU.mult), reads=[t_lg, t_m8, t_w], writes=[t_gte[b]])
            P.op("dve", lambda e, g_=g_: e.tensor_add(g_[:], g_[:], tmp[:]), reads=[t_gte[b]], writes=[t_gte[b]])
            P.dma(GATE8[i * 128:(i + 1) * 128, :], g_[:], reads=[t_gte[b]], writes=[t_gate])
        P.flush()
    t_acc = Tk()
    t_acc.w = t_hq.w
    prev = HQ
    for e_ in range(8):
        dst = ACC[e_ % 2]
        swiglu_ffn(P, C, HQ, I["ssm_norm_ffn"], I["moe_wg"][e_], I["moe_wu"][e_], I["moe_wd"][e_], 7168, NQ + 1, prev, dst,
                   t_acc, t_acc, rowscale=GATE8[:, e_:e_ + 1], t_rs=t_gate)
        prev = dst
    with ExitStack() as st:
        gt = P.sb(st, "ngt", [128, D]); t_gt = Tk()
        P.dma(gt[:], I["final_norm"].partition_broadcast(128), writes=[t_gt])
        ht = [P.sb(st, "nht", [128, D]) for _ in range(2)]; t_ht = [Tk(), Tk()]
        sq = P.sb(st, "nsq", [128, D]); t_sq = Tk()
        ss = P.sb(st, "nss", [128, 1]); t_ss = Tk()
        yo = [P.sb(st, "nyo", [128, D]) for _ in range(2)]; t_yo = [Tk(), Tk()]
        for i in range(NQ + 1):
            b = i % 2
            P.dma(ht[b][:], prev[i * 128:(i + 1) * 128, :], reads=[t_acc], writes=[t_ht[b]])
            P.act(sq[:], ht[b][:], AF.Square, reads=[t_ht[b]], writes=[t_sq, t_ss], accum_out=ss[:])
            P.op("dve", lambda e: e.tensor_scalar(ss[:], ss[:], 1.0 / D, EPS, ALU.mult, ALU.add), reads=[t_ss], writes=[t_ss])
            P.act(ss[:], ss[:], AF.Sqrt, reads=[t_ss], writes=[t_ss])
            P.op("dve", lambda e: e.reciprocal(ss[:], ss[:]), reads=[t_ss], writes=[t_ss])
            P.op("dve", lambda e, b=b: e.scalar_tensor_tensor(yo[b][:], ht[b][:], ss[:, 0:1], gt[:], ALU.mult, ALU.mult),
                 reads=[t_ht[b], t_ss, t_gt], writes=[t_yo[b]])
            if i < NQ:
                P.dma(outs["y_q"][i * 128:(i + 1) * 128, :], yo[b][:], reads=[t_yo[b]], writes=[outs["t"]])
            else:
                P.dma(outs["y_s"][0:4, :], yo[b][0:4, :], reads=[t_yo[b]], writes=[outs["t"]])
        P.flush()


def outs_dict_sconv(outs, nc, outp):
    if "sconv_p" not in outs:
        outs["sconv_p"] = outp("sconv_p", [3, 6144])
        outs["sconv_s"] = outp("sconv_s", [3, 6144])
    return outs


def build(TP=4096, debug=False, limit=None, stages="AKGN", n_cores=8):
    nc = bass.Bass("TRN2", target_bir_lowering=False)
    _FILL_REGS.clear()
    NT = TP // 128 + 1
    TT = NT * 128
    kind_dbg = "ExternalOutput" if debug else "Internal"

    build.inputs = []

    def inp(name, shape, dt=F32):
        build.inputs.append(name)
        return nc.dram_tensor(name, list(shape), dt, kind="ExternalInput").ap()

    xcat = inp("xcat", [TT, D])
    hyb_norm_mix = inp("hyb_norm_mix", [1, D])
    hyb_w_in = inp("hyb_w_in", [D, HYB_IN])
    with ExitStack() as glob:
        P = Prog(nc, glob)
        P.limit = limit
        C = Ctx()
        C.glob = glob
        make_consts(P, C)
        QKVT = nc.dram_tensor("QKVT", [NFM, TT], F32, kind=kind_dbg).ap()
        PROJ = nc.dram_tensor("PROJ", [TT, NPROJ], F32, kind=kind_dbg).ap()
        t_A = Tk()
        norm_linear(P, C, xcat, hyb_norm_mix[0:1, :], hyb_w_in, NT, HYB_IN, NFM, QKVT, PROJ, t_A)
        ROPE = inp("ROPE", [TT, 128])
        win_cache = inp("win_cache", [512, 512])

        def outp(name, shape):
            return nc.dram_tensor(name, list(shape), F32, kind="ExternalOutput").ap()
        WP = min(512, TP)
        outs = {"t": Tk(), "cmp_p": outp("cmp_p", [TP, 512]), "sel_p": outp("sel_p", [TP, 512]),
                "win_p": outp("win_p", [WP, 512]), "cmp_s": outp("cmp_s", [4, 512]),
                "sel_s": outp("sel_s", [4, 512]), "win_s": outp("win_s", [512, 512]),
                "gconv_p": outp("gconv_p", [3, NFM]), "gconv_s": outp("gconv_s", [3, NFM])}
        KVR = nc.dram_tensor("KVR", [TT, 1536], F32, kind=kind_dbg).ap()
        KT = nc.dram_tensor("KT", [8, 128, TT], F32, kind=kind_dbg).ap()
        t_kvr = Tk()
        kv_outputs(P, C, PROJ, QKVT, ROPE, win_cache, outs, TP, t_A, KVR, KT, t_kvr)
        I = {"GCONST": inp("GCONST", [128, 7 * 128]), "gdn_cw": inp("gdn_cw", [128, 8, 3, 4]),
             "gdn_a_log": inp("gdn_a_log", [1, 8]), "gdn_dt_bias": inp("gdn_dt_bias", [1, 8]),
             "gdn_norm_w": inp("gdn_norm_w", [1, 128]), "VMASK": inp("VMASK", [TT, 1]),
             "gdn_state": inp("gdn_state", [8, 128, 128]), "gdn_convT": inp("gdn_convT", [NFM, 3])}
        outs["gst_p"] = outp("gst_p", [8, 128, 128])
        outs["gst_s"] = outp("gst_s", [8, 128, 128])
        MIX = nc.dram_tensor("MIX", [TT, 2048], F32, kind=kind_dbg).ap()
        t_mix = Tk()
        if "N" in stages or "S" in stages:
            I["cmp_w1"] = inp("cmp_w1", [2, 4096, 256])
            I["cmp_w2"] = inp("cmp_w2", [2, 256, 128])
            I["cmp_peT"] = inp("cmp_peT", [2, 128, 32])
            I["CAUS"] = inp("CAUS", [128, 128])
        if "G" in stages:
            gdn_stage(P, C, QKVT, PROJ, MIX, I, outs, TP, t_A, t_mix)
        if "N" in stages:
            n_cmp = (TP - 32) // 16 + 1
            ncp = -(-n_cmp // 128) * 128
            nsel = max(8, TP // 64)
            I["MSEL_P"] = inp("MSEL_P", [ncp, nsel])
            with ExitStack() as stn:
                KCT = P.sb(stn, "KCT", [128, 2, ncp])
                VC = P.sb(stn, "VC", [128, ncp // 128, 2, 128])
                t_kc = Tk()
                compress(P, C, KT[0:4], n_cmp, I, KCT, VC, t_kvr, t_kc)
                tiles = []
                for i in range(TP // 128):
                    w0 = max(0, 128 * i - 512)
                    tiles.append(dict(r0=128 * i, qpos0=128 * i, nk=128 * (i + 1), w0=w0, nw=128 * (i + 1) - w0, wpos0=0))
                X = dict(SKT=KT[4:6], SELR=KVR[:, 512:1024], WKT=KT[6:8], WINR=KVR[:, 1024:1536], ncp=ncp, nsel=nsel,
                         msel=I["MSEL_P"])
                nsa_qtiles(P, C, tiles, X, PROJ, ROPE, MIX, I, KCT, VC, t_A, t_kvr, t_kc, t_mix)
        if "S" in stages:
            NKS = PAST + 128
            n_cmp_s = (PAST + 4 - 32) // 16 + 1
            ncp_s = -(-n_cmp_s // 128) * 128
            nsel_s = NKS // 64
            I.update({"pt_row": inp("pt_row", [1, PAST // 128], I32), "IOTA": inp("IOTA", [128, 1]),
                      "pool_cmp": inp("pool_cmp", [I_NPHYS[0] * 128, 512]), "pool_sel": inp("pool_sel", [I_NPHYS[0] * 128, 512]),
                      "win_cache": win_cache, "MSEL_S": inp("MSEL_S", [ncp_s, nsel_s])})
            XS = dict(CT=nc.dram_tensor("CT_S", [4, 128, PAST], F32).ap(), SELR=nc.dram_tensor("SELR_S", [NKS, 512], F32).ap(),
                      SKT=nc.dram_tensor("SKT_S", [2, 128, NKS], F32).ap(), WINR=nc.dram_tensor("WINR_S", [640, 512], F32).ap(),
                      WKT=nc.dram_tensor("WKT_S", [2, 128, 640], F32).ap(), ncp=ncp_s, nsel=nsel_s, msel=I["MSEL_S"])
            XS["SELR"] = XS["SELR"]
            t_sx = Tk()
            sample_ctx(P, C, I, KVR, KT, TP, t_kvr, XS, t_sx)
            with ExitStack() as stn:
                KCT = P.sb(stn, "KCTs", [128, 2, ncp_s])
                VC = P.sb(stn, "VCs", [128, ncp_s // 128, 2, 128])
                t_kc = Tk()
                compress(P, C, XS["CT"], n_cmp_s, I, KCT, VC, t_sx, t_kc)
                tiles = [dict(r0=TP, qpos0=PAST, nk=NKS, w0=0, nw=640, wpos0=PAST - 512)]
                XQ = dict(XS)
                XQ["SELR"] = XS["SELR"]
                nsa_qtiles(P, C, tiles, XQ, PROJ, ROPE, MIX, I, KCT, VC, t_A, t_sx, t_kc, t_mix)
        if "S" not in stages:
            with ExitStack() as stz:
                zt = P.sb(stz, "zt", [128, 1024])
                t_z = Tk()
                P.op("pool", lambda e: e.memset(zt[:], 0.0), writes=[t_z])
                P.dma(MIX[TP:TT, 1024:2048], zt[:], reads=[t_z], writes=[t_mix])
                P.flush()
        if "F" in stages:
            H1 = nc.dram_tensor("H1", [TT, D], F32, kind=kind_dbg).ap()
            H2 = nc.dram_tensor("H2", [TT, D], F32, kind=kind_dbg).ap()
            t_h1, t_h2 = Tk(), Tk()
            hyb_w_out = inp("hyb_w_out", [D, D])
            norm_linear(P, C, MIX, None, hyb_w_out, NT, D, 0, None, H1, t_h1, sb_tiles=17, t_in=t_mix, norm=False,
                        resid=xcat)
            ffn_g, ffn_u, ffn_d = inp("ffn_w_gate", [D, 5632]), inp("ffn_w_up", [D, 5632]), inp("ffn_w_down", [5632, D])
            hyb_norm_ffn = inp("hyb_norm_ffn", [1, D])
            swiglu_ffn(P, C, H1, hyb_norm_ffn[0:1, :], ffn_g, ffn_u, ffn_d, 5632, NT, H1, H2, t_h1, t_h2)
        if "M" in stages:
            XBCT = nc.dram_tensor("XBCT", [6144, TT], F32, kind=kind_dbg).ap()
            ZDT = nc.dram_tensor("ZDT", [TT, 4160], F32, kind=kind_dbg).ap()
            Yt = nc.dram_tensor("Yssd", [TT, 4096], F32, kind=kind_dbg).ap()
            H3 = nc.dram_tensor("H3", [TT, D], F32, kind=kind_dbg).ap()
            t_l1, t_y, t_h3 = Tk(), Tk(), Tk()
            ssm_w_in = inp("ssm_w_in_r", [D, 10304])
            ssm_norm_mix = inp("ssm_norm_mix", [1, D])
            norm_linear(P, C, H2, ssm_norm_mix[0:1, :], ssm_w_in, NT, 10304, 6144, XBCT, ZDT, t_l1, t_in=t_h2)
            for r in range(3):
                P.dma(outs_dict_sconv(outs, nc, outp)["sconv_p"][r:r + 1, :], XBCT[:, TP - 3 + r:TP - 2 + r].rearrange("c o -> o c"),
                      reads=[t_l1], writes=[outs["t"]], allow_slow_non_contiguous=True)
                P.dma(outs["sconv_s"][r:r + 1, :], XBCT[:, TP + 1 + r:TP + 2 + r].rearrange("c o -> o c"),
                      reads=[t_l1], writes=[outs["t"]], allow_slow_non_contiguous=True)
            I.update({"SCONST": inp("SCONST", [128, 512]), "ssm_cw": inp("ssm_cw", [128, 48, 5]),
                      "ssm_a_log": inp("ssm_a_log", [1, 64]), "ssm_dt_bias": inp("ssm_dt_bias", [1, 64]),
                      "ssm_d_skip": inp("ssm_d_skip", [1, 64]), "ssm_norm_w": inp("ssm_norm_w", [1, 4096]),
                      "ssm_state": inp("ssm_state", [64, 64, 128]), "ssm_convT": inp("ssm_convT", [6144, 3])})
            outs["sst_p"] = outp("sst_p", [64, 64, 128])
            outs["sst_s"] = outp("sst_s", [64, 64, 128])
            ssd_stage(P, C, XBCT, ZDT, Yt, I, outs, TP, t_l1, t_y)
            ssm_w_out = inp("ssm_w_out", [4096, D])
            norm_linear(P, C, Yt, None, ssm_w_out, NT, D, 0, None, H3, t_h3, sb_tiles=8, cb=128, t_in=t_y, norm=False,
                        resid=H2, t_res=t_h2)
        if "L" in stages:
            NTP_ = TP // 128
            nsplit = 4 if NTP_ % 4 == 0 else (2 if NTP_ % 2 == 0 else 1)
            I.update({"QIDX": inp("QIDX", [128, NTP_ // nsplit], I32), "ssm_norm_ffn": inp("ssm_norm_ffn", [1, D]),
                      "moe_router": inp("moe_router", [D, 8]), "moe_wg": inp("moe_wg", [8, D, 7168]),
                      "moe_wu": inp("moe_wu", [8, D, 7168]), "moe_wd": inp("moe_wd", [8, 7168, D]),
                      "final_norm": inp("final_norm", [1, D])})
            I["ssm_norm_ffn"] = I["ssm_norm_ffn"][0:1, :]
            I["final_norm"] = I["final_norm"][0:1, :]
            outs["y_q"] = outp("y_q", [TP // nsplit, D])
            outs["y_s"] = outp("y_s", [4, D])
            moe_local_stage(P, C, H3, I, outs, TP, t_h3, kind_dbg)
        if "E" in stages:
            I.update({"FLAGS": inp("FLAGS", [128, 12]), "ssm_norm_ffn": inp("ssm_norm_ffn", [1, D]),
                      "moe_router": inp("moe_router", [D, 8]), "moe_wg": inp("moe_wg", [D, 7168]),
                      "moe_wu": inp("moe_wu", [D, 7168]), "moe_wd": inp("moe_wd", [7168, D]),
                      "final_norm": inp("final_norm", [1, D])})
            I["ssm_norm_ffn"] = I["ssm_norm_ffn"][0:1, :]
            I["final_norm"] = I["final_norm"][0:1, :]
            outs["y_p"] = outp("y_p", [TP, D])
            outs["y_s"] = outp("y_s", [4, D])
            moe_stage(P, C, H3, I, outs, TP, NT, t_h3, n_cores, kind_dbg)
        P.flush(final=True)
        build.total = P.total
    return nc


_NC_CACHE = {}
STAGES = "AKGNSFML"


def _rope_table(TP):
    pos = np.concatenate([np.arange(TP), PAST + np.arange(128)]).astype(np.float32)
    inv = (1.0 / (10000.0 ** (np.arange(64, dtype=np.float32) / 64))).astype(np.float32)
    ang = pos[:, None] * inv[None, :]
    return np.concatenate([np.cos(ang), np.sin(ang)], axis=1).astype(np.float32)


def kernel(**inputs):
    f = lambda k: np.asarray(inputs[k])
    x_prompt, x_sample = f("x_prompt"), f("x_sample")
    B, TP, _ = x_prompt.shape
    NB = x_sample.shape[0]
    key = (TP, STAGES)
    I_NPHYS[0] = f("cache_cmp_kv").shape[1]
    if key not in _NC_CACHE:
        _NC_CACHE[key] = build(TP=TP, stages=STAGES, n_cores=8)
    nc = _NC_CACHE[key]
    TT = TP + 128
    ca = np.ascontiguousarray
    rope = _rope_table(TP)
    vmask = np.zeros((TT, 1), np.float32)
    vmask[:TP + 4] = 1
    caus, msel = host_nsa_consts(TP)
    gcw = f("hyb_gdn_conv_w")[0]
    scw = f("ssm_conv_w")[0]
    scb = f("ssm_conv_b")[0]
    ssm_cw = np.concatenate([scw.reshape(4, 48, 128).transpose(2, 1, 0), scb.reshape(48, 128).T[:, :, None]], axis=2)
    swi = f("ssm_w_in")[0]
    shared = {
        "hyb_norm_mix": ca(f("hyb_norm_mix")[0][None]), "hyb_w_in": ca(f("hyb_w_in")[0]), "ROPE": rope,
        "GCONST": host_consts(), "gdn_cw": ca(gcw.reshape(4, 3, 8, 128).transpose(3, 2, 1, 0)),
        "gdn_a_log": ca(f("hyb_gdn_a_log")[0][None]), "gdn_dt_bias": ca(f("hyb_gdn_dt_bias")[0][None]),
        "gdn_norm_w": ca(f("hyb_gdn_norm_w")[0][None]), "VMASK": vmask,
        "cmp_w1": ca(f("hyb_cmp_w1")[0]), "cmp_w2": ca(f("hyb_cmp_w2")[0]),
        "cmp_peT": ca(f("hyb_cmp_pe")[0].transpose(0, 2, 1)), "CAUS": caus, "MSEL_P": msel,
        "hyb_w_out": ca(f("hyb_w_out")[0]), "ffn_w_gate": ca(f("ffn_w_gate")[0]), "ffn_w_up": ca(f("ffn_w_up")[0]),
        "ffn_w_down": ca(f("ffn_w_down")[0]), "hyb_norm_ffn": ca(f("hyb_norm_ffn")[0][None]),
        "ssm_w_in_r": ca(np.concatenate([swi[:, 4096:10240], swi[:, :4096], swi[:, 10240:]], axis=1)),
        "ssm_norm_mix": ca(f("ssm_norm_mix")[0][None]), "SCONST": host_ssd_consts(), "ssm_cw": ca(ssm_cw.astype(np.float32)),
        "ssm_a_log": ca(f("ssm_a_log")[0][None]), "ssm_dt_bias": ca(f("ssm_dt_bias")[0][None]),
        "ssm_d_skip": ca(f("ssm_d_skip")[0][None]), "ssm_norm_w": ca(f("ssm_norm_w")[0][None]),
        "ssm_w_out": ca(f("ssm_w_out")[0]), "ssm_norm_ffn": ca(f("ssm_norm_ffn")[0][None]),
        "moe_router": ca(f("moe_router")[0]), "final_norm": ca(f("final_norm")[None]),
        "IOTA": np.arange(128, dtype=np.float32)[:, None],
        "pool_cmp": f("cache_cmp_kv")[0].reshape(-1, 512), "pool_sel": f("cache_sel_kv")[0].reshape(-1, 512),
        "MSEL_S": host_msel((PAST + 4 - 32) // 16 + 1, -(-((PAST + 4 - 32) // 16 + 1) // 128) * 128, (PAST + 128) // 64),
    }
    in_maps = []
    for c in range(8):
        xcat = np.zeros((TT, D), np.float32)
        xcat[:TP] = x_prompt[c % B]
        xcat[TP:TP + 4] = x_sample[c]
        fl = np.zeros((128, 12), np.float32)
        fl[:, 0] = float(c == 0)
        fl[:, 1] = float(c == 1)
        fl[:, 2 + c] = 1.0
        fl[:, 10 + (c % 2)] = 1.0
        m = dict(shared)
        nsplit = 4 if (TP // 128) % 4 == 0 else (2 if (TP // 128) % 2 == 0 else 1)
        NQ = TP // 128 // nsplit
        q = (c // B) % nsplit
        qidx = (q * NQ * 128 + np.arange(NQ)[None, :] * 128 + np.arange(128)[:, None]).astype(np.int32)
        m["QIDX"] = qidx
        m.update({"xcat": xcat, "win_cache": ca(f("cache_win_kv")[0, c].reshape(512, 512)),
                  "gdn_state": ca(f("state_gdn")[0, c]), "gdn_convT": ca(f("state_gdn_conv")[0, c].T),
                  "ssm_state": ca(f("state_ssm")[0, c]), "ssm_convT": ca(f("state_ssm_conv")[0, c].T),
                  "pt_row": ca(f("page_table")[c][None].astype(np.int32)),
                  "FLAGS": fl, "moe_wg": f("moe_w_gate")[0], "moe_wu": f("moe_w_up")[0],
                  "moe_wd": f("moe_w_down")[0]})
        m = {k: v for k, v in m.items() if k in build.inputs}
        in_maps.append(m)
    res = run_bass_kernel_spmd(nc, in_maps, core_ids=list(range(8))).results
    WP = min(512, TP)
    pk = lambda name, shp: np.stack([res[b][name].reshape(shp) for b in range(B)])
    sk = lambda name, shp: np.stack([res[c][name].reshape(shp) for c in range(NB)])
    nsplit = 4 if (TP // 128) % 4 == 0 else (2 if (TP // 128) % 2 == 0 else 1)
    QN = TP // nsplit
    y_p = np.zeros((B, TP, D), np.float32)
    for c in range(8):
        b, q = c % B, (c // B) % nsplit
        y_p[b, q * QN:(q + 1) * QN] = res[c]["y_q"].reshape(QN, D)
    outs = (
        y_p, sk("y_s", (4, D)),
        pk("cmp_p", (TP, 2, 2, 128))[None], sk("cmp_s", (4, 2, 2, 128))[None],
        pk("sel_p", (TP, 2, 2, 128))[None], sk("sel_s", (4, 2, 2, 128))[None],
        pk("win_p", (WP, 2, 2, 128))[None], sk("win_s", (512, 2, 2, 128))[None],
        pk("gconv_p", (3, NFM))[None], sk("gconv_s", (3, NFM))[None],
        pk("gst_p", (8, 128, 128))[None], sk("gst_s", (8, 128, 128))[None],
        pk("sconv_p", (3, 6144))[None], sk("sconv_s", (3, 6144))[None],
        pk("sst_p", (64, 64, 128))[None], sk("sst_s", (64, 64, 128))[None],
    )
    return outs
```

```python
import numpy as np
import concourse.bass as bass
import concourse.mybir as mybir
from concourse.bass_utils import run_bass_kernel_spmd
from contextlib import ExitStack

F32 = mybir.dt.float32
BF16 = mybir.dt.bfloat16
I32 = mybir.dt.int32
AF = mybir.ActivationFunctionType
ALU = mybir.AluOpType
AX = mybir.AxisListType

D = 2048
HYB_IN = 6696
NFM = 3072
NPROJ = HYB_IN - NFM
C_GZ, C_GA, C_GB, C_NQ, C_CK, C_CV, C_SK, C_SV, C_WK, C_WV, C_NG = (
    0, 1024, 1032, 1040, 2064, 2320, 2576, 2832, 3088, 3344, 3600)
PAST = 16384
EPS = 1e-6


class Tk:
    __slots__ = ("w", "r", "excl")

    def __init__(self, excl=False):
        self.w = None
        self.r = {}
        self.excl = excl


class Prog:
    ENG = ("pe", "act", "dve", "pool", "sp")

    def __init__(self, nc, es, ndma=24):
        self.nc = nc
        self.es = es
        self.ops = {e: [] for e in self.ENG}
        self.cnt = {e: 0 for e in self.ENG}
        self.sem = {e: es.enter_context(nc.semaphore("s_" + e)) for e in self.ENG}
        self.ndma = ndma
        self.dsem = [es.enter_context(nc.semaphore("d%d" % i)) for i in range(ndma)]
        self.dcnt = [0] * ndma
        self.dnext = 0
        self.seen = {e: {} for e in self.ENG}
        self.uid = 0
        self.total = 0
        self.limit = None
        self.icnt = 0
        self.isem = es.enter_context(nc.semaphore("isem"))
        self.iscr = es.enter_context(nc.sbuf_tensor("iscr", [128, 1], F32))

    def name(self, s):
        self.uid += 1
        return "%s_%d" % (s, self.uid)

    def sb(self, st, name, shape, dt=F32):
        return st.enter_context(self.nc.sbuf_tensor(self.name(name), list(shape), dt))

    def ps(self, st, name, shape, dt=F32):
        esz = 4 if dt == F32 else 2
        n = 1
        for d in shape[1:]:
            n *= d
        assert n * esz <= 2048
        t = st.enter_context(self.nc.psum_tensor(self.name(name), [128, 2048 // esz], dt))
        v = t[:, 0:n]
        if len(shape) == 3:
            v = v.rearrange("p (a b) -> p a b", a=shape[1])
        return v

    def dram(self, name, shape, dt=F32, kind="Internal"):
        return self.nc.dram_tensor(name, list(shape), dt, kind=kind).ap()

    def _need(self, eng, tok, waits):
        if tok is None:
            return
        if tok[0] == "e":
            _, e2, idx = tok
            if e2 == eng and self.cnt[eng] - idx > 1:
                return
            key = ("e", e2)
            val = idx
        else:
            _, k, val = tok
            key = ("d", k)
        if self.seen[eng].get(key, 0) >= val:
            return
        if waits.get(key, 0) < val:
            waits[key] = val

    def _deps(self, eng, reads, writes, waits=None):
        waits = {} if waits is None else waits
        for t in reads:
            self._need(eng, t.w, waits)
        for t in writes:
            self._need(eng, t.w, waits)
            for tok in t.r.values():
                self._need(eng, tok, waits)
        wl = []
        for key, val in waits.items():
            self.seen[eng][key] = val
            sem = self.sem[key[1]] if key[0] == "e" else self.dsem[key[1]]
            wl.append((sem, val))
        return wl

    def _mark(self, tok, reads, writes):
        rk = tok[:2]
        for t in reads:
            t.r[rk] = tok
        for t in writes:
            t.w = tok
            t.r = {}

    def op(self, eng, fn, reads=(), writes=()):
        self.total += 1
        if self.limit is not None and self.total > self.limit:
            return None
        if any(t.excl for t in reads):
            writes = list(writes) + [t for t in reads if t.excl]
            reads = [t for t in reads if not t.excl]
        wl = self._deps(eng, reads, writes)
        self.cnt[eng] += 1
        tok = ("e", eng, self.cnt[eng])
        self.ops[eng].append((wl, fn, (self.sem[eng], 1)))
        self._mark(tok, reads, writes)
        return tok

    def dma(self, out, in_, reads=(), writes=(), q="sp", **kw):
        self.total += 1
        if self.limit is not None and self.total > self.limit:
            return None
        k = self.dnext
        self.dnext = (self.dnext + 1) % self.ndma
        waits = {}
        if self.dcnt[k] > 0:
            self._need(q, ("d", k, self.dcnt[k]), waits)
        wl = self._deps(q, reads, writes, waits)
        self.dcnt[k] += 16
        tok = ("d", k, self.dcnt[k])
        self.ops[q].append((wl, (lambda e, o=out, i=in_, kw=kw: e.dma_start(out=o, in_=i, **kw)),
                            (self.dsem[k], 16)))
        self._mark(tok, reads, writes)
        return tok

    def idma(self, out, in_, idx, reads=(), writes=()):
        if self.total is not None:
            self.total += 1
            if self.limit is not None and self.total > self.limit:
                return None
        wl = self._deps("pool", reads, writes)
        self.cnt["pool"] += 1
        tok = ("e", "pool", self.cnt["pool"])
        isem, iscr = self.isem, self.iscr
        self.icnt += 16
        target = self.icnt

        def fn(e, o=out, i=in_, x=idx, target=target):
            e.indirect_dma_start(out=o, out_offset=None, in_=i,
                                 in_offset=bass.IndirectOffsetOnAxis(ap=x, axis=0)).then_inc(isem, 16)
            e.wait_ge(isem, target)
            return e.memset(iscr[:], 0.0)
        self.ops["pool"].append((wl, fn, (self.sem["pool"], 1)))
        self._mark(tok, reads, writes)
        return tok

    def flush(self, final=False):
        wl = [(self.dsem[k], self.dcnt[k]) for k in range(self.ndma) if self.dcnt[k] > 0]
        for e in ("pe", "act", "dve", "pool"):
            if self.cnt[e] > 0:
                wl.append((self.sem[e], self.cnt[e]))
        for e in self.ENG:
            self.ops[e].append((list(wl), None, None))
            for k in range(self.ndma):
                self.seen[e][("d", k)] = self.dcnt[k]
            for e2 in ("pe", "act", "dve", "pool"):
                self.seen[e][("e", e2)] = self.cnt[e2]
        ops = self.ops
        self.ops = {e: [] for e in self.ENG}
        with self.nc.Block() as block:
            def run(eng, lst):
                for wl, fn, inc in lst:
                    for sem, val in wl:
                        eng.wait_ge(sem, val)
                    if fn is not None:
                        fn(eng).then_inc(inc[0], inc[1])

            @block.tensor
            def _(e):
                run(e, ops["pe"])

            @block.scalar
            def _(e):
                run(e, ops["act"])

            @block.vector
            def _(e):
                run(e, ops["dve"])

            @block.gpsimd
            def _(e):
                run(e, ops["pool"])

            @block.sync
            def _(e):
                run(e, ops["sp"])

    def mm(self, out, lhsT, rhs, start=True, stop=True, reads=(), writes=()):
        return self.op("pe", lambda e: e.matmul(out, lhsT, rhs, start=start, stop=stop), reads, writes)

    def tr(self, out, in_, ident, reads=(), writes=()):
        return self.op("pe", lambda e: e.transpose(out, in_, ident), reads, writes)

    def act(self, out, in_, func, reads=(), writes=(), **kw):
        return self.op("act", lambda e: e.activation(out, in_, func, **kw), reads, writes)

    def copy(self, eng, out, in_, reads=(), writes=()):
        if eng == "act":
            return self.op("act", lambda e: e.copy(out, in_), reads, writes)
        return self.op(eng, lambda e: e.tensor_copy(out, in_), reads, writes)


class Ctx:
    pass


_FILL_REGS = {}
I_NPHYS = [1280]


def fillreg(e, val):
    key = (id(e), val)
    if key not in _FILL_REGS:
        _FILL_REGS[key] = e.to_reg(val)
    return _FILL_REGS[key]


def make_consts(P, C):
    st = C.glob
    C.idf = P.sb(st, "idf", [128, 128], F32)
    C.idb = P.sb(st, "idb", [128, 128], BF16)
    C.t_id = Tk()
    P.op("pool", lambda e: e.memset(C.idf[:], 1.0), writes=[C.t_id])
    P.op("pool", lambda e: e.affine_select(C.idf[:], C.idf[:], [[-1, 128]], ALU.is_equal, fillreg(e, 0.0),
                                           base=0, channel_multiplier=1), reads=[C.t_id], writes=[C.t_id])
    P.op("dve", lambda e: e.tensor_copy(C.idb[:], C.idf[:]), reads=[C.t_id], writes=[C.t_id])


def norm_linear(P, C, x_ap, gamma_ap, w_ap, ntiles, n_out, fm_cols, fm_out, tm_out, t_out, sb_tiles=17,
                cb=256, t_in=None, norm=True, resid=None, t_res=None):
    Dm = x_ap.shape[1]
    KC = Dm // 128
    with ExitStack() as st:
        XT = P.sb(st, "XT", [128, KC, sb_tiles * 128], BF16)
        t_XT = Tk()
        t_g = Tk()
        rd_in = [t_in] if t_in is not None else []
        rd_res = [t_res] if t_res is not None else []
        if norm:
            gt = P.sb(st, "gt", [128, Dm])
            P.dma(gt[:], gamma_ap.partition_broadcast(128), writes=[t_g])
        rb = [P.sb(st, "rb", [128, cb]) for _ in range(2)] if resid is not None else None
        t_rb = [Tk(), Tk()]
        xt = [P.sb(st, "xt", [128, Dm]) for _ in range(2)]
        t_x = [Tk(), Tk()]
        sq = P.sb(st, "sq", [128, Dm], BF16)
        t_sq = Tk()
        ss = [P.sb(st, "ss", [128, 1]) for _ in range(2)]
        t_ss = [Tk(), Tk()]
        xn = [P.sb(st, "xn", [128, Dm], BF16) for _ in range(2)]
        t_xn = [Tk(), Tk()]
        ptr = [P.ps(st, "ptr", [128, 4, 128], BF16) for _ in range(2)]
        t_ptr = [Tk(1), Tk(1)]
        wf = [P.sb(st, "wf", [128, KC, cb]) for _ in range(2)]
        t_wf = [Tk(), Tk()]
        wb = [P.sb(st, "wb", [128, KC, cb], BF16) for _ in range(2)]
        t_wb = [Tk(), Tk()]
        po = [P.ps(st, "po", [128, 512]) for _ in range(3)]
        t_po = [Tk(1) for _ in range(3)]
        ob = [P.sb(st, "ob", [128, 512]) for _ in range(3)]
        t_ob = [Tk() for _ in range(3)]
        ipo = 0
        iw = 0
        for s0 in range(0, ntiles, sb_tiles):
            nts = min(sb_tiles, ntiles - s0)
            for i in range(nts):
                b = i % 2
                r0 = (s0 + i) * 128
                P.dma(xt[b][:], x_ap[r0:r0 + 128, :], reads=rd_in, writes=[t_x[b]])
                if norm:
                    P.act(sq[:], xt[b][:], AF.Square, reads=[t_x[b]], writes=[t_sq, t_ss[b]], accum_out=ss[b][:])
                    P.op("dve", lambda e, b=b: e.tensor_scalar(ss[b][:], ss[b][:], 1.0 / Dm, EPS, ALU.mult, ALU.add),
                         reads=[t_ss[b]], writes=[t_ss[b]])
                    P.act(ss[b][:], ss[b][:], AF.Sqrt, reads=[t_ss[b]], writes=[t_ss[b]])
                    P.op("dve", lambda e, b=b: e.reciprocal(ss[b][:], ss[b][:]), reads=[t_ss[b]], writes=[t_ss[b]])
                    P.op("dve", lambda e, b=b: e.scalar_tensor_tensor(xn[b][:], xt[b][:], ss[b][:, 0:1], gt[:],
                                                                      ALU.mult, ALU.mult),
                         reads=[t_x[b], t_ss[b], t_g], writes=[t_xn[b]])
                else:
                    P.op("dve", lambda e, b=b: e.tensor_copy(xn[b][:], xt[b][:]), reads=[t_x[b]], writes=[t_xn[b]])
                for kg in range(KC // 4):
                    pb = kg % 2
                    for j in range(4):
                        k = kg * 4 + j
                        P.tr(ptr[pb][:, j, :], xn[b][:, k * 128:(k + 1) * 128], C.idb[:],
                             reads=[t_xn[b], C.t_id], writes=[t_ptr[pb]])
                    P.copy("dve" if kg % 2 == 0 else "act", XT[:, kg * 4:kg * 4 + 4, i * 128:(i + 1) * 128],
                           ptr[pb][:], reads=[t_ptr[pb]], writes=[t_XT])
            def load_w(c0, w_):
                nc_ = min(cb, n_out - c0)
                P.dma(wf[w_][:, :, 0:nc_], w_ap[:, c0:c0 + nc_].rearrange("(k p) n -> p k n", p=128),
                      writes=[t_wf[w_]])
                P.op("pool", lambda e, w_=w_, nc_=nc_: e.tensor_copy(wb[w_][:, :, 0:nc_], wf[w_][:, :, 0:nc_]),
                     reads=[t_wf[w_]], writes=[t_wb[w_]])
            w_ = iw % 2
            iw += 1
            load_w(0, w_)
            for c0 in range(0, n_out, cb):
                nc_ = min(cb, n_out - c0)
                if c0 + cb < n_out:
                    load_w(c0 + cb, 1 - w_)
                if c0 < fm_cols:
                    assert c0 + nc_ <= fm_cols
                    for h0 in range(0, nc_, 128):
                        for g0 in range(0, nts * 128, 512):
                            gw = min(512, nts * 128 - g0)
                            p_ = ipo % 3
                            ipo += 1
                            for k in range(KC):
                                P.mm(po[p_][:, 0:gw], wb[w_][:, k, h0:h0 + 128], XT[:, k, g0:g0 + gw],
                                     start=(k == 0), stop=(k == KC - 1), reads=[t_wb[w_], t_XT], writes=[t_po[p_]])
                            P.copy("act" if p_ % 2 else "dve", ob[p_][:, 0:gw], po[p_][:, 0:gw],
                                   reads=[t_po[p_]], writes=[t_ob[p_]])
                            P.dma(fm_out[c0 + h0:c0 + h0 + 128, s0 * 128 + g0:s0 * 128 + g0 + gw], ob[p_][:, 0:gw],
                                  reads=[t_ob[p_]], writes=[t_out])
                else:
                    for i in range(nts):
                        p_ = ipo % 3
                        ipo += 1
                        for k in range(KC):
                            P.mm(po[p_][:, 0:nc_], XT[:, k, i * 128:(i + 1) * 128], wb[w_][:, k, 0:nc_],
                                 start=(k == 0), stop=(k == KC - 1), reads=[t_wb[w_], t_XT], writes=[t_po[p_]])
                        r0 = (s0 + i) * 128
                        if resid is not None:
                            rb_ = ipo % 2
                            P.dma(rb[rb_][:, 0:nc_], resid[r0:r0 + 128, c0 - fm_cols:c0 - fm_cols + nc_], reads=rd_res, writes=[t_rb[rb_]])
                            P.op("dve", lambda e, p_=p_, rb_=rb_, nc_=nc_: e.tensor_tensor(ob[p_][:, 0:nc_], po[p_][:, 0:nc_], rb[rb_][:, 0:nc_], ALU.add),
                                 reads=[t_po[p_], t_rb[rb_]], writes=[t_ob[p_]])
                        else:
                            P.copy("act" if p_ % 2 else "dve", ob[p_][:, 0:nc_], po[p_][:, 0:nc_],
                                   reads=[t_po[p_]], writes=[t_ob[p_]])
                        P.dma(tm_out[r0:r0 + 128, c0 - fm_cols:c0 - fm_cols + nc_], ob[p_][:, 0:nc_],
                              reads=[t_ob[p_]], writes=[t_out])
                w_ = 1 - w_
        P.flush()


def kv_outputs(P, C, PROJ, QKVT, ROPE, win_cache, outs, TP, t_A, KVR=None, KT=None, t_kvr=None):
    NTP = TP // 128
    t_o = outs["t"]
    with ExitStack() as st:
        kv = [P.sb(st, "kv", [128, 1536]) for _ in range(2)]
        t_kv = [Tk(), Tk()]
        rp = [P.sb(st, "rp", [128, 128]) for _ in range(2)]
        t_rp = [Tk(), Tk()]
        tmp = [P.sb(st, "tmp", [128, 4, 2, 64]) for _ in range(2)]
        t_tmp = [Tk(), Tk()]
        pkt = [P.ps(st, "pkt", [128, 4, 128]) for _ in range(2)]
        t_pkt = [Tk(1), Tk(1)]
        ktb = [P.sb(st, "ktb", [128, 4, 128]) for _ in range(2)]
        t_ktb = [Tk(), Tk()]
        for i in range(NTP + 1):
            b = i % 2
            r0 = i * 128
            P.dma(kv[b][:], PROJ[r0:r0 + 128, C_CK:C_CK + 1536], reads=[t_A], writes=[t_kv[b]])
            P.dma(rp[b][:], ROPE[r0:r0 + 128, :], writes=[t_rp[b]])
            cosb = rp[b][:, 0:64].unsqueeze(1).to_broadcast([128, 2, 64])
            sinb = rp[b][:, 64:128].unsqueeze(1).to_broadcast([128, 2, 64])
            for j in range(3):
                eng = "dve" if j != 1 else "pool"
                kk = kv[b][:, j * 512:j * 512 + 256].rearrange("p (h t d) -> p h t d", h=2, t=2)
                x1 = kk[:, :, 0, :]
                x2 = kk[:, :, 1, :]
                T = tmp[b]
                rd = [t_kv[b], t_rp[b]]
                P.op(eng, lambda e, T=T, x1=x1, cosb=cosb: e.tensor_tensor(T[:, 0], x1, cosb, ALU.mult), reads=rd, writes=[t_tmp[b]])
                P.op(eng, lambda e, T=T, x2=x2, sinb=sinb: e.tensor_tensor(T[:, 1], x2, sinb, ALU.mult), reads=rd, writes=[t_tmp[b]])
                P.op(eng, lambda e, T=T, x2=x2, cosb=cosb: e.tensor_tensor(T[:, 2], x2, cosb, ALU.mult), reads=rd, writes=[t_tmp[b]])
                P.op(eng, lambda e, T=T, x1=x1, sinb=sinb: e.tensor_tensor(T[:, 3], x1, sinb, ALU.mult), reads=rd, writes=[t_tmp[b]])
                P.op(eng, lambda e, T=T, x1=x1: e.tensor_tensor(x1, T[:, 0], T[:, 1], ALU.subtract), reads=[t_tmp[b]], writes=[t_kv[b]])
                P.op(eng, lambda e, T=T, x2=x2: e.tensor_tensor(x2, T[:, 2], T[:, 3], ALU.add), reads=[t_tmp[b]], writes=[t_kv[b]])
            if KVR is not None:
                P.dma(KVR[r0:r0 + 128, :], kv[b][:], reads=[t_kv[b]], writes=[t_kvr])
                for g_, cols in enumerate(((0, 128, 256, 384), (512, 640, 1024, 1152))):
                    for j, c_ in enumerate(cols):
                        P.tr(pkt[g_][:, j, :], kv[b][:, c_:c_ + 128], C.idf[:], reads=[t_kv[b], C.t_id], writes=[t_pkt[g_]])
                    P.copy("act", ktb[g_][:], pkt[g_][:], reads=[t_pkt[g_]], writes=[t_ktb[g_]])
                    P.dma(KT[g_ * 4:g_ * 4 + 4, :, r0:r0 + 128].rearrange("a d t -> d a t"), ktb[g_][:],
                          reads=[t_ktb[g_]], writes=[t_kvr])
            if i < NTP:
                P.dma(outs["cmp_p"][r0:r0 + 128, :], kv[b][:, 0:512], reads=[t_kv[b]], writes=[t_o])
                P.dma(outs["sel_p"][r0:r0 + 128, :], kv[b][:, 512:1024], reads=[t_kv[b]], writes=[t_o])
                wr = r0 - (TP - min(512, TP))
                if wr >= 0:
                    P.dma(outs["win_p"][wr:wr + 128, :], kv[b][:, 1024:1536], reads=[t_kv[b]], writes=[t_o])
            else:
                P.dma(outs["cmp_s"][0:4, :], kv[b][0:4, 0:512], reads=[t_kv[b]], writes=[t_o])
                P.dma(outs["sel_s"][0:4, :], kv[b][0:4, 512:1024], reads=[t_kv[b]], writes=[t_o])
                P.dma(outs["win_s"][508:512, :], kv[b][0:4, 1024:1536], reads=[t_kv[b]], writes=[t_o])
        P.dma(outs["win_s"][0:508, :], win_cache[4:512, :], writes=[t_o])
        for r in range(3):
            P.dma(outs["gconv_p"][r:r + 1, :], QKVT[:, TP - 3 + r:TP - 2 + r].rearrange("c o -> o c"),
                  reads=[t_A], writes=[t_o], allow_slow_non_contiguous=True)
            P.dma(outs["gconv_s"][r:r + 1, :], QKVT[:, TP + 1 + r:TP + 2 + r].rearrange("c o -> o c"),
                  reads=[t_A], writes=[t_o], allow_slow_non_contiguous=True)
        P.flush()


def host_nsa_consts(TP):
    i = np.arange(128)
    caus = (i[None, :] <= i[:, None]).astype(np.float32)
    n_cmp = (TP - 32) // 16 + 1
    ncp = -(-n_cmp // 128) * 128
    nsel = max(8, TP // 64)
    cs = np.arange(ncp)[:, None] * 16
    ss = np.arange(nsel)[None, :] * 64
    m = ((cs < ss + 64) & (cs + 32 > ss)).astype(np.float32)
    m[n_cmp:] = 0
    return caus, m


def host_consts():
    i = np.arange(128)
    same = (i[:, None] // 64) == (i[None, :] // 64)
    c = {}
    c["TRI"] = (same & (i[:, None] <= i[None, :])).astype(np.float32)
    c["EEND"] = (i[:, None] == (i[None, :] // 64) * 64 + 63).astype(np.float32)
    c["E0"] = np.repeat((i == 63)[:, None], 128, 1).astype(np.float32)
    c["E1"] = np.repeat((i == 127)[:, None], 128, 1).astype(np.float32)
    c["MADD"] = np.where(same & (i[:, None] >= i[None, :]), 0.0, 1e4).astype(np.float32)
    c["MSTR"] = (same & (i[:, None] > i[None, :])).astype(np.float32)
    c["ONES"] = np.ones((128, 128), np.float32)
    return np.concatenate([c[k] for k in ("TRI", "EEND", "E0", "E1", "MADD", "MSTR", "ONES")], axis=1)


def softplus(P, st, x, t_x, n, tag):
    a = P.sb(st, tag + "a", [128, n])
    t_a = Tk()
    P.act(a[:], x, AF.Abs, reads=[t_x], writes=[t_a])
    P.act(a[:], a[:], AF.Exp, reads=[t_a], writes=[t_a], scale=-1.0)
    P.act(a[:], a[:], AF.Ln, reads=[t_a], writes=[t_a], bias=1.0)
    P.op("dve", lambda e: e.tensor_scalar_max(x, x, 0.0), reads=[t_x], writes=[t_x])
    P.op("dve", lambda e: e.tensor_add(x, x, a[:]), reads=[t_x, t_a], writes=[t_x])


def gdn_stage(P, C, QKVT, PROJ, MIX, I, outs, TP, t_A, t_mix):
    NTP = TP // 128
    with ExitStack() as st:
        K_ = P.sb(st, "gconst", [128, 7, 128])
        t_K = Tk()
        P.dma(K_[:], I["GCONST"].rearrange("p (k n) -> p k n", k=7), writes=[t_K])
        TRI, EEND, E0, E1, MADD, MSTR, ONES = (K_[:, k, :] for k in range(7))
        cw = P.sb(st, "cw", [128, 8, 3, 4])
        P.dma(cw[:], I["gdn_cw"], writes=[t_K])
        alog = P.sb(st, "alog", [128, 8])
        dtb = P.sb(st, "dtb", [128, 8])
        gnw = P.sb(st, "gnw", [128, 128])
        P.dma(alog[:], I["gdn_a_log"].partition_broadcast(128), writes=[t_K])
        P.dma(dtb[:], I["gdn_dt_bias"].partition_broadcast(128), writes=[t_K])
        P.dma(gnw[:], I["gdn_norm_w"].partition_broadcast(128), writes=[t_K])
        P.act(alog[:], alog[:], AF.Exp, reads=[t_K], writes=[t_K])
        P.op("dve", lambda e: e.tensor_scalar_mul(alog[:], alog[:], -1.0), reads=[t_K], writes=[t_K])
        S = [P.sb(st, "S", [128, 8, 128]) for _ in range(2)]
        t_S = [[Tk() for _ in range(8)] for _ in range(2)]
        P.op("pool", lambda e: e.memset(S[0][:], 0.0), writes=t_S[0])
        qgm = [P.sb(st, "qgm", [128, 128]) for _ in range(2)]
        kdm = [P.sb(st, "kdm", [128, 128]) for _ in range(2)]
        t_qgm, t_kdm = [Tk(), Tk()], [Tk(), Tk()]
        for b in range(2):
            P.op("pool", lambda e, b=b: e.memset(qgm[b][:], 0.0), writes=[t_qgm[b]])
            P.op("pool", lambda e, b=b: e.memset(kdm[b][:], 0.0), writes=[t_kdm[b]])
        vnew = P.sb(st, "vnew", [128, 128])
        t_vn = Tk()
        P.op("pool", lambda e: e.memset(vnew[:], 0.0), writes=[t_vn])
        gab = P.sb(st, "gab", [128, 16]); t_gab = Tk()
        vm = P.sb(st, "vm", [128, 1]); t_vm = Tk()
        gz = P.sb(st, "gz", [128, 1024]); t_gz = Tk()
        beta = P.sb(st, "beta", [128, 8]); nbeta = P.sb(st, "nbeta", [128, 8]); t_beta = Tk()
        g = P.sb(st, "g", [128, 8]); t_g = Tk()
        gam = P.sb(st, "gam", [128, 8]); t_gam = Tk()
        sc = P.sb(st, "sc", [128, 5, 8]); t_sc = Tk()
        mixo = P.sb(st, "mixo", [128, 1024]); t_mixo = Tk()
        pg = P.ps(st, "pg", [128, 4, 8]); t_pg = Tk(1)
        xq = [P.sb(st, "xq", [128, 3, 131]) for _ in range(2)]; t_xq = [Tk(), Tk()]
        acc = P.sb(st, "acc", [128, 3, 128]); t_acc = Tk()
        tm = P.sb(st, "tm", [128, 3, 128]); t_tm = Tk()
        ssq = P.sb(st, "ssq", [128, 2]); t_ssq = Tk()
        junk = P.sb(st, "junk", [128, 128]); t_junk = Tk()
        qn = P.sb(st, "qn", [128, 128]); kn = P.sb(st, "kn", [128, 128]); t_qk = Tk()
        kbg = P.sb(st, "kbg", [128, 128]); vb = P.sb(st, "vb", [128, 128]); t_kv = Tk()
        fT = P.sb(st, "fT", [128, 4, 128]); t_fT = Tk()
        diag = P.sb(st, "diag", [128, 128]); t_diag = Tk()
        dm = P.sb(st, "dm", [128, 128]); t_dm = Tk()
        X = [P.sb(st, "X", [128, 2, 128]) for _ in range(2)]; t_X = [Tk(), Tk()]
        PT = P.sb(st, "PT", [128, 128]); t_PT = Tk()
        attn = P.sb(st, "attn", [128, 128]); attnT = P.sb(st, "attnT", [128, 128]); t_at = Tk(); t_atT = Tk()
        uw = P.sb(st, "uw", [128, 2, 128]); t_uw = Tk()
        o_sb = P.sb(st, "o_sb", [128, 128]); t_o = Tk()
        oss = P.sb(st, "oss", [128, 1]); t_oss = Tk()
        pA = P.ps(st, "pA", [128, 4, 128]); t_pA = Tk(1)
        pB = P.ps(st, "pB", [128, 4, 128]); t_pB = Tk(1)
        pC = P.ps(st, "pC", [128, 128]); t_pC = Tk(1)
        pD = P.ps(st, "pD", [128, 128]); t_pD = Tk(1)
        pE = P.ps(st, "pE", [128, 4, 128]); t_pE = Tk(1)
        pF = P.ps(st, "pF", [128, 2, 128]); t_pF = Tk(1)
        pS = P.ps(st, "pS", [128, 128]); t_pS = Tk(1)
        idf = C.idf
        cur = 0
        ih = 0
        for i in range(NTP + 1):
            r0 = i * 128
            sample = (i == NTP)
            if sample:
                P.dma(outs["gst_p"].rearrange("h k v -> k h v"), S[cur][:], reads=t_S[cur], writes=[outs["t"]])
                P.dma(S[cur][:], I["gdn_state"].rearrange("h k v -> k h v"), writes=t_S[cur])
            P.dma(gab[:], PROJ[r0:r0 + 128, C_GA:C_GA + 16], reads=[t_A], writes=[t_gab])
            P.dma(vm[:], I["VMASK"][r0:r0 + 128, :], writes=[t_vm])
            P.dma(gz[:], PROJ[r0:r0 + 128, C_GZ:C_GZ + 1024], reads=[t_A], writes=[t_gz])
            P.act(gz[:], gz[:], AF.Silu, reads=[t_gz], writes=[t_gz])
            P.act(beta[:], gab[:, 8:16], AF.Sigmoid, reads=[t_gab], writes=[t_beta])
            P.op("dve", lambda e: e.tensor_scalar(beta[:], beta[:], vm[:, 0:1], None, ALU.mult), reads=[t_beta, t_vm], writes=[t_beta])
            P.op("dve", lambda e: e.tensor_scalar_mul(nbeta[:], beta[:], -1.0), reads=[t_beta], writes=[t_beta])
            P.op("dve", lambda e: e.tensor_add(g[:], gab[:, 0:8], dtb[:]), reads=[t_gab, t_K], writes=[t_g])
            softplus(P, st, g[:], t_g, 8, "sp%d" % i)
            P.op("dve", lambda e: e.tensor_mul(g[:], g[:], alog[:]), reads=[t_g, t_K], writes=[t_g])
            P.op("dve", lambda e: e.tensor_scalar(g[:], g[:], vm[:, 0:1], None, ALU.mult), reads=[t_g, t_vm], writes=[t_g])
            P.mm(pg[:, 0, :], TRI, g[:], reads=[t_K, t_g], writes=[t_pg])
            P.copy("dve", gam[:], pg[:, 0, :], reads=[t_pg], writes=[t_gam])
            P.mm(pg[:, 1, :], EEND, gam[:], reads=[t_K, t_gam], writes=[t_pg])
            P.mm(pg[:, 2, :], E0, gam[:], reads=[t_K, t_gam], writes=[t_pg])
            P.mm(pg[:, 3, :], E1, gam[:], reads=[t_K, t_gam], writes=[t_pg])
            P.act(sc[:, 0, :], gam[:], AF.Exp, reads=[t_gam], writes=[t_sc])
            P.op("dve", lambda e: e.tensor_sub(sc[:, 1, :], pg[:, 1, :], gam[:]), reads=[t_pg, t_gam], writes=[t_sc])
            P.act(sc[:, 1, :], sc[:, 1, :], AF.Exp, reads=[t_sc], writes=[t_sc])
            P.op("dve", lambda e: e.tensor_mul(sc[:, 2, :], sc[:, 0, :], beta[:]), reads=[t_sc, t_beta], writes=[t_sc])
            P.act(sc[:, 3, :], pg[:, 2, :], AF.Exp, reads=[t_pg], writes=[t_sc])
            P.act(sc[:, 4, :], pg[:, 3, :], AF.Exp, reads=[t_pg], writes=[t_sc])
            for h in range(8):
                xb = ih % 2
                ih += 1
                src = QKVT.rearrange("(w c) t -> c w t", w=3)[h * 128:(h + 1) * 128]
                if i == 0:
                    P.op("pool", lambda e, xb=xb: e.memset(xq[xb][:, :, 0:3], 0.0), writes=[t_xq[xb]])
                    P.dma(xq[xb][:, :, 3:131], src[:, :, 0:128], reads=[t_A], writes=[t_xq[xb]])
                elif sample:
                    P.dma(xq[xb][:, :, 0:3], I["gdn_convT"].rearrange("(w c) t -> c w t", w=3)[h * 128:(h + 1) * 128],
                          writes=[t_xq[xb]])
                    P.dma(xq[xb][:, :, 3:131], src[:, :, r0:r0 + 128], reads=[t_A], writes=[t_xq[xb]])
                else:
                    P.dma(xq[xb][:], src[:, :, r0 - 3:r0 + 128], reads=[t_A], writes=[t_xq[xb]])
                for w in range(3):
                    eng = "dve"
                    for j in range(4):
                        if j == 0:
                            P.op(eng, lambda e, xb=xb, w=w, h=h: e.tensor_scalar(acc[:, w, :], xq[xb][:, w, 0:128], cw[:, h, w, 0:1], None, ALU.mult),
                                 reads=[t_xq[xb], t_K], writes=[t_acc])
                        else:
                            P.op(eng, lambda e, xb=xb, w=w, h=h, j=j: e.scalar_tensor_tensor(acc[:, w, :], xq[xb][:, w, j:j + 128], cw[:, h, w, j:j + 1], acc[:, w, :], ALU.mult, ALU.add),
                                 reads=[t_xq[xb], t_K, t_acc], writes=[t_acc])
                P.act(acc[:], acc[:], AF.Silu, reads=[t_acc], writes=[t_acc])
                for w in range(3):
                    P.tr(pA[:, w, :], acc[:, w, :], idf[:], reads=[t_acc, C.t_id], writes=[t_pA])
                P.copy("dve", tm[:], pA[:, 0:3, :], reads=[t_pA], writes=[t_tm])
                P.act(junk[:], tm[:, 0, :], AF.Square, reads=[t_tm], writes=[t_junk, t_ssq], accum_out=ssq[:, 0:1])
                P.act(junk[:], tm[:, 1, :], AF.Square, reads=[t_tm], writes=[t_junk, t_ssq], accum_out=ssq[:, 1:2])
                P.op("dve", lambda e: e.tensor_scalar_add(ssq[:], ssq[:], 1e-6), reads=[t_ssq], writes=[t_ssq])
                P.act(ssq[:], ssq[:], AF.Sqrt, reads=[t_ssq], writes=[t_ssq])
                P.op("dve", lambda e: e.reciprocal(ssq[:], ssq[:]), reads=[t_ssq], writes=[t_ssq])
                P.op("dve", lambda e: e.tensor_scalar(qn[:], tm[:, 0, :], ssq[:, 0:1], 128.0 ** -0.5, ALU.mult, ALU.mult), reads=[t_tm, t_ssq], writes=[t_qk])
                P.op("dve", lambda e: e.tensor_scalar(kn[:], tm[:, 1, :], ssq[:, 1:2], None, ALU.mult), reads=[t_tm, t_ssq], writes=[t_qk])
                P.op("pool", lambda e, h=h: e.tensor_scalar(kbg[:], kn[:], sc[:, 2, h:h + 1], None, ALU.mult), reads=[t_qk, t_sc], writes=[t_kv])
                P.op("pool", lambda e, h=h: e.tensor_scalar(vb[:], tm[:, 2, :], beta[:, h:h + 1], None, ALU.mult), reads=[t_tm, t_beta], writes=[t_kv])
                for a in range(2):
                    rs = slice(a * 64, a * 64 + 64)
                    P.op("dve", lambda e, a=a, rs=rs, h=h: e.tensor_scalar(qgm[a][rs, :], qn[rs, :], sc[rs, 0, h:h + 1], None, ALU.mult), reads=[t_qk, t_sc], writes=[t_qgm[a]])
                    P.op("pool", lambda e, a=a, rs=rs, h=h: e.tensor_scalar(kdm[a][rs, :], kn[rs, :], sc[rs, 1, h:h + 1], None, ALU.mult), reads=[t_qk, t_sc], writes=[t_kdm[a]])
                P.tr(pB[:, 0, :], kn[:], idf[:], reads=[t_qk, C.t_id], writes=[t_pB])
                P.tr(pB[:, 1, :], qn[:], idf[:], reads=[t_qk, C.t_id], writes=[t_pB])
                P.tr(pB[:, 2, :], qgm[0][:], idf[:], reads=[t_qgm[0], C.t_id], writes=[t_pB])
                P.tr(pB[:, 3, :], qgm[1][:], idf[:], reads=[t_qgm[1], C.t_id], writes=[t_pB])
                P.copy("act", fT[:], pB[:], reads=[t_pB], writes=[t_fT])
                kT, qT = fT[:, 0, :], fT[:, 1, :]
                P.op("dve", lambda e, h=h: e.tensor_scalar(diag[:], idf[:], gam[:, h:h + 1], None, ALU.mult), reads=[C.t_id, t_gam], writes=[t_diag])
                P.mm(pC[:], ONES, diag[:], start=True, stop=False, reads=[t_K, t_diag], writes=[t_pC])
                P.mm(pC[:], idf[:], MADD, start=False, stop=True, reads=[t_K, C.t_id], writes=[t_pC])
                P.act(dm[:], pC[:], AF.Exp, reads=[t_pC, t_gam], writes=[t_dm], scale=-1.0, bias=gam[:, h:h + 1])
                P.mm(pD[:], kT, kT, reads=[t_fT], writes=[t_pD])
                x0 = X[0]
                P.op("dve", lambda e: e.tensor_tensor(x0[:, 0, :], pD[:], dm[:], ALU.mult), reads=[t_pD, t_dm], writes=[t_X[0]])
                P.op("dve", lambda e, h=h: e.scalar_tensor_tensor(x0[:, 0, :], x0[:, 0, :], nbeta[:, h:h + 1], MSTR, ALU.mult, ALU.mult), reads=[t_X[0], t_beta, t_K], writes=[t_X[0]])
                P.mm(pC[:], qT, kT, reads=[t_fT], writes=[t_pC])
                P.op("dve", lambda e: e.tensor_tensor(attn[:], pC[:], dm[:], ALU.mult), reads=[t_pC, t_dm], writes=[t_at])
                P.tr(pE[:, 0, :], x0[:, 0, :], idf[:], reads=[t_X[0], C.t_id], writes=[t_pE])
                P.tr(pE[:, 1, :], attn[:], idf[:], reads=[t_at, C.t_id], writes=[t_pE])
                P.copy("act", x0[:, 1, :], pE[:, 0, :], reads=[t_pE], writes=[t_X[0]])
                P.copy("act", attnT[:], pE[:, 1, :], reads=[t_pE], writes=[t_atT])
                P.op("dve", lambda e: e.tensor_add(PT[:], pE[:, 0, :], idf[:]), reads=[t_pE, C.t_id], writes=[t_PT])
                xc = 0
                for step in range(5):
                    xa, xn_ = X[xc], X[1 - xc]
                    P.mm(pE[:, 2, :], xa[:, 1, :], xa[:, 0, :], reads=[t_X[xc]], writes=[t_pE])
                    if step < 4:
                        P.mm(pE[:, 3, :], xa[:, 0, :], xa[:, 1, :], reads=[t_X[xc]], writes=[t_pE])
                        P.copy("act", xn_[:], pE[:, 2:4, :], reads=[t_pE], writes=[t_X[1 - xc]])
                    else:
                        P.copy("act", xn_[:, 0, :], pE[:, 2, :], reads=[t_pE], writes=[t_X[1 - xc]])
                    P.mm(pD[:], xn_[:, 0, :], PT[:], reads=[t_X[1 - xc], t_PT], writes=[t_pD])
                    P.op("dve", lambda e: e.tensor_add(PT[:], pD[:], PT[:]), reads=[t_pD, t_PT], writes=[t_PT])
                    xc = 1 - xc
                P.mm(pF[:, 0, :], PT[:], vb[:], reads=[t_PT, t_kv], writes=[t_pF])
                P.mm(pF[:, 1, :], kbg[:], PT[:], reads=[t_PT, t_kv], writes=[t_pF])
                P.copy("act", uw[:], pF[:], reads=[t_pF], writes=[t_uw])
                u, wT = uw[:, 0, :], uw[:, 1, :]
                Sa, Sb = S[cur], S[1 - cur]
                tSa, tSb = t_S[cur][h], t_S[1 - cur][h]
                P.mm(pS[:], wT, Sa[:, h, :], reads=[t_uw, tSa], writes=[t_pS])
                P.op("dve", lambda e: e.tensor_sub(vnew[0:64, :], u[0:64, :], pS[0:64, :]), reads=[t_uw, t_pS], writes=[t_vn])
                P.mm(pD[:], kdm[0][:], vnew[:], reads=[t_kdm[0], t_vn], writes=[t_pD])
                P.op("dve", lambda e, h=h, Sa=Sa, Sb=Sb: e.scalar_tensor_tensor(Sb[:, h, :], Sa[:, h, :], sc[:, 3, h:h + 1], pD[:], ALU.mult, ALU.add),
                     reads=[tSa, t_sc, t_pD], writes=[tSb])
                P.mm(pS[:], wT, Sb[:, h, :], reads=[t_uw, tSb], writes=[t_pS])
                P.op("dve", lambda e: e.tensor_sub(vnew[64:128, :], u[64:128, :], pS[64:128, :]), reads=[t_uw, t_pS], writes=[t_vn])
                P.mm(pC[:], fT[:, 2, :], Sa[:, h, :], start=True, stop=False, reads=[t_fT, tSa], writes=[t_pC])
                P.mm(pC[:], fT[:, 3, :], Sb[:, h, :], start=False, stop=False, reads=[t_fT, tSb], writes=[t_pC])
                P.mm(pC[:], attnT[:], vnew[:], start=False, stop=True, reads=[t_atT, t_vn], writes=[t_pC])
                P.mm(pD[:], kdm[1][:], vnew[:], reads=[t_kdm[1], t_vn], writes=[t_pD])
                P.op("dve", lambda e, h=h, Sa=Sa, Sb=Sb: e.scalar_tensor_tensor(Sa[:, h, :], Sb[:, h, :], sc[:, 4, h:h + 1], pD[:], ALU.mult, ALU.add),
                     reads=[tSb, t_sc, t_pD], writes=[tSa])
                P.copy("act", o_sb[:], pC[:], reads=[t_pC], writes=[t_o])
                P.act(junk[:], o_sb[:], AF.Square, reads=[t_o], writes=[t_junk, t_oss], accum_out=oss[:])
                P.op("dve", lambda e: e.tensor_scalar(oss[:], oss[:], 1.0 / 128, EPS, ALU.mult, ALU.add), reads=[t_oss], writes=[t_oss])
                P.act(oss[:], oss[:], AF.Sqrt, reads=[t_oss], writes=[t_oss])
                P.op("dve", lambda e: e.reciprocal(oss[:], oss[:]), reads=[t_oss], writes=[t_oss])
                P.op("dve", lambda e: e.scalar_tensor_tensor(o_sb[:], o_sb[:], oss[:, 0:1], gnw[:], ALU.mult, ALU.mult), reads=[t_o, t_oss, t_K], writes=[t_o])
                P.op("dve", lambda e, h=h: e.tensor_mul(mixo[:, h * 128:(h + 1) * 128], o_sb[:], gz[:, h * 128:(h + 1) * 128]), reads=[t_o, t_gz], writes=[t_mixo])
            P.dma(MIX[r0:r0 + 128, 0:1024], mixo[:], reads=[t_mixo], writes=[t_mix])
        P.dma(outs["gst_s"].rearrange("h k v -> k h v"), S[cur][:], reads=t_S[cur], writes=[outs["t"]])
        P.flush()


def compress(P, C, CT, n_cmp, I, KCT, VC, t_ct, t_kc):
    with ExitStack() as st:
        w1 = P.sb(st, "w1", [128, 32, 256]); t_w1 = Tk()
        w2 = P.sb(st, "w2", [128, 2, 128]); t_w2 = Tk()
        pe = P.sb(st, "pe", [128, 32]); t_pe = Tk()
        rows = P.sb(st, "rows", [128, 4112]); t_rows = Tk()
        AT = P.sb(st, "AT", [128, 4112]); BT = P.sb(st, "BT", [128, 4112]); t_AB = Tk()
        hidT = P.sb(st, "hidT", [128, 2, 256]); t_hid = Tk()
        ph = [P.ps(st, "ph", [128, 256]) for _ in range(2)]; t_ph = [Tk(1), Tk(1)]
        pk = P.ps(st, "pk", [128, 256]); t_pk = Tk(1)
        P.op("pool", lambda e: e.memset(KCT[:], 0.0), writes=[t_kc])
        P.op("pool", lambda e: e.memset(VC[:], 0.0), writes=[t_kc])
        for s_ in range(2):
            P.dma(w1[:], I["cmp_w1"][s_].rearrange("(r d) h -> d r h", d=128), writes=[t_w1])
            P.dma(w2[:], I["cmp_w2"][s_].rearrange("(c h) e -> h c e", h=128), writes=[t_w2])
            P.dma(pe[:], I["cmp_peT"][s_], writes=[t_pe])
            for kvh in range(2):
                a = s_ * 2 + kvh
                for c0 in range(0, n_cmp, 256):
                    nb = min(256, n_cmp - c0)
                    ncol = 16 * nb + 16
                    P.dma(rows[:, 0:ncol], CT[a, :, 16 * c0:16 * c0 + ncol], reads=[t_ct], writes=[t_rows])
                    rv = rows[:, 0:ncol].rearrange("p (m r) -> p m r", r=16)
                    av = AT[:, 0:ncol].rearrange("p (m r) -> p m r", r=16)
                    bv = BT[:, 0:ncol].rearrange("p (m r) -> p m r", r=16)
                    plo = pe[:, 0:16].unsqueeze(1).to_broadcast([128, nb + 1, 16])
                    phi = pe[:, 16:32].unsqueeze(1).to_broadcast([128, nb + 1, 16])
                    P.op("dve", lambda e, av=av, rv=rv, plo=plo: e.tensor_tensor(av, rv, plo, ALU.add), reads=[t_rows, t_pe], writes=[t_AB])
                    P.op("pool", lambda e, bv=bv, rv=rv, phi=phi: e.tensor_tensor(bv, rv, phi, ALU.add), reads=[t_rows, t_pe], writes=[t_AB])
                    for hc in range(2):
                        for r in range(32):
                            src = AT if r < 16 else BT
                            P.mm(ph[hc][:, 0:nb], w1[:, r, hc * 128:(hc + 1) * 128], src[:, r:r + 16 * (nb - 1) + 1:16],
                                 start=(r == 0), stop=(r == 31), reads=[t_w1, t_AB], writes=[t_ph[hc]])
                        P.act(hidT[:, hc, 0:nb], ph[hc][:, 0:nb], AF.Silu, reads=[t_ph[hc]], writes=[t_hid])
                    if s_ == 0:
                        for hc in range(2):
                            P.mm(pk[:, 0:nb], w2[:, hc, :], hidT[:, hc, 0:nb], start=(hc == 0), stop=(hc == 1),
                                 reads=[t_w2, t_hid], writes=[t_pk])
                        P.copy("dve", KCT[:, kvh, c0:c0 + nb], pk[:, 0:nb], reads=[t_pk], writes=[t_kc])
                    else:
                        for sub in range(0, nb, 128):
                            ns = min(128, nb - sub)
                            for hc in range(2):
                                P.mm(pk[0:ns, 0:128], hidT[:, hc, sub:sub + ns], w2[:, hc, :], start=(hc == 0), stop=(hc == 1),
                                     reads=[t_w2, t_hid], writes=[t_pk])
                            P.copy("dve", VC[0:ns, (c0 + sub) // 128, kvh, :], pk[0:ns, 0:128], reads=[t_pk], writes=[t_kc])
        P.flush()


def nsa_qtiles(P, C, tiles, X, PROJ, ROPE, MIX, I, KCT, VC, t_A, t_kvr, t_kc, t_mix):
    ncp, nsel = X["ncp"], X["nsel"]
    ncc = ncp // 128
    nkmax = max(t["nk"] for t in tiles)
    with ExitStack() as st:
        msel = P.sb(st, "msel", [128, ncc, nsel]); t_c = Tk()
        P.dma(msel[:], X["msel"].rearrange("(c p) j -> p c j", p=128), writes=[t_c])
        caus = P.sb(st, "caus", [128, 128])
        P.dma(caus[:], I["CAUS"], writes=[t_c])
        qr = P.sb(st, "qr", [128, 8, 2, 64]); t_qr = Tk()
        rp = P.sb(st, "rp", [128, 128]); t_rp = Tk()
        tmp = P.sb(st, "tmpq", [128, 4, 8, 64]); t_tmp = Tk()
        qT = P.sb(st, "qT", [128, 8, 128]); t_qT = Tk()
        gts = P.sb(st, "gts", [128, 24]); t_gts = Tk()
        O = P.sb(st, "Oall", [128, 8, 128]); t_O = Tk()
        sc = P.sb(st, "sc", [128, ncp]); t_sc = Tk()
        st4 = P.sb(st, "st4", [128, 4]); t_st = Tk()
        pcT = P.sb(st, "pcT", [128, ncc, 128]); t_pcT = Tk()
        score = P.sb(st, "score", [128, nsel]); work = P.sb(st, "work", [128, nsel]); t_score = Tk()
        bonus = P.sb(st, "bonus", [128, nsel]); t_bon = Tk()
        m8 = P.sb(st, "m8", [128, 16]); t_m8 = Tk()
        mk = P.sb(st, "mk", [128, nsel]); t_mk = Tk()
        S = P.sb(st, "Sall", [128, nkmax]); t_S = Tk()
        ktc = [P.sb(st, "ktc", [128, 2048]) for _ in range(2)]; t_ktc = [Tk(), Tk()]
        vch = [P.sb(st, "vch", [128, 16, 128]) for _ in range(2)]; t_vch = [Tk(), Tk()]
        pT = [P.sb(st, "pT", [128, 4, 128]) for _ in range(2)]; t_pT = [Tk(), Tk()]
        wS = P.sb(st, "wS", [128, 640]); t_wS = Tk()
        wkt = P.sb(st, "wkt", [128, 640]); t_wkt = Tk()
        wv = P.sb(st, "wv", [128, 5, 128]); t_wv = Tk()
        psc = [P.ps(st, "psc", [128, 512]) for _ in range(2)]; t_psc = [Tk(1), Tk(1)]
        ptr = [P.ps(st, "ptrn", [128, 4, 128]) for _ in range(2)]; t_ptr = [Tk(1), Tk(1)]
        po = P.ps(st, "pon", [128, 128]); t_po = Tk(1)
        pimp = P.ps(st, "pimp", [128, nsel]); t_pimp = Tk(1)
        idf = C.idf
        ik = 0; iv = 0; ipt = 0; ips = 0
        for T in tiles:
            r0, qpos0, nk = T["r0"], T["qpos0"], T["nk"]
            nblk = nk // 64
            P.dma(qr[:], PROJ[r0:r0 + 128, C_NQ:C_NQ + 1024].rearrange("p (h t d) -> p h t d", h=8, t=2), reads=[t_A], writes=[t_qr])
            P.dma(rp[:], ROPE[r0:r0 + 128, :], writes=[t_rp])
            P.dma(gts[:], PROJ[r0:r0 + 128, C_NG:C_NG + 24], reads=[t_A], writes=[t_gts])
            P.act(gts[:], gts[:], AF.Sigmoid, reads=[t_gts], writes=[t_gts])
            cosb = rp[:, 0:64].unsqueeze(1).to_broadcast([128, 8, 64])
            sinb = rp[:, 64:128].unsqueeze(1).to_broadcast([128, 8, 64])
            x1, x2 = qr[:, :, 0, :], qr[:, :, 1, :]
            rd = [t_qr, t_rp]
            P.op("dve", lambda e: e.tensor_tensor(tmp[:, 0], x1, cosb, ALU.mult), reads=rd, writes=[t_tmp])
            P.op("pool", lambda e: e.tensor_tensor(tmp[:, 1], x2, sinb, ALU.mult), reads=rd, writes=[t_tmp])
            P.op("dve", lambda e: e.tensor_tensor(tmp[:, 2], x2, cosb, ALU.mult), reads=rd, writes=[t_tmp])
            P.op("pool", lambda e: e.tensor_tensor(tmp[:, 3], x1, sinb, ALU.mult), reads=rd, writes=[t_tmp])
            P.op("dve", lambda e: e.tensor_tensor(x1, tmp[:, 0], tmp[:, 1], ALU.subtract), reads=[t_tmp], writes=[t_qr])
            P.op("dve", lambda e: e.tensor_tensor(x2, tmp[:, 2], tmp[:, 3], ALU.add), reads=[t_tmp], writes=[t_qr])
            qf = qr[:].rearrange("p h t d -> p h (t d)")
            for g_ in range(2):
                for j in range(4):
                    P.tr(ptr[g_][:, j, :], qf[:, g_ * 4 + j, :], idf[:], reads=[t_qr, C.t_id], writes=[t_ptr[g_]])
                P.act(qT[:, g_ * 4:g_ * 4 + 4, :], ptr[g_][:], AF.Copy, reads=[t_ptr[g_]], writes=[t_qT], scale=128.0 ** -0.5)
            P.op("pool", lambda e: e.memset(bonus[:], 1e4), writes=[t_bon])
            P.op("pool", lambda e, qpos0=qpos0: e.affine_select(bonus[:], bonus[:], [[-64, nsel]], ALU.is_ge, fillreg(e, 0.0), base=qpos0, channel_multiplier=1), reads=[t_bon], writes=[t_bon])
            P.op("pool", lambda e, qpos0=qpos0: e.affine_select(bonus[:], bonus[:], [[64, nsel]], ALU.is_ge, fillreg(e, 0.0), base=127 - qpos0, channel_multiplier=-1), reads=[t_bon], writes=[t_bon])
            P.op("pool", lambda e: e.memset(bonus[:, 0:1], 1e4), reads=[t_bon], writes=[t_bon])
            for kvh in range(2):
                for g in range(4):
                    h = kvh * 4 + g
                    for c0 in range(0, ncp, 512):
                        cw_ = min(512, ncp - c0)
                        p_ = ips % 2; ips += 1
                        P.mm(psc[p_][:, 0:cw_], qT[:, h, :], KCT[:, kvh, c0:c0 + cw_], reads=[t_qT, t_kc], writes=[t_psc[p_]])
                        P.copy("act", sc[:, c0:c0 + cw_], psc[p_][:, 0:cw_], reads=[t_psc[p_]], writes=[t_sc])
                    P.op("pool", lambda e, qpos0=qpos0: e.affine_select(sc[:], sc[:], [[-16, ncp]], ALU.is_ge, fillreg(e, -1e30), base=qpos0 - 31, channel_multiplier=1), reads=[t_sc], writes=[t_sc])
                    P.op("dve", lambda e: e.reduce_max(st4[:, 0:1], sc[:], AX.X), reads=[t_sc], writes=[t_st])
                    P.op("dve", lambda e: e.tensor_scalar(st4[:, 0:1], st4[:, 0:1], -1e20, -1.0, ALU.max, ALU.mult), reads=[t_st], writes=[t_st])
                    P.act(sc[:], sc[:], AF.Exp, reads=[t_sc, t_st], writes=[t_sc, t_st], bias=st4[:, 0:1], accum_out=st4[:, 1:2])
                    P.op("dve", lambda e: e.tensor_scalar_max(st4[:, 1:2], st4[:, 1:2], 1e-30), reads=[t_st], writes=[t_st])
                    P.op("dve", lambda e: e.reciprocal(st4[:, 2:3], st4[:, 1:2]), reads=[t_st], writes=[t_st])
                    P.op("dve", lambda e: e.tensor_scalar(sc[:], sc[:], st4[:, 2:3], None, ALU.mult), reads=[t_sc, t_st], writes=[t_sc])
                    for c4 in range(0, ncc, 4):
                        n4 = min(4, ncc - c4)
                        p_ = ipt % 2; ipt += 1
                        for j in range(n4):
                            P.tr(ptr[p_][:, j, :], sc[:, (c4 + j) * 128:(c4 + j + 1) * 128], idf[:], reads=[t_sc, C.t_id], writes=[t_ptr[p_]])
                        P.copy("act", pcT[:, c4:c4 + n4, :], ptr[p_][:, 0:n4, :], reads=[t_ptr[p_]], writes=[t_pcT])
                    for ch in range(ncc):
                        P.mm(po[:], pcT[:, ch, :], VC[:, ch, kvh, :], start=(ch == 0), stop=(ch == ncc - 1), reads=[t_pcT, t_kc], writes=[t_po])
                    P.op("dve", lambda e, h=h: e.tensor_scalar(O[:, h, :], po[:], gts[:, h:h + 1], None, ALU.mult), reads=[t_po, t_gts], writes=[t_O])
                    for ch in range(ncc):
                        P.mm(pimp[:], pcT[:, ch, :], msel[:, ch, :], start=(g == 0 and ch == 0), stop=(g == 3 and ch == ncc - 1),
                             reads=[t_pcT, t_c], writes=[t_pimp])
                P.op("dve", lambda e: e.tensor_tensor(score[:], pimp[:], bonus[:], ALU.add), reads=[t_pimp, t_bon], writes=[t_score])
                P.op("pool", lambda e, qpos0=qpos0: e.affine_select(score[:], score[:], [[-64, nsel]], ALU.is_ge, fillreg(e, -1e30), base=qpos0, channel_multiplier=1), reads=[t_score], writes=[t_score])
                P.op("dve", lambda e: e.max(m8[:, 0:8], score[:]), reads=[t_score], writes=[t_m8])
                P.op("dve", lambda e: e.match_replace(work[:], m8[:, 0:8], score[:], -1e30), reads=[t_score, t_m8], writes=[t_score])
                P.op("dve", lambda e: e.max(m8[:, 8:16], work[:]), reads=[t_score], writes=[t_m8])
                P.op("dve", lambda e: e.tensor_scalar(mk[:], score[:], m8[:, 15:16], None, ALU.is_ge), reads=[t_score, t_m8], writes=[t_mk])
                P.op("dve", lambda e: e.tensor_scalar(work[:], score[:], -1e29, None, ALU.is_gt), reads=[t_score], writes=[t_score])
                P.op("dve", lambda e: e.tensor_tensor(mk[:], mk[:], work[:], ALU.mult), reads=[t_score, t_mk], writes=[t_mk])
                for g in range(4):
                    h = kvh * 4 + g
                    for k0 in range(0, nk, 2048):
                        kw_ = min(2048, nk - k0)
                        kb_ = ik % 2; ik += 1
                        P.dma(ktc[kb_][:, 0:kw_], X["SKT"][kvh, :, k0:k0 + kw_], reads=[t_kvr], writes=[t_ktc[kb_]])
                        for c0 in range(0, kw_, 512):
                            cw_ = min(512, kw_ - c0)
                            p_ = ips % 2; ips += 1
                            P.mm(psc[p_][:, 0:cw_], qT[:, h, :], ktc[kb_][:, c0:c0 + cw_], reads=[t_qT, t_ktc[kb_]], writes=[t_psc[p_]])
                            P.copy("act", S[:, k0 + c0:k0 + c0 + cw_], psc[p_][:, 0:cw_], reads=[t_psc[p_]], writes=[t_S])
                    Sv = S[:, 0:nk]
                    P.op("dve", lambda e, Sv=Sv: e.reduce_max(st4[:, 0:1], Sv, AX.X), reads=[t_S], writes=[t_st])
                    P.op("dve", lambda e: e.tensor_scalar_mul(st4[:, 0:1], st4[:, 0:1], -1.0), reads=[t_st], writes=[t_st])
                    P.act(Sv, Sv, AF.Exp, reads=[t_S, t_st], writes=[t_S], bias=st4[:, 0:1])
                    Sb = Sv.rearrange("p (j k) -> p j k", k=64)
                    mb = mk[:, 0:nblk].unsqueeze(2).to_broadcast([128, nblk, 64])
                    P.op("dve", lambda e, Sb=Sb, mb=mb: e.tensor_tensor(Sb, Sb, mb, ALU.mult), reads=[t_S, t_mk], writes=[t_S])
                    Sd = S[:, nk - 128:nk]
                    P.op("dve", lambda e, Sd=Sd: e.tensor_tensor(Sd, Sd, caus[:], ALU.mult), reads=[t_S, t_c], writes=[t_S])
                    P.op("dve", lambda e, Sv=Sv: e.reduce_sum(st4[:, 1:2], Sv, AX.X), reads=[t_S], writes=[t_st])
                    P.op("dve", lambda e: e.tensor_scalar_max(st4[:, 1:2], st4[:, 1:2], 1e-30), reads=[t_st], writes=[t_st])
                    P.op("dve", lambda e: e.reciprocal(st4[:, 2:3], st4[:, 1:2]), reads=[t_st], writes=[t_st])
                    P.op("dve", lambda e, h=h: e.tensor_tensor(st4[:, 3:4], st4[:, 2:3], gts[:, 8 + h:9 + h], ALU.mult), reads=[t_st, t_gts], writes=[t_st])
                    nch = nk // 128
                    for v0 in range(0, nch, 16):
                        nv = min(16, nch - v0)
                        vb_ = iv % 2; iv += 1
                        P.dma(vch[vb_][:, 0:nv, :], X["SELR"][v0 * 128:(v0 + nv) * 128, 256 + kvh * 128:384 + kvh * 128].rearrange("(c p) d -> p c d", p=128),
                              reads=[t_kvr], writes=[t_vch[vb_]])
                        for c4 in range(0, nv, 4):
                            n4 = min(4, nv - c4)
                            p_ = ipt % 2; ipt += 1
                            for j in range(n4):
                                cc = v0 + c4 + j
                                P.tr(ptr[p_][:, j, :], S[:, cc * 128:(cc + 1) * 128], idf[:], reads=[t_S, C.t_id], writes=[t_ptr[p_]])
                            P.copy("act", pT[p_][:, 0:n4, :], ptr[p_][:, 0:n4, :], reads=[t_ptr[p_]], writes=[t_pT[p_]])
                            for j in range(n4):
                                cc = v0 + c4 + j
                                P.mm(po[:], pT[p_][:, j, :], vch[vb_][:, c4 + j, :], start=(cc == 0), stop=(cc == nch - 1),
                                     reads=[t_pT[p_], t_vch[vb_]], writes=[t_po])
                    P.op("dve", lambda e, h=h: e.scalar_tensor_tensor(O[:, h, :], po[:], st4[:, 3:4], O[:, h, :], ALU.mult, ALU.add), reads=[t_po, t_st, t_O], writes=[t_O])
                w0, nw, wpos0 = T["w0"], T["nw"], T["wpos0"]
                P.dma(wkt[:, 0:nw], X["WKT"][kvh, :, w0:w0 + nw], reads=[t_kvr], writes=[t_wkt])
                nwc = nw // 128
                P.dma(wv[:, 0:nwc, :], X["WINR"][w0:w0 + nw, 256 + kvh * 128:384 + kvh * 128].rearrange("(c p) d -> p c d", p=128),
                      reads=[t_kvr], writes=[t_wv])
                for g in range(4):
                    h = kvh * 4 + g
                    for c0 in range(0, nw, 512):
                        cw_ = min(512, nw - c0)
                        p_ = ips % 2; ips += 1
                        P.mm(psc[p_][:, 0:cw_], qT[:, h, :], wkt[:, c0:c0 + cw_], reads=[t_qT, t_wkt], writes=[t_psc[p_]])
                        P.copy("act", wS[:, c0:c0 + cw_], psc[p_][:, 0:cw_], reads=[t_psc[p_]], writes=[t_wS])
                    Wv = wS[:, 0:nw]
                    d0 = qpos0 - wpos0 - w0
                    P.op("pool", lambda e, Wv=Wv, d0=d0, nw=nw: e.affine_select(Wv, Wv, [[-1, nw]], ALU.is_ge, fillreg(e, -1e30), base=d0, channel_multiplier=1), reads=[t_wS], writes=[t_wS])
                    if d0 + 127 > 511:
                        P.op("pool", lambda e, Wv=Wv, d0=d0, nw=nw: e.affine_select(Wv, Wv, [[1, nw]], ALU.is_ge, fillreg(e, -1e30), base=511 - d0, channel_multiplier=-1), reads=[t_wS], writes=[t_wS])
                    P.op("dve", lambda e, Wv=Wv: e.reduce_max(st4[:, 0:1], Wv, AX.X), reads=[t_wS], writes=[t_st])
                    P.op("dve", lambda e: e.tensor_scalar(st4[:, 0:1], st4[:, 0:1], -1e20, -1.0, ALU.max, ALU.mult), reads=[t_st], writes=[t_st])
                    P.act(Wv, Wv, AF.Exp, reads=[t_wS, t_st], writes=[t_wS, t_st], bias=st4[:, 0:1], accum_out=st4[:, 1:2])
                    P.op("dve", lambda e: e.tensor_scalar_max(st4[:, 1:2], st4[:, 1:2], 1e-30), reads=[t_st], writes=[t_st])
                    P.op("dve", lambda e: e.reciprocal(st4[:, 2:3], st4[:, 1:2]), reads=[t_st], writes=[t_st])
                    P.op("dve", lambda e, h=h: e.tensor_tensor(st4[:, 3:4], st4[:, 2:3], gts[:, 16 + h:17 + h], ALU.mult), reads=[t_st, t_gts], writes=[t_st])
                    for c4 in range(0, nwc, 4):
                        n4 = min(4, nwc - c4)
                        p_ = ipt % 2; ipt += 1
                        for j in range(n4):
                            P.tr(ptr[p_][:, j, :], wS[:, (c4 + j) * 128:(c4 + j + 1) * 128], idf[:], reads=[t_wS, C.t_id], writes=[t_ptr[p_]])
                        P.copy("act", pT[p_][:, 0:n4, :], ptr[p_][:, 0:n4, :], reads=[t_ptr[p_]], writes=[t_pT[p_]])
                        for j in range(n4):
                            P.mm(po[:], pT[p_][:, j, :], wv[:, c4 + j, :], start=(c4 + j == 0), stop=(c4 + j == nwc - 1),
                                 reads=[t_pT[p_], t_wv], writes=[t_po])
                    P.op("dve", lambda e, h=h: e.scalar_tensor_tensor(O[:, h, :], po[:], st4[:, 3:4], O[:, h, :], ALU.mult, ALU.add), reads=[t_po, t_st, t_O], writes=[t_O])
            P.dma(MIX[r0:r0 + 128, 1024:2048], O[:].rearrange("p h d -> p (h d)"), reads=[t_O], writes=[t_mix])
        P.flush()


def swiglu_ffn(P, C, x_ap, gamma_ap, wg, wu, wd, Dff, ntiles, resid, out_ap, t_in, t_out, rowscale=None, sbt=4,
               t_rs=None):
    Dm = x_ap.shape[1]
    KC = Dm // 128
    FC = Dff // 128
    NOC = Dm // 128
    rd_in = [t_in] if t_in is not None else []
    with ExitStack() as st:
        XT = P.sb(st, "fXT", [128, KC, sbt * 128], BF16); t_XT = Tk()
        AT = P.sb(st, "fAT", [128, FC, sbt * 128], BF16); t_AT = Tk()
        gt = P.sb(st, "fgt", [128, Dm]); t_g = Tk()
        P.dma(gt[:], gamma_ap.partition_broadcast(128), writes=[t_g])
        xt = P.sb(st, "fxt", [128, Dm]); t_x = Tk()
        sq = P.sb(st, "fsq", [128, Dm], BF16); t_sq = Tk()
        ss = P.sb(st, "fss", [128, 1]); t_ss = Tk()
        xn = P.sb(st, "fxn", [128, Dm], BF16); t_xn = Tk()
        ptr = [P.ps(st, "fptr", [128, 4, 128], BF16) for _ in range(2)]; t_ptr = [Tk(1), Tk(1)]
        wgs = P.sb(st, "wgs", [128, KC, 128]); wus = P.sb(st, "wus", [128, KC, 128]); t_wgs = Tk(); t_wus = Tk()
        wgb = [P.sb(st, "wgb", [128, KC, 128], BF16) for _ in range(2)]; t_wgb = [Tk(), Tk()]
        wub = [P.sb(st, "wub", [128, KC, 128], BF16) for _ in range(2)]; t_wub = [Tk(), Tk()]
        wds = P.sb(st, "wds", [128, FC, 128]); t_wds = Tk()
        wdb = [P.sb(st, "wdb", [128, FC, 128], BF16) for _ in range(2)]; t_wdb = [Tk(), Tk()]
        pg = [P.ps(st, "fpg", [128, 512]) for _ in range(2)]; t_pg = [Tk(1), Tk(1)]
        pu = [P.ps(st, "fpu", [128, 512]) for _ in range(2)]; t_pu = [Tk(1), Tk(1)]
        pd = P.ps(st, "fpd", [128, 512]); t_pd = Tk(1)
        pt2 = P.ps(st, "fpt2", [128, 4, 128]); t_pt2 = Tk(1)
        sg = [P.sb(st, "fsg", [128, 512]) for _ in range(2)]; t_sg = [Tk(), Tk()]
        oT = P.sb(st, "foT", [128, 512]); t_oT = Tk()
        rb = [P.sb(st, "frb", [128, sbt, 128]) for _ in range(2)]; t_rb = [Tk(), Tk()]
        ob = [P.sb(st, "fob", [128, sbt, 128]) for _ in range(2)]; t_ob = [Tk(), Tk()]
        rs = P.sb(st, "frs", [128, sbt]); t_rsb = Tk()
        iw = 0
        io = 0
        for s0 in range(0, ntiles, sbt):
            nts = min(sbt, ntiles - s0)
            ntok = nts * 128
            for i in range(nts):
                r0 = (s0 + i) * 128
                P.dma(xt[:], x_ap[r0:r0 + 128, :], reads=rd_in, writes=[t_x])
                P.act(sq[:], xt[:], AF.Square, reads=[t_x], writes=[t_sq, t_ss], accum_out=ss[:])
                P.op("dve", lambda e: e.tensor_scalar(ss[:], ss[:], 1.0 / Dm, EPS, ALU.mult, ALU.add), reads=[t_ss], writes=[t_ss])
                P.act(ss[:], ss[:], AF.Sqrt, reads=[t_ss], writes=[t_ss])
                P.op("dve", lambda e: e.reciprocal(ss[:], ss[:]), reads=[t_ss], writes=[t_ss])
                P.op("dve", lambda e: e.scalar_tensor_tensor(xn[:], xt[:], ss[:, 0:1], gt[:], ALU.mult, ALU.mult),
                     reads=[t_x, t_ss, t_g], writes=[t_xn])
                for kg in range(KC // 4):
                    pb = kg % 2
                    for j in range(4):
                        k = kg * 4 + j
                        P.tr(ptr[pb][:, j, :], xn[:, k * 128:(k + 1) * 128], C.idb[:], reads=[t_xn, C.t_id], writes=[t_ptr[pb]])
                    P.copy("dve" if kg % 2 == 0 else "act", XT[:, kg * 4:kg * 4 + 4, i * 128:(i + 1) * 128], ptr[pb][:],
                           reads=[t_ptr[pb]], writes=[t_XT])
            if rowscale is not None:
                P.dma(rs[:, 0:nts], rowscale[s0 * 128:s0 * 128 + ntok, :].rearrange("(t p) o -> p (t o)", p=128),
                      reads=[t_rs] if t_rs is not None else [], writes=[t_rsb], allow_slow_non_contiguous=True)
            for fc in range(FC):
                b = iw % 2; iw += 1
                P.dma(wgs[:], wg[:, fc * 128:(fc + 1) * 128].rearrange("(k p) n -> p k n", p=128), writes=[t_wgs])
                P.dma(wus[:], wu[:, fc * 128:(fc + 1) * 128].rearrange("(k p) n -> p k n", p=128), writes=[t_wus])
                P.op("pool", lambda e, b=b: e.tensor_copy(wgb[b][:], wgs[:]), reads=[t_wgs], writes=[t_wgb[b]])
                P.op("pool", lambda e, b=b: e.tensor_copy(wub[b][:], wus[:]), reads=[t_wus], writes=[t_wub[b]])
                for k in range(KC):
                    P.mm(pg[b][:, 0:ntok], wgb[b][:, k, :], XT[:, k, 0:ntok], start=(k == 0), stop=(k == KC - 1),
                         reads=[t_wgb[b], t_XT], writes=[t_pg[b]])
                for k in range(KC):
                    P.mm(pu[b][:, 0:ntok], wub[b][:, k, :], XT[:, k, 0:ntok], start=(k == 0), stop=(k == KC - 1),
                         reads=[t_wub[b], t_XT], writes=[t_pu[b]])
                P.act(sg[b][:, 0:ntok], pg[b][:, 0:ntok], AF.Silu, reads=[t_pg[b]], writes=[t_sg[b]])
                P.op("dve", lambda e, b=b, fc=fc, ntok=ntok: e.tensor_tensor(AT[:, fc, 0:ntok], sg[b][:, 0:ntok], pu[b][:, 0:ntok], ALU.mult),
                     reads=[t_sg[b], t_pu[b]], writes=[t_AT])
            def load_wd(cb, b):
                P.dma(wds[:], wd[:, cb * 128:(cb + 1) * 128].rearrange("(c p) n -> p c n", p=128), writes=[t_wds])
                P.op("pool", lambda e, b=b: e.tensor_copy(wdb[b][:], wds[:]), reads=[t_wds], writes=[t_wdb[b]])
            b = iw % 2; iw += 1
            load_wd(0, b)
            for cb in range(NOC):
                if cb + 1 < NOC:
                    load_wd(cb + 1, 1 - b)
                for kc in range(FC):
                    P.mm(pd[:, 0:ntok], wdb[b][:, kc, :], AT[:, kc, 0:ntok], start=(kc == 0), stop=(kc == FC - 1),
                         reads=[t_wdb[b], t_AT], writes=[t_pd])
                P.copy("act", oT[:, 0:ntok], pd[:, 0:ntok], reads=[t_pd], writes=[t_oT])
                for i in range(nts):
                    P.tr(pt2[:, i, :], oT[:, i * 128:(i + 1) * 128], C.idf[:], reads=[t_oT, C.t_id], writes=[t_pt2])
                o_ = io % 2; io += 1
                dst = out_ap[s0 * 128:s0 * 128 + ntok, cb * 128:(cb + 1) * 128].rearrange("(t p) n -> p t n", p=128)
                if resid is not None:
                    src = resid[s0 * 128:s0 * 128 + ntok, cb * 128:(cb + 1) * 128].rearrange("(t p) n -> p t n", p=128)
                    P.dma(rb[o_][:, 0:nts, :], src, reads=rd_in, writes=[t_rb[o_]])
                if rowscale is None:
                    if resid is not None:
                        P.op("dve", lambda e, o_=o_, nts=nts: e.tensor_tensor(ob[o_][:, 0:nts, :], pt2[:, 0:nts, :], rb[o_][:, 0:nts, :], ALU.add),
                             reads=[t_pt2, t_rb[o_]], writes=[t_ob[o_]])
                    else:
                        P.copy("dve", ob[o_][:, 0:nts, :], pt2[:, 0:nts, :], reads=[t_pt2], writes=[t_ob[o_]])
                else:
                    for i in range(nts):
                        if resid is not None:
                            P.op("dve", lambda e, o_=o_, i=i: e.scalar_tensor_tensor(ob[o_][:, i, :], pt2[:, i, :], rs[:, i:i + 1], rb[o_][:, i, :], ALU.mult, ALU.add),
                                 reads=[t_pt2, t_rb[o_], t_rsb], writes=[t_ob[o_]])
                        else:
                            P.op("dve", lambda e, o_=o_, i=i: e.tensor_scalar(ob[o_][:, i, :], pt2[:, i, :], rs[:, i:i + 1], None, ALU.mult),
                                 reads=[t_pt2, t_rsb], writes=[t_ob[o_]])
                P.dma(dst, ob[o_][:, 0:nts, :], reads=[t_ob[o_]], writes=[t_out])
                b = 1 - b
        P.flush()


def host_ssd_consts():
    i = np.arange(128)
    tri = (i[:, None] <= i[None, :]).astype(np.float32)
    e127 = np.repeat((i == 127)[:, None], 128, 1).astype(np.float32)
    maddT = np.where(i[None, :] >= i[:, None], 0.0, -1e4).astype(np.float32)
    ones = np.ones((128, 128), np.float32)
    return np.concatenate([tri, e127, maddT, ones], axis=1)


def ssd_stage(P, C, XBCT, ZDT, Y, I, outs, TP, t_in, t_y):
    NTP = TP // 128
    with ExitStack() as st:
        K_ = P.sb(st, "sconst", [128, 4, 128]); t_K = Tk()
        P.dma(K_[:], I["SCONST"].rearrange("p (k n) -> p k n", k=4), writes=[t_K])
        TRI, E127, MADDT, ONES = (K_[:, k, :] for k in range(4))
        cw = P.sb(st, "scw", [128, 48, 5])
        P.dma(cw[:], I["ssm_cw"], writes=[t_K])
        alog = P.sb(st, "salog", [128, 64]); dtb = P.sb(st, "sdtb", [128, 64]); dsk = P.sb(st, "sdsk", [128, 64])
        nw = P.sb(st, "snw", [128, 4096])
        P.dma(alog[:], I["ssm_a_log"].partition_broadcast(128), writes=[t_K])
        P.dma(dtb[:], I["ssm_dt_bias"].partition_broadcast(128), writes=[t_K])
        P.dma(dsk[:], I["ssm_d_skip"].partition_broadcast(128), writes=[t_K])
        P.dma(nw[:], I["ssm_norm_w"].partition_broadcast(128), writes=[t_K])
        P.act(alog[:], alog[:], AF.Exp, reads=[t_K], writes=[t_K])
        P.op("dve", lambda e: e.tensor_scalar_mul(alog[:], alog[:], -1.0), reads=[t_K], writes=[t_K])
        hT = P.sb(st, "hT", [128, 64, 64]); t_h = [Tk() for _ in range(64)]
        P.op("pool", lambda e: e.memset(hT[:], 0.0), writes=t_h)
        hio = P.sb(st, "hio", [64, 64, 128]); t_hio = Tk()
        xq = [P.sb(st, "sxq", [128, 131]) for _ in range(3)]; t_xq = [Tk(), Tk(), Tk()]
        acc = [P.sb(st, "sacc", [128, 128]) for _ in range(2)]; t_acc = [Tk(), Tk()]
        xtm = P.sb(st, "xtm", [128, 64, 64]); t_xtm = Tk()
        xdt = P.sb(st, "xdt", [128, 64, 64]); xdec = P.sb(st, "xdec", [128, 64, 64]); t_xd = Tk()
        Btm = P.sb(st, "Btm", [128, 8, 128]); t_B = Tk()
        CT = P.sb(st, "CTs", [128, 8, 128]); BTf = P.sb(st, "BTf", [128, 8, 128]); t_CT = Tk()
        cbT = P.sb(st, "cbT", [128, 8, 128]); t_cb = Tk()
        zd = P.sb(st, "zd", [128, 4160]); t_zd = Tk()
        vm = P.sb(st, "svm", [128, 1]); t_vm = Tk()
        dt = P.sb(st, "sdt", [128, 64]); t_dt = Tk()
        gam = P.sb(st, "sgam", [128, 64]); t_gam = Tk()
        sc = P.sb(st, "ssc", [128, 3, 64]); t_sc = Tk()
        yb = P.sb(st, "yb", [128, 64, 64]); t_yb = Tk()
        diag2 = [P.sb(st, "sdiag", [128, 128]) for _ in range(2)]; t_diag2 = [Tk(), Tk()]
        LT2 = [P.sb(st, "sLT", [128, 128]) for _ in range(2)]; t_LT2 = [Tk(), Tk()]
        yin2 = [P.sb(st, "syin", [128, 64]) for _ in range(2)]; t_yin2 = [Tk(), Tk()]
        t_gsb = [Tk(), Tk()]
        gs = P.sb(st, "sgs", [128, 16]); t_gs = Tk()
        junk = P.sb(st, "sjunk", [128, 512]); t_junk = Tk()
        pt = [P.ps(st, "spt", [128, 4, 128]) for _ in range(2)]; t_pt = [Tk(1), Tk(1)]
        pg = P.ps(st, "spg", [128, 2, 64]); t_pg = Tk(1)
        pL2 = [P.ps(st, "spL", [128, 128]) for _ in range(2)]; t_pL2 = [Tk(1), Tk(1)]
        py = P.ps(st, "spy", [128, 2, 64]); t_py = Tk(1)
        ph = P.ps(st, "sph", [128, 64]); t_ph = Tk(1)
        pc = P.ps(st, "spc", [128, 128]); t_pc = Tk(1)
        idf = C.idf
        ix = 0
        for i in range(NTP + 1):
            r0 = i * 128
            sample = (i == NTP)
            if sample:
                for g4 in range(16):
                    for j in range(4):
                        h = g4 * 4 + j
                        P.tr(pt[g4 % 2][0:64, j, :], hT[:, h, :], idf[:], reads=[t_h[h], C.t_id], writes=[t_pt[g4 % 2]])
                    P.copy("act", hio[:, g4 * 4:g4 * 4 + 4, :], pt[g4 % 2][0:64, :, :], reads=[t_pt[g4 % 2]], writes=[t_hio])
                P.dma(outs["sst_p"].rearrange("h p n -> p h n"), hio[:], reads=[t_hio], writes=[outs["t"]])
                P.dma(hio[:], I["ssm_state"].rearrange("h p n -> p h n"), writes=[t_hio])
                for g4 in range(16):
                    for j in range(4):
                        h = g4 * 4 + j
                        P.tr(pt[g4 % 2][:, j, 0:64], hio[:, h, :], idf[0:64, 0:64], reads=[t_hio, C.t_id], writes=[t_pt[g4 % 2]])
                    P.copy("act", hT[:, g4 * 4:g4 * 4 + 4, :], pt[g4 % 2][:, :, 0:64], reads=[t_pt[g4 % 2]], writes=t_h[g4 * 4:g4 * 4 + 4])
            P.dma(zd[:], ZDT[r0:r0 + 128, :], reads=[t_in], writes=[t_zd])
            P.dma(vm[:], I["VMASK"][r0:r0 + 128, :], writes=[t_vm])
            P.op("dve", lambda e: e.tensor_add(dt[:], zd[:, 4096:4160], dtb[:]), reads=[t_zd, t_K], writes=[t_dt])
            softplus(P, st, dt[:], t_dt, 64, "ssp%d" % i)
            P.op("dve", lambda e: e.tensor_scalar(dt[:], dt[:], vm[:, 0:1], None, ALU.mult), reads=[t_dt, t_vm], writes=[t_dt])
            P.op("dve", lambda e: e.tensor_mul(gam[:], dt[:], alog[:]), reads=[t_dt, t_K], writes=[t_gam])
            P.mm(pg[:, 0, :], TRI, gam[:], reads=[t_K, t_gam], writes=[t_pg])
            P.copy("dve", gam[:], pg[:, 0, :], reads=[t_pg], writes=[t_gam])
            P.mm(pg[:, 1, :], E127, gam[:], reads=[t_K, t_gam], writes=[t_pg])
            P.act(sc[:, 0, :], gam[:], AF.Exp, reads=[t_gam], writes=[t_sc])
            P.op("dve", lambda e: e.tensor_sub(sc[:, 1, :], pg[:, 1, :], gam[:]), reads=[t_pg, t_gam], writes=[t_sc])
            P.act(sc[:, 1, :], sc[:, 1, :], AF.Exp, reads=[t_sc], writes=[t_sc])
            P.act(sc[:, 2, :], pg[:, 1, :], AF.Exp, reads=[t_pg], writes=[t_sc])
            P.act(zd[:, 0:4096], zd[:, 0:4096], AF.Silu, reads=[t_zd], writes=[t_zd])
            for ch in range(48):
                xb = ix % 3; ab = ix % 2; ix += 1
                src = XBCT[ch * 128:(ch + 1) * 128]
                if i == 0:
                    P.op("pool", lambda e, xb=xb: e.memset(xq[xb][:, 0:3], 0.0), writes=[t_xq[xb]])
                    P.dma(xq[xb][:, 3:131], src[:, 0:128], reads=[t_in], writes=[t_xq[xb]])
                elif sample:
                    P.dma(xq[xb][:, 0:3], I["ssm_convT"][ch * 128:(ch + 1) * 128, :], writes=[t_xq[xb]])
                    P.dma(xq[xb][:, 3:131], src[:, r0:r0 + 128], reads=[t_in], writes=[t_xq[xb]])
                else:
                    P.dma(xq[xb][:], src[:, r0 - 3:r0 + 128], reads=[t_in], writes=[t_xq[xb]])
                a_ = acc[ab]
                P.op("dve", lambda e, xb=xb, a_=a_, ch=ch: e.tensor_scalar(a_[:], xq[xb][:, 0:128], cw[:, ch, 0:1], cw[:, ch, 4:5], ALU.mult, ALU.add),
                     reads=[t_xq[xb], t_K], writes=[t_acc[ab]])
                for j in range(1, 4):
                    P.op("dve", lambda e, xb=xb, a_=a_, ch=ch, j=j: e.scalar_tensor_tensor(a_[:], xq[xb][:, j:j + 128], cw[:, ch, j:j + 1], a_[:], ALU.mult, ALU.add),
                         reads=[t_xq[xb], t_K, t_acc[ab]], writes=[t_acc[ab]])
                if ch < 32:
                    P.act(a_[:], a_[:], AF.Silu, reads=[t_acc[ab]], writes=[t_acc[ab]])
                    p_ = (ch // 4) % 2
                    P.tr(pt[p_][:, ch % 4, :], a_[:], idf[:], reads=[t_acc[ab], C.t_id], writes=[t_pt[p_]])
                    if ch % 4 == 3:
                        P.copy("act", xtm[:, (ch - 3) * 2:(ch + 1) * 2, :].rearrange("p h d -> p (h d)"),
                               pt[p_][:].rearrange("p a b -> p (a b)"), reads=[t_pt[p_]], writes=[t_xtm])
                elif ch < 40:
                    P.act(BTf[:, ch - 32, :], a_[:], AF.Silu, reads=[t_acc[ab]], writes=[t_CT])
                else:
                    P.act(CT[:, ch - 40, :], a_[:], AF.Silu, reads=[t_acc[ab]], writes=[t_CT])
            for g4 in range(2):
                for j in range(4):
                    P.tr(pt[g4][:, j, :], BTf[:, g4 * 4 + j, :], idf[:], reads=[t_CT, C.t_id], writes=[t_pt[g4]])
                P.copy("act", Btm[:, g4 * 4:g4 * 4 + 4, :], pt[g4][:], reads=[t_pt[g4]], writes=[t_B])
            for g_ in range(8):
                P.mm(pc[:], BTf[:, g_, :], CT[:, g_, :], reads=[t_CT], writes=[t_pc])
                P.copy("act", cbT[:, g_, :], pc[:], reads=[t_pc], writes=[t_cb])
            dtb_ = dt[:].unsqueeze(2).to_broadcast([128, 64, 64])
            decb = sc[:, 1, :].unsqueeze(2).to_broadcast([128, 64, 64])
            P.op("dve", lambda e: e.tensor_tensor(xdt[:], xtm[:], dtb_, ALU.mult), reads=[t_xtm, t_dt], writes=[t_xd])
            P.op("pool", lambda e: e.tensor_tensor(xdec[:], xdt[:], decb, ALU.mult), reads=[t_xd, t_sc], writes=[t_xd])
            for h in range(64):
                g_ = h // 8
                hb = h % 2
                diag, t_diag, LT, t_LT, yin, t_yin = diag2[hb], t_diag2[hb], LT2[hb], t_LT2[hb], yin2[hb], t_yin2[hb]
                pL, t_pL, t_gh = pL2[hb], t_pL2[hb], t_gsb[hb]
                P.op("dve", lambda e, h=h, diag=diag: e.tensor_scalar(diag[:], idf[:], gam[:, h:h + 1], None, ALU.mult), reads=[C.t_id, t_gam], writes=[t_diag])
                P.op("dve", lambda e, h=h, hb=hb: e.tensor_scalar_mul(gs[:, hb:hb + 1], gam[:, h:h + 1], -1.0), reads=[t_gam], writes=[t_gh])
                P.mm(pL[:], ONES, diag[:], start=True, stop=False, reads=[t_K, t_diag], writes=[t_pL])
                P.mm(pL[:], idf[:], MADDT, start=False, stop=True, reads=[t_K, C.t_id], writes=[t_pL])
                P.act(LT[:], pL[:], AF.Exp, reads=[t_pL, t_gh], writes=[t_LT], bias=gs[:, hb:hb + 1])
                P.op("dve", lambda e, g_=g_, LT=LT: e.tensor_tensor(LT[:], LT[:], cbT[:, g_, :], ALU.mult), reads=[t_LT, t_cb], writes=[t_LT])
                P.mm(py[:, 0, :], LT[:], xdt[:, h, :], reads=[t_LT, t_xd], writes=[t_py])
                P.mm(py[:, 1, :], CT[:, g_, :], hT[:, h, :], reads=[t_CT, t_h[h]], writes=[t_py])
                P.copy("act", yin[:], py[:, 0, :], reads=[t_py], writes=[t_yin])
                P.op("dve", lambda e, h=h, yin=yin: e.scalar_tensor_tensor(yb[:, h, :], py[:, 1, :], sc[:, 0, h:h + 1], yin[:], ALU.mult, ALU.add),
                     reads=[t_py, t_sc, t_yin], writes=[t_yb])
                P.mm(ph[:], Btm[:, g_, :], xdec[:, h, :], reads=[t_B, t_xd], writes=[t_ph])
                P.op("dve", lambda e, h=h: e.scalar_tensor_tensor(hT[:, h, :], hT[:, h, :], sc[:, 2, h:h + 1], ph[:], ALU.mult, ALU.add),
                     reads=[t_h[h], t_sc, t_ph], writes=[t_h[h]])
            dskb = dsk[:].unsqueeze(2).to_broadcast([128, 64, 64])
            P.op("pool", lambda e: e.tensor_tensor(xdt[:], xtm[:], dskb, ALU.mult), reads=[t_xtm, t_K, t_xd], writes=[t_xd])
            P.op("dve", lambda e: e.tensor_add(yb[:], yb[:], xdt[:]), reads=[t_yb, t_xd], writes=[t_yb])
            ybf = yb[:].rearrange("p h d -> p (h d)")
            P.op("dve", lambda e: e.tensor_mul(ybf, ybf, zd[:, 0:4096]), reads=[t_yb, t_zd], writes=[t_yb])
            for g_ in range(8):
                P.act(junk[:], ybf[:, g_ * 512:(g_ + 1) * 512], AF.Square, reads=[t_yb], writes=[t_junk, t_gs], accum_out=gs[:, 8 + g_:9 + g_])
            P.op("dve", lambda e: e.tensor_scalar(gs[:, 8:16], gs[:, 8:16], 1.0 / 512, EPS, ALU.mult, ALU.add), reads=[t_gs], writes=[t_gs])
            P.act(gs[:, 8:16], gs[:, 8:16], AF.Sqrt, reads=[t_gs], writes=[t_gs])
            P.op("dve", lambda e: e.reciprocal(gs[:, 8:16], gs[:, 8:16]), reads=[t_gs], writes=[t_gs])
            rb_ = gs[:, 8:16].unsqueeze(2).to_broadcast([128, 8, 512])
            ybg = yb[:].rearrange("p (g h) d -> p g (h d)", g=8)
            P.op("dve", lambda e: e.tensor_tensor(ybg, ybg, rb_, ALU.mult), reads=[t_yb, t_gs], writes=[t_yb])
            P.op("dve", lambda e: e.tensor_mul(ybf, ybf, nw[:]), reads=[t_yb, t_K], writes=[t_yb])
            P.dma(Y[r0:r0 + 128, :], ybf, reads=[t_yb], writes=[t_y])
        for g4 in range(16):
            for j in range(4):
                h = g4 * 4 + j
                P.tr(pt[g4 % 2][0:64, j, :], hT[:, h, :], idf[:], reads=[t_h[h], C.t_id], writes=[t_pt[g4 % 2]])
            P.copy("act", hio[:, g4 * 4:g4 * 4 + 4, :], pt[g4 % 2][0:64, :, :], reads=[t_pt[g4 % 2]], writes=[t_hio])
        P.dma(outs["sst_s"].rearrange("h p n -> p h n"), hio[:], reads=[t_hio], writes=[outs["t"]])
        P.flush()


def host_msel(n_cmp, ncp, nsel):
    cs = np.arange(ncp)[:, None] * 16
    ss = np.arange(nsel)[None, :] * 64
    m = ((cs < ss + 64) & (cs + 32 > ss)).astype(np.float32)
    m[n_cmp:] = 0
    return m


def sample_ctx(P, C, I, KVR, KT, TP, t_kvr, X, t_sx):
    NPG = PAST // 128
    with ExitStack() as st:
        pti = P.sb(st, "pti", [128, NPG], I32); t_pt = Tk()
        P.dma(pti[:], I["pt_row"].partition_broadcast(128), writes=[t_pt])
        ptf = P.sb(st, "ptf", [128, NPG]); io = P.sb(st, "iota", [128, 1])
        P.dma(io[:], I["IOTA"], writes=[t_pt])
        P.op("dve", lambda e: e.tensor_copy(ptf[:], pti[:]), reads=[t_pt], writes=[t_pt])
        P.op("dve", lambda e: e.tensor_scalar(ptf[:], ptf[:], 128.0, io[:, 0:1], ALU.mult, ALU.add), reads=[t_pt], writes=[t_pt])
        idx = P.sb(st, "pidx", [128, NPG], I32)
        P.op("dve", lambda e: e.tensor_copy(idx[:], ptf[:]), reads=[t_pt], writes=[t_pt])
        pg_ = [P.sb(st, "pgt", [128, 512]) for _ in range(3)]; t_pg = [Tk(), Tk(), Tk()]
        ptr = [P.ps(st, "sptr", [128, 4, 128]) for _ in range(2)]; t_ptr = [Tk(1), Tk(1)]
        tb = [P.sb(st, "stb", [128, 4, 128]) for _ in range(2)]; t_tb = [Tk(), Tk()]
        ip = 0
        for which, pool in enumerate((I["pool_cmp"], I["pool_sel"])):
            for p in range(NPG):
                b = ip % 3; pb = ip % 2; ip += 1
                P.idma(pg_[b][:], pool, idx[:, p:p + 1], reads=[t_pt], writes=[t_pg[b]])
                if which == 0:
                    for j in range(4):
                        P.tr(ptr[pb][:, j, :], pg_[b][:, j * 128:(j + 1) * 128], C.idf[:], reads=[t_pg[b], C.t_id], writes=[t_ptr[pb]])
                    P.copy("act", tb[pb][:], ptr[pb][:], reads=[t_ptr[pb]], writes=[t_tb[pb]])
                    P.dma(X["CT"][:, :, p * 128:(p + 1) * 128].rearrange("a d t -> d a t"), tb[pb][:], reads=[t_tb[pb]], writes=[t_sx])
                else:
                    P.dma(X["SELR"][p * 128:(p + 1) * 128, :], pg_[b][:], reads=[t_pg[b]], writes=[t_sx])
                    for j in range(2):
                        P.tr(ptr[pb][:, j, :], pg_[b][:, j * 128:(j + 1) * 128], C.idf[:], reads=[t_pg[b], C.t_id], writes=[t_ptr[pb]])
                    P.copy("act", tb[pb][:, 0:2, :], ptr[pb][:, 0:2, :], reads=[t_ptr[pb]], writes=[t_tb[pb]])
                    P.dma(X["SKT"][:, :, p * 128:(p + 1) * 128].rearrange("a d t -> d a t"), tb[pb][:, 0:2, :], reads=[t_tb[pb]], writes=[t_sx])
        P.dma(X["SELR"][PAST:PAST + 128, :], KVR[TP:TP + 128, 512:1024], reads=[t_kvr], writes=[t_sx])
        P.dma(X["SKT"][:, :, PAST:PAST + 128], KT[4:6, :, TP:TP + 128], reads=[t_kvr], writes=[t_sx])
        P.dma(X["WINR"][0:512, :], I["win_cache"], writes=[t_sx])
        P.dma(X["WINR"][512:640, :], KVR[TP:TP + 128, 1024:1536], reads=[t_kvr], writes=[t_sx])
        P.dma(X["WKT"][:, :, 512:640], KT[6:8, :, TP:TP + 128], reads=[t_kvr], writes=[t_sx])
        for p in range(4):
            b = ip % 3; pb = ip % 2; ip += 1
            P.dma(pg_[b][:], I["win_cache"][p * 128:(p + 1) * 128, :], writes=[t_pg[b]])
            for j in range(2):
                P.tr(ptr[pb][:, j, :], pg_[b][:, j * 128:(j + 1) * 128], C.idf[:], reads=[t_pg[b], C.t_id], writes=[t_ptr[pb]])
            P.copy("act", tb[pb][:, 0:2, :], ptr[pb][:, 0:2, :], reads=[t_ptr[pb]], writes=[t_tb[pb]])
            P.dma(X["WKT"][:, :, p * 128:(p + 1) * 128].rearrange("a d t -> d a t"), tb[pb][:, 0:2, :], reads=[t_tb[pb]], writes=[t_sx])
        P.flush()


def allreduce(P, src, dst, nrows, t_src, t_dst, n_cores, chunk=1024):
    for r0 in range(0, nrows, chunk):
        r1 = min(nrows, r0 + chunk)
        P.op("pool", lambda e, r0=r0, r1=r1: e.collective_compute(
            "AllReduce", ALU.add, replica_groups=[list(range(n_cores))],
            ins=[src[r0:r1, :]], outs=[dst[r0:r1, :]]), reads=[t_src], writes=[t_dst])


def moe_stage(P, C, H3, I, outs, TP, NT, t_h3, n_cores, dbg_kind):
    nc = P.nc
    NG = 2 * TP + 128
    NGT = NG // 128
    NTP = TP // 128
    GS = nc.dram_tensor("GATHsrc", [NG, D], F32).ap()
    G = nc.dram_tensor("GATH", [NG, D], F32).ap()
    ES = nc.dram_tensor("EOUTsrc", [NG, D], F32).ap()
    EO = nc.dram_tensor("EOUT", [NG, D], F32, kind=dbg_kind).ap()
    GATE = nc.dram_tensor("GATE", [NG, 1], F32, kind=dbg_kind).ap()
    t_gs, t_g, t_es, t_eo, t_gate = Tk(), Tk(), Tk(), Tk(), Tk()
    with ExitStack() as st:
        fl = P.sb(st, "fl", [128, 12]); t_fl = Tk()
        P.dma(fl[:], I["FLAGS"], writes=[t_fl])
        ht = [P.sb(st, "mht", [128, D]) for _ in range(2)]; t_ht = [Tk(), Tk()]
        o0 = [P.sb(st, "mo0", [128, D]) for _ in range(2)]; t_o0 = [Tk(), Tk()]
        o1 = [P.sb(st, "mo1", [128, D]) for _ in range(2)]; t_o1 = [Tk(), Tk()]
        for i in range(NTP):
            b = i % 2
            P.dma(ht[b][:], H3[i * 128:(i + 1) * 128, :], reads=[t_h3], writes=[t_ht[b]])
            P.op("dve", lambda e, b=b: e.tensor_scalar(o0[b][:], ht[b][:], fl[:, 0:1], None, ALU.mult), reads=[t_ht[b], t_fl], writes=[t_o0[b]])
            P.op("pool", lambda e, b=b: e.tensor_scalar(o1[b][:], ht[b][:], fl[:, 1:2], None, ALU.mult), reads=[t_ht[b], t_fl], writes=[t_o1[b]])
            P.dma(GS[i * 128:(i + 1) * 128, :], o0[b][:], reads=[t_o0[b]], writes=[t_gs])
            P.dma(GS[TP + i * 128:TP + (i + 1) * 128, :], o1[b][:], reads=[t_o1[b]], writes=[t_gs])
        P.op("pool", lambda e: e.memset(o1[0][:], 0.0), reads=[t_o1[0]], writes=[t_o1[0]])
        P.dma(GS[2 * TP:2 * TP + 128, :], o1[0][:], reads=[t_o1[0]], writes=[t_gs])
        P.dma(ht[0][0:4, :], H3[TP:TP + 4, :], reads=[t_h3], writes=[t_ht[0]])
        for s_ in range(8):
            b = s_ % 2
            P.op("dve", lambda e, b=b, s_=s_: e.tensor_scalar(o0[b][0:4, :], ht[0][0:4, :], fl[0:4, 2 + s_:3 + s_], None, ALU.mult),
                 reads=[t_ht[0], t_fl], writes=[t_o0[b]])
            P.dma(GS[2 * TP + 4 * s_:2 * TP + 4 * s_ + 4, :], o0[b][0:4, :], reads=[t_o0[b]], writes=[t_gs])
        allreduce(P, GS, G, NG, t_gs, t_g, n_cores)
        P.flush()
    with ExitStack() as st:
        fl = P.sb(st, "fl2", [128, 12]); t_fl = Tk()
        P.dma(fl[:], I["FLAGS"], writes=[t_fl])
        gt = P.sb(st, "rgt", [128, D]); t_gt = Tk()
        P.dma(gt[:], I["ssm_norm_ffn"].partition_broadcast(128), writes=[t_gt])
        rw = P.sb(st, "rw", [128, 16, 8]); t_rw = Tk()
        P.dma(rw[:], I["moe_router"].rearrange("(k p) e -> p k e", p=128), writes=[t_rw])
        xt = [P.sb(st, "rxt", [128, D]) for _ in range(2)]; t_x = [Tk(), Tk()]
        sq = P.sb(st, "rsq", [128, D]); t_sq = Tk()
        ss = P.sb(st, "rss", [128, 1]); t_ss = Tk()
        xT = P.sb(st, "rxT", [128, 16, 128]); t_xT = Tk()
        ptr = [P.ps(st, "rptr", [128, 4, 128]) for _ in range(2)]; t_ptr = [Tk(1), Tk(1)]
        pl = P.ps(st, "rpl", [128, 8]); t_pl = Tk(1)
        lg = P.sb(st, "rlg", [128, 8]); t_lg = Tk()
        m8 = P.sb(st, "rm8", [128, 8]); t_m8 = Tk()
        w12 = P.sb(st, "rw12", [128, 2]); t_w = Tk()
        gte = P.sb(st, "rgte", [128, 8]); tmp = P.sb(st, "rtmp", [128, 8]); t_gte = Tk()
        gm = [P.sb(st, "rgm", [128, 1]) for _ in range(2)]; t_gm = [Tk(), Tk()]
        for i in range(NGT):
            b = i % 2
            P.dma(xt[b][:], G[i * 128:(i + 1) * 128, :], reads=[t_g], writes=[t_x[b]])
            P.act(sq[:], xt[b][:], AF.Square, reads=[t_x[b]], writes=[t_sq, t_ss], accum_out=ss[:])
            P.op("dve", lambda e: e.tensor_scalar(ss[:], ss[:], 1.0 / D, EPS, ALU.mult, ALU.add), reads=[t_ss], writes=[t_ss])
            P.act(ss[:], ss[:], AF.Sqrt, reads=[t_ss], writes=[t_ss])
            P.op("dve", lambda e: e.reciprocal(ss[:], ss[:]), reads=[t_ss], writes=[t_ss])
            P.op("dve", lambda e, b=b: e.scalar_tensor_tensor(sq[:], xt[b][:], ss[:, 0:1], gt[:], ALU.mult, ALU.mult),
                 reads=[t_x[b], t_ss, t_gt], writes=[t_sq])
            for kg in range(4):
                pb = kg % 2
                for j in range(4):
                    k = kg * 4 + j
                    P.tr(ptr[pb][:, j, :], sq[:, k * 128:(k + 1) * 128], C.idf[:], reads=[t_sq, C.t_id], writes=[t_ptr[pb]])
                P.copy("act", xT[:, kg * 4:kg * 4 + 4, :], ptr[pb][:], reads=[t_ptr[pb]], writes=[t_xT])
            for k in range(16):
                P.mm(pl[:], xT[:, k, :], rw[:, k, :], start=(k == 0), stop=(k == 15), reads=[t_xT, t_rw], writes=[t_pl])
            P.copy("dve", lg[:], pl[:], reads=[t_pl], writes=[t_lg])
            P.op("dve", lambda e: e.max(m8[:], lg[:]), reads=[t_lg], writes=[t_m8])
            P.op("dve", lambda e: e.tensor_sub(w12[:, 0:1], m8[:, 1:2], m8[:, 0:1]), reads=[t_m8], writes=[t_w])
            P.act(w12[:, 0:1], w12[:, 0:1], AF.Exp, reads=[t_w], writes=[t_w])
            P.op("dve", lambda e: e.tensor_scalar_add(w12[:, 0:1], w12[:, 0:1], 1.0), reads=[t_w], writes=[t_w])
            P.op("dve", lambda e: e.reciprocal(w12[:, 0:1], w12[:, 0:1]), reads=[t_w], writes=[t_w])
            P.op("dve", lambda e: e.tensor_scalar(w12[:, 1:2], w12[:, 0:1], -1.0, 1.0, ALU.mult, ALU.add), reads=[t_w], writes=[t_w])
            P.op("dve", lambda e: e.tensor_scalar(gte[:], lg[:], m8[:, 0:1], w12[:, 0:1], ALU.is_equal, ALU.mult), reads=[t_lg, t_m8, t_w], writes=[t_gte])
            P.op("dve", lambda e: e.tensor_scalar(tmp[:], lg[:], m8[:, 1:2], w12[:, 1:2], ALU.is_equal, ALU.mult), reads=[t_lg, t_m8, t_w], writes=[t_gte])
            P.op("dve", lambda e: e.tensor_add(gte[:], gte[:], tmp[:]), reads=[t_gte], writes=[t_gte])
            P.op("dve", lambda e: e.tensor_mul(gte[:], gte[:], fl[:, 2:10]), reads=[t_gte, t_fl], writes=[t_gte])
            P.op("dve", lambda e, b=b: e.reduce_sum(gm[b][:], gte[:], AX.X), reads=[t_gte], writes=[t_gm[b]])
            P.dma(GATE[i * 128:(i + 1) * 128, :], gm[b][:], reads=[t_gm[b]], writes=[t_gate])
        P.flush()
    swiglu_ffn(P, C, G, I["ssm_norm_ffn"], I["moe_wg"], I["moe_wu"], I["moe_wd"], 7168, NGT, None, ES, t_g, t_es,
               rowscale=GATE, t_rs=t_gate)
    allreduce(P, ES, EO, NG, t_es, t_eo, n_cores)
    P.flush()
    with ExitStack() as st:
        fl = P.sb(st, "fl3", [128, 12]); t_fl = Tk()
        P.dma(fl[:], I["FLAGS"], writes=[t_fl])
        gt = P.sb(st, "ngt", [128, D]); t_gt = Tk()
        P.dma(gt[:], I["final_norm"].partition_broadcast(128), writes=[t_gt])
        ht = [P.sb(st, "nht", [128, D]) for _ in range(2)]; t_ht = [Tk(), Tk()]
        e0 = [P.sb(st, "ne0", [128, D]) for _ in range(2)]; t_e0 = [Tk(), Tk()]
        e1 = [P.sb(st, "ne1", [128, D]) for _ in range(2)]; t_e1 = [Tk(), Tk()]
        sq = P.sb(st, "nsq", [128, D]); t_sq = Tk()
        ss = P.sb(st, "nss", [128, 1]); t_ss = Tk()
        yo = [P.sb(st, "nyo", [128, D]) for _ in range(2)]; t_yo = [Tk(), Tk()]
        es = P.sb(st, "nes", [128, 8, D // 4]); t_esb = Tk()
        for i in range(NTP + 1):
            b = i % 2
            P.dma(ht[b][:], H3[i * 128:(i + 1) * 128, :], reads=[t_h3], writes=[t_ht[b]])
            if i < NTP:
                P.dma(e0[b][:], EO[i * 128:(i + 1) * 128, :], reads=[t_eo], writes=[t_e0[b]])
                P.dma(e1[b][:], EO[TP + i * 128:TP + (i + 1) * 128, :], reads=[t_eo], writes=[t_e1[b]])
                P.op("dve", lambda e, b=b: e.scalar_tensor_tensor(ht[b][:], e0[b][:], fl[:, 10:11], ht[b][:], ALU.mult, ALU.add),
                     reads=[t_e0[b], t_fl, t_ht[b]], writes=[t_ht[b]])
                P.op("dve", lambda e, b=b: e.scalar_tensor_tensor(ht[b][:], e1[b][:], fl[:, 11:12], ht[b][:], ALU.mult, ALU.add),
                     reads=[t_e1[b], t_fl, t_ht[b]], writes=[t_ht[b]])
            else:
                for s_ in range(8):
                    P.dma(e0[b][0:4, :], EO[2 * TP + 4 * s_:2 * TP + 4 * s_ + 4, :], reads=[t_eo], writes=[t_e0[b]])
                    P.op("dve", lambda e, b=b, s_=s_: e.scalar_tensor_tensor(ht[b][0:4, :], e0[b][0:4, :], fl[0:4, 2 + s_:3 + s_], ht[b][0:4, :], ALU.mult, ALU.add),
                         reads=[t_e0[b], t_fl, t_ht[b]], writes=[t_ht[b]])
            P.act(sq[:], ht[b][:], AF.Square, reads=[t_ht[b]], writes=[t_sq, t_ss], accum_out=ss[:])
            P.op("dve", lambda e: e.tensor_scalar(ss[:], ss[:], 1.0 / D, EPS, ALU.mult, ALU.add), reads=[t_ss], writes=[t_ss])
            P.act(ss[:], ss[:], AF.Sqrt, reads=[t_ss], writes=[t_ss])
            P.op("dve", lambda e: e.reciprocal(ss[:], ss[:]), reads=[t_ss], writes=[t_ss])
            P.op("dve", lambda e, b=b: e.scalar_tensor_tensor(yo[b][:], ht[b][:], ss[:, 0:1], gt[:], ALU.mult, ALU.mult),
                 reads=[t_ht[b], t_ss, t_gt], writes=[t_yo[b]])
            if i < NTP:
                P.dma(outs["y_p"][i * 128:(i + 1) * 128, :], yo[b][:], reads=[t_yo[b]], writes=[outs["t"]])
            else:
                P.dma(outs["y_s"][0:4, :], yo[b][0:4, :], reads=[t_yo[b]], writes=[outs["t"]])
        P.flush()


def moe_local_stage(P, C, H3, I, outs, TP, t_h3, dbg_kind):
    nc = P.nc
    NTP = TP // 128
    nsplit = 4 if NTP % 4 == 0 else (2 if NTP % 2 == 0 else 1)
    NQ = NTP // nsplit
    NR = (NQ + 1) * 128
    HQ = nc.dram_tensor("HQ", [NR, D], F32, kind=dbg_kind).ap()
    ACC = [nc.dram_tensor("ACC%d" % i, [NR, D], F32).ap() for i in range(2)]
    GATE8 = nc.dram_tensor("GATE8", [NR, 8], F32, kind=dbg_kind).ap()
    t_hq, t_gate = Tk(), Tk()
    with ExitStack() as st:
        qi = P.sb(st, "qidx", [128, NQ], I32); t_qi = Tk()
        P.dma(qi[:], I["QIDX"], writes=[t_qi])
        gt = P.sb(st, "rgt", [128, D]); t_gt = Tk()
        P.dma(gt[:], I["ssm_norm_ffn"].partition_broadcast(128), writes=[t_gt])
        rw = P.sb(st, "rw", [128, 16, 8]); t_rw = Tk()
        P.dma(rw[:], I["moe_router"].rearrange("(k p) e -> p k e", p=128), writes=[t_rw])
        xt = [P.sb(st, "rxt", [128, D]) for _ in range(2)]; t_x = [Tk(), Tk()]
        sq = P.sb(st, "rsq", [128, D]); t_sq = Tk()
        ss = P.sb(st, "rss", [128, 1]); t_ss = Tk()
        xT = P.sb(st, "rxT", [128, 16, 128]); t_xT = Tk()
        ptr = [P.ps(st, "rptr", [128, 4, 128]) for _ in range(2)]; t_ptr = [Tk(1), Tk(1)]
        pl = P.ps(st, "rpl", [128, 8]); t_pl = Tk(1)
        lg = P.sb(st, "rlg", [128, 8]); t_lg = Tk()
        m8 = P.sb(st, "rm8", [128, 8]); t_m8 = Tk()
        w12 = P.sb(st, "rw12", [128, 2]); t_w = Tk()
        gte = [P.sb(st, "rgte", [128, 8]) for _ in range(2)]; tmp = P.sb(st, "rtmp", [128, 8]); t_gte = [Tk(), Tk()]
        for i in range(NQ + 1):
            b = i % 2
            if i < NQ:
                P.idma(xt[b][:], H3, qi[:, i:i + 1], reads=[t_h3, t_qi], writes=[t_x[b]])
            else:
                P.dma(xt[b][:], H3[TP:TP + 128, :], reads=[t_h3], writes=[t_x[b]])
            P.dma(HQ[i * 128:(i + 1) * 128, :], xt[b][:], reads=[t_x[b]], writes=[t_hq])
            P.act(sq[:], xt[b][:], AF.Square, reads=[t_x[b]], writes=[t_sq, t_ss], accum_out=ss[:])
            P.op("dve", lambda e: e.tensor_scalar(ss[:], ss[:], 1.0 / D, EPS, ALU.mult, ALU.add), reads=[t_ss], writes=[t_ss])
            P.act(ss[:], ss[:], AF.Sqrt, reads=[t_ss], writes=[t_ss])
            P.op("dve", lambda e: e.reciprocal(ss[:], ss[:]), reads=[t_ss], writes=[t_ss])
            P.op("dve", lambda e, b=b: e.scalar_tensor_tensor(sq[:], xt[b][:], ss[:, 0:1], gt[:], ALU.mult, ALU.mult),
                 reads=[t_x[b], t_ss, t_gt], writes=[t_sq])
            for kg in range(4):
                pb = kg % 2
                for j in range(4):
                    k = kg * 4 + j
                    P.tr(ptr[pb][:, j, :], sq[:, k * 128:(k + 1) * 128], C.idf[:], reads=[t_sq, C.t_id], writes=[t_ptr[pb]])
                P.copy("act", xT[:, kg * 4:kg * 4 + 4, :], ptr[pb][:], reads=[t_ptr[pb]], writes=[t_xT])
            for k in range(16):
                P.mm(pl[:], xT[:, k, :], rw[:, k, :], start=(k == 0), stop=(k == 15), reads=[t_xT, t_rw], writes=[t_pl])
            P.copy("dve", lg[:], pl[:], reads=[t_pl], writes=[t_lg])
            P.op("dve", lambda e: e.max(m8[:], lg[:]), reads=[t_lg], writes=[t_m8])
            P.op("dve", lambda e: e.tensor_sub(w12[:, 0:1], m8[:, 1:2], m8[:, 0:1]), reads=[t_m8], writes=[t_w])
            P.act(w12[:, 0:1], w12[:, 0:1], AF.Exp, reads=[t_w], writes=[t_w])
            P.op("dve", lambda e: e.tensor_scalar_add(w12[:, 0:1], w12[:, 0:1], 1.0), reads=[t_w], writes=[t_w])
            P.op("dve", lambda e: e.reciprocal(w12[:, 0:1], w12[:, 0:1]), reads=[t_w], writes=[t_w])
            P.op("dve", lambda e: e.tensor_scalar(w12[:, 1:2], w12[:, 0:1], -1.0, 1.0, ALU.mult, ALU.add), reads=[t_w], writes=[t_w])
            g_ = gte[b]
            P.op("dve", lambda e, g_=g_: e.tensor_scalar(g_[:], lg[:], m8[:, 0:1], w12[:, 0:1], ALU.is_equal, ALU.mult), reads=[t_lg, t_m8, t_w], writes=[t_gte[b]])
            P.op("dve", lambda e: e.tensor_scalar(tmp[:], lg[:], m8[:, 1:2], w12[:, 1:2], ALU.is_equal, ALU.mult), reads=[t_lg, t_m8, t_w], writes=[t_gte[b]])
            P.op("dve", lambda e, g_=g_: e.tensor_add(g_[:], g_[:], tmp[:]), reads=[t_gte[b]], writes=[t_gte[b]])
            P.dma(GATE8[i * 128:(i + 1) * 128, :], g_[:], reads=[t_gte[b]], writes=[t_gate])
        P.flush()
    t_acc = Tk()
    t_acc.w = t_hq.w
    prev = HQ
    for e_ in range(8):
        dst = ACC[e_ % 2]
        swiglu_ffn(P, C, HQ, I["ssm_norm_ffn"], I["moe_wg"][e_], I["moe_wu"][e_], I["moe_wd"][e_], 7168, NQ + 1, prev, dst,
                   t_acc, t_acc, rowscale=GATE8[:, e_:e_ + 1], t_rs=t_gate)
        prev = dst
    with ExitStack() as st:
        gt = P.sb(st, "ngt", [128, D]); t_gt = Tk()
        P.dma(gt[:], I["final_norm"].partition_broadcast(128), writes=[t_gt])
        ht = [P.sb(st, "nht", [128, D]) for _ in range(2)]; t_ht = [Tk(), Tk()]
        sq = P.sb(st, "nsq", [128, D]); t_sq = Tk()
        ss = P.sb(st, "nss", [128, 1]); t_ss = Tk()
        yo = [P.sb(st, "nyo", [128, D]) for _ in range(2)]; t_yo = [Tk(), Tk()]
        for i in range(NQ + 1):
            b = i % 2
            P.dma(ht[b][:], prev[i * 128:(i + 1) * 128, :], reads=[t_acc], writes=[t_ht[b]])
            P.act(sq[:], ht[b][:], AF.Square, reads=[t_ht[b]], writes=[t_sq, t_ss], accum_out=ss[:])
            P.op("dve", lambda e: e.tensor_scalar(ss[:], ss[:], 1.0 / D, EPS, ALU.mult, ALU.add), reads=[t_ss], writes=[t_ss])
            P.act(ss[:], ss[:], AF.Sqrt, reads=[t_ss], writes=[t_ss])
            P.op("dve", lambda e: e.reciprocal(ss[:], ss[:]), reads=[t_ss], writes=[t_ss])
            P.op("dve", lambda e, b=b: e.scalar_tensor_tensor(yo[b][:], ht[b][:], ss[:, 0:1], gt[:], ALU.mult, ALU.mult),
                 reads=[t_ht[b], t_ss, t_gt], writes=[t_yo[b]])
            if i < NQ:
                P.dma(outs["y_q"][i * 128:(i + 1) * 128, :], yo[b][:], reads=[t_yo[b]], writes=[outs["t"]])
            else:
                P.dma(outs["y_s"][0:4, :], yo[b][0:4, :], reads=[t_yo[b]], writes=[outs["t"]])
        P.flush()


def outs_dict_sconv(outs, nc, outp):
    if "sconv_p" not in outs:
        outs["sconv_p"] = outp("sconv_p", [3, 6144])
        outs["sconv_s"] = outp("sconv_s", [3, 6144])
    return outs


def build(TP=4096, debug=False, limit=None, stages="AKGN", n_cores=8):
    nc = bass.Bass("TRN2", target_bir_lowering=False)
    _FILL_REGS.clear()
    NT = TP // 128 + 1
    TT = NT * 128
    kind_dbg = "ExternalOutput" if debug else "Internal"

    build.inputs = []

    def inp(name, shape, dt=F32):
        build.inputs.append(name)
        return nc.dram_tensor(name, list(shape), dt, kind="ExternalInput").ap()

    xcat = inp("xcat", [TT, D])
    hyb_norm_mix = inp("hyb_norm_mix", [1, D])
    hyb_w_in = inp("hyb_w_in", [D, HYB_IN])
    with ExitStack() as glob:
        P = Prog(nc, glob)
        P.limit = limit
        C = Ctx()
        C.glob = glob
        make_consts(P, C)
        QKVT = nc.dram_tensor("QKVT", [NFM, TT], F32, kind=kind_dbg).ap()
        PROJ = nc.dram_tensor("PROJ", [TT, NPROJ], F32, kind=kind_dbg).ap()
        t_A = Tk()
        norm_linear(P, C, xcat, hyb_norm_mix[0:1, :], hyb_w_in, NT, HYB_IN, NFM, QKVT, PROJ, t_A)
        ROPE = inp("ROPE", [TT, 128])
        win_cache = inp("win_cache", [512, 512])

        def outp(name, shape):
            return nc.dram_tensor(name, list(shape), F32, kind="ExternalOutput").ap()
        WP = min(512, TP)
        outs = {"t": Tk(), "cmp_p": outp("cmp_p", [TP, 512]), "sel_p": outp("sel_p", [TP, 512]),
                "win_p": outp("win_p", [WP, 512]), "cmp_s": outp("cmp_s", [4, 512]),
                "sel_s": outp("sel_s", [4, 512]), "win_s": outp("win_s", [512, 512]),
                "gconv_p": outp("gconv_p", [3, NFM]), "gconv_s": outp("gconv_s", [3, NFM])}
        KVR = nc.dram_tensor("KVR", [TT, 1536], F32, kind=kind_dbg).ap()
        KT = nc.dram_tensor("KT", [8, 128, TT], F32, kind=kind_dbg).ap()
        t_kvr = Tk()
        kv_outputs(P, C, PROJ, QKVT, ROPE, win_cache, outs, TP, t_A, KVR, KT, t_kvr)
        I = {"GCONST": inp("GCONST", [128, 7 * 128]), "gdn_cw": inp("gdn_cw", [128, 8, 3, 4]),
             "gdn_a_log": inp("gdn_a_log", [1, 8]), "gdn_dt_bias": inp("gdn_dt_bias", [1, 8]),
             "gdn_norm_w": inp("gdn_norm_w", [1, 128]), "VMASK": inp("VMASK", [TT, 1]),
             "gdn_state": inp("gdn_state", [8, 128, 128]), "gdn_convT": inp("gdn_convT", [NFM, 3])}
        outs["gst_p"] = outp("gst_p", [8, 128, 128])
        outs["gst_s"] = outp("gst_s", [8, 128, 128])
        MIX = nc.dram_tensor("MIX", [TT, 2048], F32, kind=kind_dbg).ap()
        t_mix = Tk()
        if "N" in stages or "S" in stages:
            I["cmp_w1"] = inp("cmp_w1", [2, 4096, 256])
            I["cmp_w2"] = inp("cmp_w2", [2, 256, 128])
            I["cmp_peT"] = inp("cmp_peT", [2, 128, 32])
            I["CAUS"] = inp("CAUS", [128, 128])
        if "G" in stages:
            gdn_stage(P, C, QKVT, PROJ, MIX, I, outs, TP, t_A, t_mix)
        if "N" in stages:
            n_cmp = (TP - 32) // 16 + 1
            ncp = -(-n_cmp // 128) * 128
            nsel = max(8, TP // 64)
            I["MSEL_P"] = inp("MSEL_P", [ncp, nsel])
            with ExitStack() as stn:
                KCT = P.sb(stn, "KCT", [128, 2, ncp])
                VC = P.sb(stn, "VC", [128, ncp // 128, 2, 128])
                t_kc = Tk()
                compress(P, C, KT[0:4], n_cmp, I, KCT, VC, t_kvr, t_kc)
                tiles = []
                for i in range(TP // 128):
                    w0 = max(0, 128 * i - 512)
                    tiles.append(dict(r0=128 * i, qpos0=128 * i, nk=128 * (i + 1), w0=w0, nw=128 * (i + 1) - w0, wpos0=0))
                X = dict(SKT=KT[4:6], SELR=KVR[:, 512:1024], WKT=KT[6:8], WINR=KVR[:, 1024:1536], ncp=ncp, nsel=nsel,
                         msel=I["MSEL_P"])
                nsa_qtiles(P, C, tiles, X, PROJ, ROPE, MIX, I, KCT, VC, t_A, t_kvr, t_kc, t_mix)
        if "S" in stages:
            NKS = PAST + 128
            n_cmp_s = (PAST + 4 - 32) // 16 + 1
            ncp_s = -(-n_cmp_s // 128) * 128
            nsel_s = NKS // 64
            I.update({"pt_row": inp("pt_row", [1, PAST // 128], I32), "IOTA": inp("IOTA", [128, 1]),
                      "pool_cmp": inp("pool_cmp", [I_NPHYS[0] * 128, 512]), "pool_sel": inp("pool_sel", [I_NPHYS[0] * 128, 512]),
                      "win_cache": win_cache, "MSEL_S": inp("MSEL_S", [ncp_s, nsel_s])})
            XS = dict(CT=nc.dram_tensor("CT_S", [4, 128, PAST], F32).ap(), SELR=nc.dram_tensor("SELR_S", [NKS, 512], F32).ap(),
                      SKT=nc.dram_tensor("SKT_S", [2, 128, NKS], F32).ap(), WINR=nc.dram_tensor("WINR_S", [640, 512], F32).ap(),
                      WKT=nc.dram_tensor("WKT_S", [2, 128, 640], F32).ap(), ncp=ncp_s, nsel=nsel_s, msel=I["MSEL_S"])
            XS["SELR"] = XS["SELR"]
            t_sx = Tk()
            sample_ctx(P, C, I, KVR, KT, TP, t_kvr, XS, t_sx)
            with ExitStack() as stn:
                KCT = P.sb(stn, "KCTs", [128, 2, ncp_s])
                VC = P.sb(stn, "VCs", [128, ncp_s // 128, 2, 128])
                t_kc = Tk()
                compress(P, C, XS["CT"], n_cmp_s, I, KCT, VC, t_sx, t_kc)
                tiles = [dict(r0=TP, qpos0=PAST, nk=NKS, w0=0, nw=640, wpos0=PAST - 512)]
                XQ = dict(XS)
                XQ["SELR"] = XS["SELR"]
                nsa_qtiles(P, C, tiles, XQ, PROJ, ROPE, MIX, I, KCT, VC, t_A, t_sx, t_kc, t_mix)
        if "S" not in stages:
            with ExitStack() as stz:
                zt = P.sb(stz, "zt", [128, 1024])
                t_z = Tk()
                P.op("pool", lambda e: e.memset(zt[:], 0.0), writes=[t_z])
                P.dma(MIX[TP:TT, 1024:2048], zt[:], reads=[t_z], writes=[t_mix])
                P.flush()
        if "F" in stages:
            H1 = nc.dram_tensor("H1", [TT, D], F32, kind=kind_dbg).ap()
            H2 = nc.dram_tensor("H2", [TT, D], F32, kind=kind_dbg).ap()
            t_h1, t_h2 = Tk(), Tk()
            hyb_w_out = inp("hyb_w_out", [D, D])
            norm_linear(P, C, MIX, None, hyb_w_out, NT, D, 0, None, H1, t_h1, sb_tiles=17, t_in=t_mix, norm=False,
                        resid=xcat)
            ffn_g, ffn_u, ffn_d = inp("ffn_w_gate", [D, 5632]), inp("ffn_w_up", [D, 5632]), inp("ffn_w_down", [5632, D])
            hyb_norm_ffn = inp("hyb_norm_ffn", [1, D])
            swiglu_ffn(P, C, H1, hyb_norm_ffn[0:1, :], ffn_g, ffn_u, ffn_d, 5632, NT, H1, H2, t_h1, t_h2)
        if "M" in stages:
            XBCT = nc.dram_tensor("XBCT", [6144, TT], F32, kind=kind_dbg).ap()
            ZDT = nc.dram_tensor("ZDT", [TT, 4160], F32, kind=kind_dbg).ap()
            Yt = nc.dram_tensor("Yssd", [TT, 4096], F32, kind=kind_dbg).ap()
            H3 = nc.dram_tensor("H3", [TT, D], F32, kind=kind_dbg).ap()
            t_l1, t_y, t_h3 = Tk(), Tk(), Tk()
            ssm_w_in = inp("ssm_w_in_r", [D, 10304])
            ssm_norm_mix = inp("ssm_norm_mix", [1, D])
            norm_linear(P, C, H2, ssm_norm_mix[0:1, :], ssm_w_in, NT, 10304, 6144, XBCT, ZDT, t_l1, t_in=t_h2)
            for r in range(3):
                P.dma(outs_dict_sconv(outs, nc, outp)["sconv_p"][r:r + 1, :], XBCT[:, TP - 3 + r:TP - 2 + r].rearrange("c o -> o c"),
                      reads=[t_l1], writes=[outs["t"]], allow_slow_non_contiguous=True)
                P.dma(outs["sconv_s"][r:r + 1, :], XBCT[:, TP + 1 + r:TP + 2 + r].rearrange("c o -> o c"),
                      reads=[t_l1], writes=[outs["t"]], allow_slow_non_contiguous=True)
            I.update({"SCONST": inp("SCONST", [128, 512]), "ssm_cw": inp("ssm_cw", [128, 48, 5]),
                      "ssm_a_log": inp("ssm_a_log", [1, 64]), "ssm_dt_bias": inp("ssm_dt_bias", [1, 64]),
                      "ssm_d_skip": inp("ssm_d_skip", [1, 64]), "ssm_norm_w": inp("ssm_norm_w", [1, 4096]),
                      "ssm_state": inp("ssm_state", [64, 64, 128]), "ssm_convT": inp("ssm_convT", [6144, 3])})
            outs["sst_p"] = outp("sst_p", [64, 64, 128])
            outs["sst_s"] = outp("sst_s", [64, 64, 128])
            ssd_stage(P, C, XBCT, ZDT, Yt, I, outs, TP, t_l1, t_y)
            ssm_w_out = inp("ssm_w_out", [4096, D])
            norm_linear(P, C, Yt, None, ssm_w_out, NT, D, 0, None, H3, t_h3, sb_tiles=8, cb=128, t_in=t_y, norm=False,
                        resid=H2, t_res=t_h2)
        if "L" in stages:
            NTP_ = TP // 128
            nsplit = 4 if NTP_ % 4 == 0 else (2 if NTP_ % 2 == 0 else 1)
            I.update({"QIDX": inp("QIDX", [128, NTP_ // nsplit], I32), "ssm_norm_ffn": inp("ssm_norm_ffn", [1, D]),
                      "moe_router": inp("moe_router", [D, 8]), "moe_wg": inp("moe_wg", [8, D, 7168]),
                      "moe_wu": inp("moe_wu", [8, D, 7168]), "moe_wd": inp("moe_wd", [8, 7168, D]),
                      "final_norm": inp("final_norm", [1, D])})
            I["ssm_norm_ffn"] = I["ssm_norm_ffn"][0:1, :]
            I["final_norm"] = I["final_norm"][0:1, :]
            outs["y_q"] = outp("y_q", [TP // nsplit, D])
            outs["y_s"] = outp("y_s", [4, D])
            moe_local_stage(P, C, H3, I, outs, TP, t_h3, kind_dbg)
        if "E" in stages:
            I.update({"FLAGS": inp("FLAGS", [128, 12]), "ssm_norm_ffn": inp("ssm_norm_ffn", [1, D]),
                      "moe_router": inp("moe_router", [D, 8]), "moe_wg": inp("moe_wg", [D, 7168]),
                      "moe_wu": inp("moe_wu", [D, 7168]), "moe_wd": inp("moe_wd", [7168, D]),
                      "final_norm": inp("final_norm", [1, D])})
            I["ssm_norm_ffn"] = I["ssm_norm_ffn"][0:1, :]
            I["final_norm"] = I["final_norm"][0:1, :]
            outs["y_p"] = outp("y_p", [TP, D])
            outs["y_s"] = outp("y_s", [4, D])
            moe_stage(P, C, H3, I, outs, TP, NT, t_h3, n_cores, kind_dbg)
        P.flush(final=True)
        build.total = P.total
    return nc


_NC_CACHE = {}
STAGES = "AKGNSFML"


def _rope_table(TP):
    pos = np.concatenate([np.arange(TP), PAST + np.arange(128)]).astype(np.float32)
    inv = (1.0 / (10000.0 ** (np.arange(64, dtype=np.float32) / 64))).astype(np.float32)
    ang = pos[:, None] * inv[None, :]
    return np.concatenate([np.cos(ang), np.sin(ang)], axis=1).astype(np.float32)


def kernel(**inputs):
    f = lambda k: np.asarray(inputs[k])
    x_prompt, x_sample = f("x_prompt"), f("x_sample")
    B, TP, _ = x_prompt.shape
    NB = x_sample.shape[0]
    key = (TP, STAGES)
    I_NPHYS[0] = f("cache_cmp_kv").shape[1]
    if key not in _NC_CACHE:
        _NC_CACHE[key] = build(TP=TP, stages=STAGES, n_cores=8)
    nc = _NC_CACHE[key]
    TT = TP + 128
    ca = np.ascontiguousarray
    rope = _rope_table(TP)
    vmask = np.zeros((TT, 1), np.float32)
    vmask[:TP + 4] = 1
    caus, msel = host_nsa_consts(TP)
    gcw = f("hyb_gdn_conv_w")[0]
    scw = f("ssm_conv_w")[0]
    scb = f("ssm_conv_b")[0]
    ssm_cw = np.concatenate([scw.reshape(4, 48, 128).transpose(2, 1, 0), scb.reshape(48, 128).T[:, :, None]], axis=2)
    swi = f("ssm_w_in")[0]
    shared = {
        "hyb_norm_mix": ca(f("hyb_norm_mix")[0][None]), "hyb_w_in": ca(f("hyb_w_in")[0]), "ROPE": rope,
        "GCONST": host_consts(), "gdn_cw": ca(gcw.reshape(4, 3, 8, 128).transpose(3, 2, 1, 0)),
        "gdn_a_log": ca(f("hyb_gdn_a_log")[0][None]), "gdn_dt_bias": ca(f("hyb_gdn_dt_bias")[0][None]),
        "gdn_norm_w": ca(f("hyb_gdn_norm_w")[0][None]), "VMASK": vmask,
        "cmp_w1": ca(f("hyb_cmp_w1")[0]), "cmp_w2": ca(f("hyb_cmp_w2")[0]),
        "cmp_peT": ca(f("hyb_cmp_pe")[0].transpose(0, 2, 1)), "CAUS": caus, "MSEL_P": msel,
        "hyb_w_out": ca(f("hyb_w_out")[0]), "ffn_w_gate": ca(f("ffn_w_gate")[0]), "ffn_w_up": ca(f("ffn_w_up")[0]),
        "ffn_w_down": ca(f("ffn_w_down")[0]), "hyb_norm_ffn": ca(f("hyb_norm_ffn")[0][None]),
        "ssm_w_in_r": ca(np.concatenate([swi[:, 4096:10240], swi[:, :4096], swi[:, 10240:]], axis=1)),
        "ssm_norm_mix": ca(f("ssm_norm_mix")[0][None]), "SCONST": host_ssd_consts(), "ssm_cw": ca(ssm_cw.astype(np.float32)),
        "ssm_a_log": ca(f("ssm_a_log")[0][None]), "ssm_dt_bias": ca(f("ssm_dt_bias")[0][None]),
        "ssm_d_skip": ca(f("ssm_d_skip")[0][None]), "ssm_norm_w": ca(f("ssm_norm_w")[0][None]),
        "ssm_w_out": ca(f("ssm_w_out")[0]), "ssm_norm_ffn": ca(f("ssm_norm_ffn")[0][None]),
        "moe_router": ca(f("moe_router")[0]), "final_norm": ca(f("final_norm")[None]),
        "IOTA": np.arange(128, dtype=np.float32)[:, None],
        "pool_cmp": f("cache_cmp_kv")[0].reshape(-1, 512), "pool_sel": f("cache_sel_kv")[0].reshape(-1, 512),
        "MSEL_S": host_msel((PAST + 4 - 32) // 16 + 1, -(-((PAST + 4 - 32) // 16 + 1) // 128) * 128, (PAST + 128) // 64),
    }
    in_maps = []
    for c in range(8):
        xcat = np.zeros((TT, D), np.float32)
        xcat[:TP] = x_prompt[c % B]
        xcat[TP:TP + 4] = x_sample[c]
        fl = np.zeros((128, 12), np.float32)
        fl[:, 0] = float(c == 0)
        fl[:, 1] = float(c == 1)
        fl[:, 2 + c] = 1.0
        fl[:, 10 + (c % 2)] = 1.0
        m = dict(shared)
        nsplit = 4 if (TP // 128) % 4 == 0 else (2 if (TP // 128) % 2 == 0 else 1)
        NQ = TP // 128 // nsplit
        q = (c // B) % nsplit
        qidx = (q * NQ * 128 + np.arange(NQ)[None, :] * 128 + np.arange(128)[:, None]).astype(np.int32)
        m["QIDX"] = qidx
        m.update({"xcat": xcat, "win_cache": ca(f("cache_win_kv")[0, c].reshape(512, 512)),
                  "gdn_state": ca(f("state_gdn")[0, c]), "gdn_convT": ca(f("state_gdn_conv")[0, c].T),
                  "ssm_state": ca(f("state_ssm")[0, c]), "ssm_convT": ca(f("state_ssm_conv")[0, c].T),
                  "pt_row": ca(f("page_table")[c][None].astype(np.int32)),
                  "FLAGS": fl, "moe_wg": f("moe_w_gate")[0], "moe_wu": f("moe_w_up")[0],
                  "moe_wd": f("moe_w_down")[0]})
        m = {k: v for k, v in m.items() if k in build.inputs}
        in_maps.append(m)
    res = run_bass_kernel_spmd(nc, in_maps, core_ids=list(range(8))).results
    WP = min(512, TP)
    pk = lambda name, shp: np.stack([res[b][name].reshape(shp) for b in range(B)])
    sk = lambda name, shp: np.stack([res[c][name].reshape(shp) for c in range(NB)])
    nsplit = 4 if (TP // 128) % 4 == 0 else (2 if (TP // 128) % 2 == 0 else 1)
    QN = TP // nsplit
    y_p = np.zeros((B, TP, D), np.float32)
    for c in range(8):
        b, q = c % B, (c // B) % nsplit
        y_p[b, q * QN:(q + 1) * QN] = res[c]["y_q"].reshape(QN, D)
    outs = (
        y_p, sk("y_s", (4, D)),
        pk("cmp_p", (TP, 2, 2, 128))[None], sk("cmp_s", (4, 2, 2, 128))[None],
        pk("sel_p", (TP, 2, 2, 128))[None], sk("sel_s", (4, 2, 2, 128))[None],
        pk("win_p", (WP, 2, 2, 128))[None], sk("win_s", (512, 2, 2, 128))[None],
        pk("gconv_p", (3, NFM))[None], sk("gconv_s", (3, NFM))[None],
        pk("gst_p", (8, 128, 128))[None], sk("gst_s", (8, 128, 128))[None],
        pk("sconv_p", (3, 6144))[None], sk("sconv_s", (3, 6144))[None],
        pk("sst_p", (64, 64, 128))[None], sk("sst_s", (64, 64, 128))[None],
    )
    return outs
```
